# Optimizing a Trainium2 kernel written in Bass

```python
import jax, jax.numpy as jnp
from jax import lax
import numpy as np

D_MODEL = 1024
BATCH = 16
SEQ = 4096
DEPTH = 1

CONV_WIDTH = 512
CONV_K = 3
N_HEADS = 8
HEAD_DIM = 64
ATTN_WIDTH = N_HEADS * HEAD_DIM
Q_BLOCK = 128
N_GROUPS = 4
EXPERTS_PER_GROUP = 8
N_EXPERTS = N_GROUPS * EXPERTS_PER_GROUP
TOP_K_IN_GROUP = 2
D_EXPERT = 256
EPS = 1e-6
IN_COLS = 3 * CONV_WIDTH + 3 * ATTN_WIDTH + N_HEADS + 2 * D_MODEL

kernel_name = "hybrid_shortconv_fox_hmoe_block"


def rmsnorm(x, w):
    xf = x.astype(jnp.float32)
    y = xf * lax.rsqrt(jnp.mean(xf * xf, axis=-1, keepdims=True) + EPS)
    return (y * w.astype(jnp.float32)).astype(x.dtype)


def modulate(x, norm_w, shift, scale):
    return rmsnorm(x, norm_w) * (1 + scale[:, None, :]) + shift[:, None, :]


def causal_depthwise_conv(u, w):
    S = u.shape[1]
    up = jnp.pad(u, ((0, 0), (CONV_K - 1, 0), (0, 0)))
    out = w[0] * up[:, 0:S]
    for j in range(1, CONV_K):
        out = out + w[j] * up[:, j:j + S]
    return out


def forgetting_attention(q, k, v, log_f):
    B, H, S, Dh = q.shape
    nblk = S // Q_BLOCK
    F = jnp.cumsum(log_f, axis=-1)
    scale = HEAD_DIM ** -0.5
    k_pos = jnp.arange(S)
    q_blocks = q.reshape(B, H, nblk, Q_BLOCK, Dh).transpose(2, 0, 1, 3, 4)
    F_blocks = F.reshape(B, H, nblk, Q_BLOCK).transpose(2, 0, 1, 3)

    def one_block(args):
        i, q_i, F_i = args
        s = jnp.einsum('bhqd,bhkd->bhqk', q_i, k,
                       preferred_element_type=jnp.float32) * scale
        s = s + (F_i[..., :, None] - F[..., None, :])
        q_pos = i * Q_BLOCK + jnp.arange(Q_BLOCK)
        s = jnp.where(k_pos[None, :] <= q_pos[:, None], s, -jnp.inf)
        p = jax.nn.softmax(s, axis=-1)
        return jnp.einsum('bhqk,bhkd->bhqd', p.astype(v.dtype), v)

    o = lax.map(one_block, (jnp.arange(nblk), q_blocks, F_blocks))
    return o.transpose(1, 0, 3, 2, 4).reshape(B, S, H * Dh)


def hierarchical_moe(h, w_rg, b_rg, w_re, b_re, w_gate, w_up, w_down):
    B, S, _ = h.shape
    lg = (h @ w_rg).astype(jnp.float32) + b_rg.astype(jnp.float32)
    p_g = jax.nn.softmax(lg, axis=-1)
    g_idx = jnp.argmax(lg, axis=-1)
    p_sel = jnp.take_along_axis(p_g, g_idx[..., None], axis=-1)
    le = ((h @ w_re).astype(jnp.float32) + b_re.astype(jnp.float32)
          ).reshape(B, S, N_GROUPS, EXPERTS_PER_GROUP)
    le_g = jnp.take_along_axis(le, g_idx[..., None, None], axis=2)[:, :, 0]
    top_v, top_i = lax.top_k(le_g, TOP_K_IN_GROUP)
    w_k = jax.nn.softmax(top_v, axis=-1) * p_sel
    e_idx = g_idx[..., None] * EXPERTS_PER_GROUP + top_i
    comb = jnp.sum(jax.nn.one_hot(e_idx, N_EXPERTS, dtype=jnp.float32)
                   * w_k[..., None], axis=-2).astype(h.dtype)
    out = jnp.zeros_like(h)
    for e in range(N_EXPERTS):
        a = jax.nn.silu(h @ w_gate[e]) * (h @ w_up[e])
        out = out + comb[..., e:e + 1] * (a @ w_down[e])
    return out


def hybrid_layer(x, c, w_ada, b_ada, norm1_w, w_in, b_forget, conv_w,
                 q_norm_w, k_norm_w, w_out_conv, w_out_attn, w_o, norm2_w,
                 w_router_group, b_router_group, w_router_expert, b_router_expert,
                 w_gate, w_up, w_down):
    B, S, _ = x.shape
    mod = jax.nn.silu(c) @ w_ada + b_ada
    shift1, scale1, gate1, shift2, scale2, gate2 = jnp.split(mod, 6, axis=-1)

    h = modulate(x, norm1_w, shift1, scale1)
    proj = h @ w_in
    sizes = [CONV_WIDTH, CONV_WIDTH, CONV_WIDTH, ATTN_WIDTH, ATTN_WIDTH,
             ATTN_WIDTH, N_HEADS, D_MODEL]
    cuts = [int(v) for v in np.cumsum(sizes)]
    x_in, conv_b, conv_c, q, k, v, f_logit, gate_conv_l, gate_attn_l = jnp.split(
        proj, cuts, axis=-1)

    y_a = conv_b * causal_depthwise_conv(conv_c * x_in, conv_w)
    p_a = y_a @ w_out_conv

    q = rmsnorm(q.reshape(B, S, N_HEADS, HEAD_DIM), q_norm_w).transpose(0, 2, 1, 3)
    k = rmsnorm(k.reshape(B, S, N_HEADS, HEAD_DIM), k_norm_w).transpose(0, 2, 1, 3)
    v = v.reshape(B, S, N_HEADS, HEAD_DIM).transpose(0, 2, 1, 3)
    log_f = jax.nn.log_sigmoid((f_logit + b_forget).astype(jnp.float32)).transpose(0, 2, 1)
    y_b = forgetting_attention(q, k, v, log_f)
    p_b = y_b @ w_out_attn

    merged = jax.nn.sigmoid(gate_conv_l) * p_a + jax.nn.sigmoid(gate_attn_l) * p_b
    x = x + gate1[:, None, :] * (merged @ w_o)

    h2 = modulate(x, norm2_w, shift2, scale2)
    moe = hierarchical_moe(h2, w_router_group, b_router_group, w_router_expert,
                           b_router_expert, w_gate, w_up, w_down)
    return x + gate2[:, None, :] * moe


def setup_inputs(seed: int = 0) -> dict:
    key = jax.random.key(seed)
    ks = jax.random.split(key, 22)
    f32 = jnp.float32
    L = DEPTH

    def nrm(k, shape, scale):
        return jax.random.normal(k, shape, f32) * scale

    return {
        "x": nrm(ks[0], (BATCH, SEQ, D_MODEL), 1.0),
        "c": nrm(ks[1], (BATCH, D_MODEL), 1.0),
        "w_ada": nrm(ks[2], (L, D_MODEL, 6 * D_MODEL), 0.5 * D_MODEL ** -0.5),
        "b_ada": nrm(ks[3], (L, 6 * D_MODEL), 0.02),
        "norm1_w": 1.0 + nrm(ks[4], (L, D_MODEL), 0.02),
        "w_in": nrm(ks[5], (L, D_MODEL, IN_COLS), D_MODEL ** -0.5),
        "b_forget": 4.0 + nrm(ks[6], (L, N_HEADS), 0.5),
        "conv_w": nrm(ks[7], (L, CONV_K, CONV_WIDTH), CONV_K ** -0.5),
        "q_norm_w": 1.0 + nrm(ks[8], (L, HEAD_DIM), 0.02),
        "k_norm_w": 1.0 + nrm(ks[9], (L, HEAD_DIM), 0.02),
        "w_out_conv": nrm(ks[10], (L, CONV_WIDTH, D_MODEL), CONV_WIDTH ** -0.5),
        "w_out_attn": nrm(ks[11], (L, ATTN_WIDTH, D_MODEL), ATTN_WIDTH ** -0.5),
        "w_o": nrm(ks[12], (L, D_MODEL, D_MODEL), D_MODEL ** -0.5),
        "norm2_w": 1.0 + nrm(ks[13], (L, D_MODEL), 0.02),
        "w_router_group": nrm(ks[14], (L, D_MODEL, N_GROUPS), D_MODEL ** -0.5),
        "b_router_group": nrm(ks[15], (L, N_GROUPS), 0.01),
        "w_router_expert": nrm(ks[16], (L, D_MODEL, N_EXPERTS), D_MODEL ** -0.5),
        "b_router_expert": nrm(ks[17], (L, N_EXPERTS), 0.01),
        "w_gate": nrm(ks[18], (L, N_EXPERTS, D_MODEL, D_EXPERT), D_MODEL ** -0.5),
        "w_up": nrm(ks[19], (L, N_EXPERTS, D_MODEL, D_EXPERT), D_MODEL ** -0.5),
        "w_down": nrm(ks[20], (L, N_EXPERTS, D_EXPERT, D_MODEL), D_EXPERT ** -0.5),
    }


def reference(x, c, w_ada, b_ada, norm1_w, w_in, b_forget, conv_w, q_norm_w,
              k_norm_w, w_out_conv, w_out_attn, w_o, norm2_w, w_router_group,
              b_router_group, w_router_expert, b_router_expert, w_gate, w_up,
              w_down):
    for l in range(DEPTH):
        x = hybrid_layer(x, c, w_ada[l], b_ada[l], norm1_w[l], w_in[l], b_forget[l],
                         conv_w[l], q_norm_w[l], k_norm_w[l], w_out_conv[l],
                         w_out_attn[l], w_o[l], norm2_w[l], w_router_group[l],
                         b_router_group[l], w_router_expert[l], b_router_expert[l],
                         w_gate[l], w_up[l], w_down[l])
    return x
```

```python
import contextlib
import numpy as np
import ml_dtypes
import concourse.bass as bass
import concourse.mybir as mybir
from concourse.bass_utils import run_bass_kernel_spmd
from concourse.alu_op_type import AluOpType as ALU

F32 = mybir.dt.float32
BF16 = mybir.dt.bfloat16
AF = mybir.ActivationFunctionType
AX = mybir.AxisListType

D = 1024
KC = 8
NH = 8
HD = 64
NE = 32
FE = 256
IN_COLS = 5128
EPS = 1e-6
COMPUTE = ("pe", "act", "dve", "pool")


class Buf:
    __slots__ = ("name", "w", "r")

    ALL = []

    def __init__(self, name):
        self.name = name
        self.w = None
        self.r = []
        Buf.ALL.append(self)


class Sched:
    def __init__(self, nc, tag):
        self.nc = nc
        self.tag = tag
        self.streams = {k: [] for k in ("pe", "act", "dve", "pool", "sp")}
        self.cnt = {}
        self.waited = {}
        for b in Buf.ALL:
            b.w = None
            b.r = []

    def _deps(self, eng, reads, writes):
        deps = []
        for b in reads:
            if b.w is not None:
                deps.append(b.w)
        for b in writes:
            if b.w is not None:
                deps.append(b.w)
            deps.extend(b.r)
        out = {}
        for (sk, val, e2) in deps:
            if eng == "pe" and e2 == "pe":
                continue
            if self.waited.get((eng, sk), 0) >= val:
                continue
            if out.get(sk, 0) < val:
                out[sk] = val
        for sk, val in out.items():
            self.waited[(eng, sk)] = val
        return list(out.items())

    def op(self, eng, fn, reads=(), writes=(), chan=None, ndma=1):
        reads = [b for b in reads if b is not None]
        writes = [b for b in writes if b is not None]
        waits = self._deps(eng, reads, writes)
        if chan is not None:
            sk = ("dma", chan)
            prev = self.cnt.get(sk, 0)
            if prev > 0 and self.waited.get((eng, sk), 0) < prev:
                waits.append((sk, prev))
                self.waited[(eng, sk)] = prev
            val = prev + 16 * ndma
            inc = 16
        else:
            sk = ("eng", eng)
            val = self.cnt.get(sk, 0) + 1
            inc = 1
        self.cnt[sk] = val
        ev = (sk, val, eng if chan is None else "dma")
        self.streams[eng].append((waits, fn, sk, inc))
        for b in writes:
            b.w = ev
            b.r = []
        for b in reads:
            if b not in writes:
                b.r.append(ev)
        return ev

    def fence(self, eng, prefix):
        waits = []
        for sk, v in self.cnt.items():
            if sk[0] == "dma" and str(sk[1]).startswith(prefix) and self.waited.get((eng, sk), 0) < v:
                waits.append((sk, v))
                self.waited[(eng, sk)] = v
        self.streams[eng].append((waits, None, None, 0))

    def finish(self):
        waits = [(sk, v) for sk, v in self.cnt.items() if sk[0] == "dma"]
        self.streams["sp"].append((waits, None, None, 0))
        nc = self.nc
        with contextlib.ExitStack() as st:
            sems = {}
            for i, sk in enumerate(self.cnt.keys()):
                sems[sk] = st.enter_context(nc.semaphore("%s_s%d" % (self.tag, i)))
            block = st.enter_context(nc.Block())

            def run(stream):
                def body(e):
                    for (waits, fn, sk, inc) in stream:
                        for (wsk, wval) in waits:
                            e.wait_ge(sems[wsk], wval)
                        if fn is None:
                            continue
                        r = fn(e)
                        if isinstance(r, (list, tuple)):
                            for ins in r:
                                ins.then_inc(sems[sk], inc)
                        else:
                            r.then_inc(sems[sk], inc)
                return body

            for name, attr in (("pe", "tensor"), ("act", "scalar"), ("dve", "vector"),
                               ("pool", "gpsimd"), ("sp", "sync")):
                if self.streams[name]:
                    getattr(block, attr)(run(self.streams[name]))


class Ring:
    def __init__(self, items):
        self.items = items
        self.i = 0

    def next(self):
        it = self.items[self.i % len(self.items)]
        self.i += 1
        return it


class Ops:
    def __init__(self, S):
        self.S = S

    def mm(self, out, lhsT, rhs, start, stop, reads, writes):
        self.S.op("pe", lambda e: e.matmul(out, lhsT=lhsT, rhs=rhs, start=start, stop=stop),
                  reads, writes)

    def tr(self, out, in_, ident, reads, writes):
        self.S.op("pe", lambda e: e.transpose(out, in_, ident), reads, writes)

    def act(self, out, in_, func, reads, writes, bias=None, scale=None, accum=None):
        kw = {}
        if bias is not None:
            kw["bias"] = bias
        if scale is not None:
            kw["scale"] = scale
        if accum is not None:
            kw["accum_out"] = accum
        self.S.op("act", lambda e: e.activation(out=out, in_=in_, func=func, **kw), reads, writes)

    def ts(self, eng, out, in0, s1, s2, op0, op1, reads, writes):
        if op1 is None:
            self.S.op(eng, lambda e: e.tensor_scalar(out=out, in0=in0, scalar1=s1, scalar2=None, op0=op0),
                      reads, writes)
        else:
            self.S.op(eng, lambda e: e.tensor_scalar(out=out, in0=in0, scalar1=s1, scalar2=s2, op0=op0, op1=op1),
                      reads, writes)

    def tt(self, eng, out, in0, in1, op, reads, writes):
        self.S.op(eng, lambda e: e.tensor_tensor(out=out, in0=in0, in1=in1, op=op), reads, writes)

    def stt(self, out, in0, scalar, in1, op0, op1, reads, writes):
        self.S.op("dve", lambda e: e.scalar_tensor_tensor(out=out, in0=in0, scalar=scalar, in1=in1,
                                                          op0=op0, op1=op1), reads, writes)

    def copy(self, eng, out, in_, reads, writes):
        self.S.op(eng, lambda e: e.tensor_copy(out=out, in_=in_), reads, writes)

    def memset(self, eng, ap, val, writes):
        self.S.op(eng, lambda e: e.memset(ap, val), (), writes)

    def dma(self, q, out, in_, reads, writes, chan):
        self.S.op(q, lambda e: e.dma_start(out=out, in_=in_), reads, writes, chan=chan)

    def dman(self, q, pairs, reads, writes, chan):
        self.S.op(q, lambda e: [e.dma_start(out=o, in_=i) for (o, i) in pairs], reads, writes, chan=chan,
                  ndma=len(pairs))


def build_nc(NB, S, debug=False, run_b=True, taps=()):
    assert S % 512 == 0
    NCH = S // 512
    NBLK = S // 128
    NTOK = NB * S
    TB = min(2048, S)
    nc = bass.Bass("TRN2", target_bir_lowering=False)
    Buf.ALL = []

    def din(name, shape, dt=F32):
        return nc.dram_tensor(name, list(shape), dt, kind="ExternalInput").ap()

    x_d = din("x", [NTOK, D])
    cT_d = din("cT", [128, KC, NB])
    wada_d = din("w_ada", [D, 6 * D])
    badaT_d = din("b_adaT", [128, 48])
    n1w_d = din("n1wT", [128, KC])
    n2w_d = din("n2wT", [128, KC])
    win_d = din("w_in", [D, IN_COLS])
    bfg_d = din("bfg", [128, NH])
    convT_d = din("convT", [128, 4, 3])
    qkw_d = din("qkw", [128, 2])
    woc_d = din("w_oc", [512, D])
    woa_d = din("w_oa", [512, D])
    wo_d = din("w_o", [D, D])
    wr_d = din("w_r", [D, 36])
    br_d = din("b_r", [128, 36])
    wg_d = din("w_gate", [NE, D, FE])
    wu_d = din("w_up", [NE, D, FE])
    wd_d = din("w_down", [NE, FE, D])
    consts_d = din("consts", [128, 6, 128])
    jv_d = din("jv", [128, (2 * NTOK) // 512 + NE + 1])
    zeros_d = din("zeros_bf", [1024, D], BF16)
    out_d = nc.dram_tensor("out", [NTOK, D], F32, kind="ExternalOutput").ap()

    win_bf = nc.dram_tensor("win_bf", [D, IN_COLS], BF16).ap()
    woc_bf = nc.dram_tensor("woc_bf", [512, D], BF16).ap()
    woa_bf = nc.dram_tensor("woa_bf", [512, D], BF16).ap()
    wos_bf = nc.dram_tensor("wos_bf", [NB, D, D], BF16).ap()
    W1 = nc.dram_tensor("W1_bf", [NE * 128, 4096], BF16).ap()
    W2 = nc.dram_tensor("W2_bf", [NE * 128, 2048], BF16).ap()

    NST_ = (2 * NTOK) // 512 + NE
    Xs = nc.dram_tensor("Xs", [NST_ * 512, D], BF16).ap()
    dbg_outs = {}
    tap_state = {"S": None}

    def dtap(name, ap, bufs, shape, dt):
        if name not in taps or name in dbg_outs:
            return
        t = nc.dram_tensor("dbg_" + name, list(shape), dt, kind="ExternalOutput").ap()
        dbg_outs[name] = t
        tap_state["S"].op("sp", lambda e: e.dma_start(out=t, in_=ap), bufs, (), chan="dbg_" + name)

    with contextlib.ExitStack() as top:
        def sbt(ctx, name, shape, dt):
            return ctx.enter_context(nc.sbuf_tensor(name, list(shape), dt))

        constsF = sbt(top, "constsF", [128, 6, 128], F32)
        identF = constsF[:, 0, :]
        triF = constsF[:, 1, :]
        elastF = constsF[:, 2, :]
        onesF = constsF[:, 4, :]
        identB = sbt(top, "identB", [128, 128], BF16)
        triB = sbt(top, "triB", [128, 128], BF16)
        bonesB = sbt(top, "bonesB", [128, 128], BF16)
        diagW = sbt(top, "diagW", [128, 12, 128], BF16)
        modT = sbt(top, "modT", [128, 48, NB], F32)
        A1 = sbt(top, "A1", [128, NB, KC], F32)
        B1 = sbt(top, "B1", [128, NB, KC], F32)
        A2 = sbt(top, "A2", [128, NB, KC], F32)
        B2 = sbt(top, "B2", [128, NB, KC], F32)
        qkws = sbt(top, "qkws", [128, 2], F32)
        bfg = sbt(top, "bfg_sb", [128, NH], F32)
        wfB = sbt(top, "wfB", [128, KC, NH], BF16)
        wrB = sbt(top, "wrB", [128, KC, 36], BF16)
        brt = sbt(top, "brt", [128, 36], F32)
        psum = [top.enter_context(nc.psum_tensor("ps%d" % i, [128, 512], F32)) for i in range(8)]
        pbuf = [Buf("ps%d" % i) for i in range(8)]
        bconst = Buf("consts")
        bmoe_w = [Buf("moew%d" % e) for e in range(NE)]
        b_winbf = Buf("winbf")
        b_wos = Buf("wosbf")
        b_outd = [Buf("out%d" % i) for i in range(NTOK // 128)]

        with contextlib.ExitStack() as ph:
            S_ = Sched(nc, "P")
            tap_state["S"] = S_
            O = Ops(S_)
            cT = sbt(ph, "cT_sb", [128, KC, NB], F32)
            sc = sbt(ph, "sc_sb", [128, KC, NB], F32)
            th = sbt(ph, "th_sb", [128, KC, NB], F32)
            badaT = sbt(ph, "badaT_sb", [128, 48], F32)
            n1w = sbt(ph, "n1w_sb", [128, KC], F32)
            n2w = sbt(ph, "n2w_sb", [128, KC], F32)
            convT = sbt(ph, "convT_sb", [128, 4, 3], F32)
            qkw = sbt(ph, "qkw_sb", [128, 2], F32)
            wfF = sbt(ph, "wfF", [128, KC, NH], F32)
            wrF = sbt(ph, "wrF", [128, KC, 36], F32)
            wa = [sbt(ph, "wa%d" % i, [128, KC, 768], F32) for i in range(2)]
            bwa = [Buf("wa%d" % i) for i in range(2)]
            woF = sbt(ph, "woF", [128, KC, D], F32)
            g1bc = sbt(ph, "g1bc", [128, NB, D], F32)
            diag = [sbt(ph, "diag%d" % i, [128, 128], F32) for i in range(2)]
            bdiag = [Buf("diag%d" % i) for i in range(2)]
            wtmp = [sbt(ph, "wtmp%d" % i, [128, D], BF16) for i in range(2)]
            bwtmp = [Buf("wtmp%d" % i) for i in range(2)]
            b_small = Buf("small_in")
            b_sc = Buf("sc")
            b_woF = Buf("woF")
            b_g1 = Buf("g1bc")
            b_modps = pbuf[0]

            ci = 0
            for r in range(8):
                O.dma("pool", win_bf[r * 128:(r + 1) * 128, :], win_d[r * 128:(r + 1) * 128, :], (), [b_winbf],
                      "cast%d" % (ci % 8)); ci += 1
            for r in range(4):
                O.dma("pool", woc_bf[r * 128:(r + 1) * 128, :], woc_d[r * 128:(r + 1) * 128, :], (), [b_winbf],
                      "cast%d" % (ci % 8)); ci += 1
                O.dma("pool", woa_bf[r * 128:(r + 1) * 128, :], woa_d[r * 128:(r + 1) * 128, :], (), [b_winbf],
                      "cast%d" % (ci % 8)); ci += 1

            nsm = [0]
            for dst, src in ((constsF[:], consts_d[:, :, :]), (cT[:], cT_d[:, :, :]), (badaT[:], badaT_d[:, :]),
                             (n1w[:], n1w_d[:, :]), (n2w[:], n2w_d[:, :]), (convT[:], convT_d[:, :, :]),
                             (qkw[:], qkw_d[:, :]), (bfg[:], bfg_d[:, :]), (brt[:], br_d[:, :])):
                O.dma("sp", dst, src, (), [b_small], "small%d" % nsm[0]); nsm[0] += 1
            O.dma("sp", wfF[:], win_d.rearrange("(kc p) n -> p kc n", p=128)[:, :, 3072:3080], (), [b_small], "smallA")
            O.dma("sp", wrF[:], wr_d.rearrange("(kc p) n -> p kc n", p=128), (), [b_small], "smallB")
            O.dma("sp", woF[:], wo_d.rearrange("(kc p) n -> p kc n", p=128), (), [b_woF], "woF")

            O.act(th[:], cT[:], AF.Tanh, [b_small], [b_sc], scale=0.5)
            O.stt(sc[:], th[:], 1.0, cT[:], ALU.add, ALU.mult, [b_small, b_sc], [b_sc])
            O.ts("dve", sc[:], sc[:], 0.5, None, ALU.mult, None, [b_sc], [b_sc])
            O.copy("dve", identB[:], identF, [b_small], [bconst])
            O.copy("dve", triB[:], triF, [b_small], [bconst])
            O.copy("dve", bonesB[:], constsF[:, 3, :], [b_small], [bconst])
            O.copy("dve", wfB[:], wfF[:], [b_small], [bconst])
            O.copy("dve", wrB[:], wrF[:], [b_small], [bconst])
            for j in range(4):
                for tap in range(3):
                    O.ts("dve", diagW[:, j * 3 + tap, :], identF, convT[:, j, tap:tap + 1], None, ALU.mult, None,
                         [b_small], [bconst])
            O.ts("dve", qkws[:, 0:1], qkw[:, 0:1], HD ** -0.5, None, ALU.mult, None, [b_small], [bconst])
            O.copy("dve", qkws[:, 1:2], qkw[:, 1:2], [b_small], [bconst])

            wada_v = wada_d.rearrange("(kc p) n -> p kc n", p=128)
            modps = psum[0]
            for g in range(8):
                w_, bw_ = wa[g % 2], bwa[g % 2]
                O.dma("sp", w_[:], wada_v[:, :, g * 768:(g + 1) * 768], (), [bw_], "wa%d" % (g % 2))
                for jj in range(6):
                    col = g * 6 + jj
                    for kc in range(KC):
                        O.mm(modps[:, col * NB:(col + 1) * NB], w_[:, kc, jj * 128:(jj + 1) * 128], sc[:, kc, :],
                             kc == 0, kc == KC - 1, [bw_, b_sc], [b_modps])
            O.tt("dve", modT[:], modps[:, 0:48 * NB].rearrange("p (a b) -> p a b", b=NB),
                 badaT[:, :, None].broadcast_to([128, 48, NB]), ALU.add, [b_modps, b_small], [bconst])
            for b in range(NB):
                O.stt(A1[:, b, :], modT[:, 8:16, b], 1.0, n1w[:], ALU.add, ALU.mult, [bconst, b_small], [bconst])
                O.copy("dve", B1[:, b, :], modT[:, 0:8, b], [bconst], [bconst])
                O.stt(A2[:, b, :], modT[:, 32:40, b], 1.0, n2w[:], ALU.add, ALU.mult, [bconst, b_small], [bconst])
                O.copy("dve", B2[:, b, :], modT[:, 24:32, b], [bconst], [bconst])
            k = 0
            for b in range(NB):
                for half in range(2):
                    pb_, bb_ = psum[1 + half], pbuf[1 + half]
                    for q in range(4):
                        kc = half * 4 + q
                        dg, bdg = diag[k % 2], bdiag[k % 2]
                        k += 1
                        O.ts("dve", dg[:], identF, modT[:, 16 + kc, b:b + 1], 0.5, ALU.mult, ALU.mult,
                             [bconst, b_small], [bdg])
                        O.mm(pb_[:, q * 128:(q + 1) * 128], onesF, dg[:], True, True, [b_small, bdg], [bb_])
                    O.copy("dve", g1bc[:, b, half * 512:(half + 1) * 512], pb_[:, :], [bb_], [b_g1])
            k = 0
            for b in range(NB):
                for kc in range(KC):
                    wt, bwt = wtmp[k % 2], bwtmp[k % 2]
                    k += 1
                    O.tt("pool", wt[:], woF[:, kc, :], g1bc[:, b, :], ALU.mult, [b_woF, b_g1], [bwt])
                    O.dma("sp", wos_bf[b, kc * 128:(kc + 1) * 128, :], wt[:], [bwt], [b_wos], "wos%d" % (k % 2))
            dtap("modT", modT[:], [bconst], [128, 48, NB], F32)
            dtap("A1", A1[:], [bconst], [128, NB, KC], F32)
            dtap("g1bc", g1bc[:], [b_g1], [128, NB, D], F32)
            S_.finish()

        with contextlib.ExitStack() as ph:
            S_ = Sched(nc, "A")
            tap_state["S"] = S_
            O = Ops(S_)
            Kc = sbt(ph, "Kc", [128, 4, S], BF16)
            Vc = sbt(ph, "Vc", [128, NBLK, NH, HD + 1], BF16)
            Gall = sbt(ph, "Gall", [128, NBLK, NH], F32)
            nbias = sbt(ph, "nbias", [128, NBLK, NH], F32)
            bKc = [Buf("Kc%d" % i) for i in range(NCH)]
            bVc = [Buf("Vc%d" % i) for i in range(NCH)]
            bVones = Buf("Vones")
            bGall = [Buf("Gall%d" % i) for i in range(NBLK)]
            bnbias = Buf("nbias")
            NSLOT = 4
            wslot = [sbt(ph, "wslot%d" % i, [128, 4096], BF16) for i in range(NSLOT)]
            bslot = [Buf("wslot%d" % i) for i in range(NSLOT)]
            slots = Ring(list(range(NSLOT)))
            xr = [sbt(ph, "xr%d" % i, [128, D], F32) for i in range(2)]
            xring = Ring([(xr[i], Buf("xr%d" % i), "xr%d" % i) for i in range(2)])
            onesB = sbt(ph, "onesB", [128, 64], BF16)
            rlb = [sbt(ph, "rlb%d" % i, [128, 512], BF16) for i in range(2)]
            brlb = [Buf("rlb%d" % i) for i in range(2)]
            x1ts = [sbt(ph, "x1t%d" % i, [128, D], F32) for i in range(2)]
            x1ring = Ring([(x1ts[i], Buf("x1t%d" % i), "x1st%d" % i) for i in range(2)])
            xn = [sbt(ph, "xn%d" % i, [128, D], BF16) for i in range(4)]
            bxn = [Buf("xn%d" % i) for i in range(4)]
            ss = sbt(ph, "ss", [128, 4], F32); bss = Buf("ss")
            lnv = sbt(ph, "lnv", [128, 4], F32)
            rstd = sbt(ph, "rstd", [128, 4], F32); brstd = Buf("rstd")
            hTs = [(sbt(ph, "hT%d" % i, [128, KC, 512], BF16), Buf("hT%d" % i)) for i in range(2)]
            xin_sb = sbt(ph, "xin_sb", [128, 4, 512], BF16); bxin = Buf("xin")
            b_sb = sbt(ph, "b_sb", [128, 4, 512], BF16); bbsb = Buf("bsb")
            ub = [sbt(ph, "ub%d" % j, [128, 514], BF16) for j in range(4)]
            bub = [Buf("ub%d" % j) for j in range(4)]
            yaT = sbt(ph, "yaT", [128, 4, 512], BF16); byaT = Buf("yaT")
            sq = sbt(ph, "sq", [128, 512], BF16); bsq = Buf("sq")
            rsb = [sbt(ph, "rsb%d" % i, [128, 512], F32) for i in range(1)]
            brsb = [Buf("rsb%d" % i) for i in range(1)]
            rsring = Ring(list(range(1)))
            qT = sbt(ph, "qTz", [128, NH, 512], BF16); bqT = Buf("qT")
            PT = [sbt(ph, "PT%d" % i, [128, 512], BF16) for i in range(4)]
            PTring = Ring([(PT[i], Buf("PT%d" % i)) for i in range(4)])
            zf = sbt(ph, "zf", [128, 32], F32); bzf = Buf("zf")
            spf = sbt(ph, "spf", [128, 32], F32); bspf = Buf("spf")
            Gmid = sbt(ph, "Gmid", [128, NH], F32); bGmid = Buf("Gmid")
            rl = [sbt(ph, "rl%d" % i, [128, 512], F32) for i in range(1)]
            brl = [Buf("rl0"), Buf("rl0b")]
            rl = [rl[0], rl[0]]
            brl = [brl[0], brl[0]]
            rlring = Ring(list(range(2)))
            bcs = sbt(ph, "bcs", [64, 512], F32); bbcs = Buf("bcs")
            ybT = sbt(ph, "ybT", [128, 4, 512], BF16); bybT = Buf("ybT")
            thr = [sbt(ph, "thr%d" % i, [128, 512], BF16) for i in range(2)]
            thring = Ring([(thr[i], Buf("thr%d" % i)) for i in range(2)])
            t12 = [sbt(ph, "t12_%d" % i, [128, 512], BF16) for i in range(2)]
            t12ring = Ring([(t12[i], Buf("t12_%d" % i)) for i in range(2)])
            mT = sbt(ph, "mT", [128, KC, 512], BF16); bmT = Buf("mT")

            Gring = Ring([(psum[i], pbuf[i]) for i in range(6)])
            Bring = Ring([(psum[i], pbuf[i]) for i in range(2)])
            Spairs = [[(psum[3], pbuf[3]), (psum[4], pbuf[4])], [(psum[5], pbuf[5]), (psum[2], pbuf[2])]]
            Oring = Ring([(psum[i], pbuf[i]) for i in (6, 7)])
            win_v = win_bf.rearrange("(kc p) n -> p kc n", p=128)
            woc_v = woc_bf.rearrange("(kc p) n -> p kc n", p=128)
            woa_v = woa_bf.rearrange("(kc p) n -> p kc n", p=128)

            def v8(t):
                return t[:, :].rearrange("p (k n) -> p k n", k=8)

            def v4(t):
                return t[:, :].rearrange("p (k n) -> p k n", k=4)

            def load_seg(c0, ncol=512):
                si = slots.next()
                O.dma("sp", v8(wslot[si])[:, :, 0:ncol], win_v[:, :, c0:c0 + ncol], [b_winbf], [bslot[si]],
                      "wsl%d" % si)
                return si

            moe_casts = []
            for e in range(NE):
                rows = slice(e * 128, (e + 1) * 128)
                moe_casts.append((W1[rows, 0:2048].rearrange("p (k n) -> p k n", k=KC),
                                  wg_d[e].rearrange("(kc p) n -> p kc n", p=128), e))
                moe_casts.append((W1[rows, 2048:4096].rearrange("p (k n) -> p k n", k=KC),
                                  wu_d[e].rearrange("(kc p) n -> p kc n", p=128), e))
                moe_casts.append((W2[rows, :].rearrange("p (f n) -> p f n", f=2),
                                  wd_d[e].rearrange("(f p) n -> p f n", p=128), e))
            n_total_chunks = NB * NCH
            per_chunk = -(-len(moe_casts) // n_total_chunks)
            cast_i = [0]

            def issue_casts(n):
                for _ in range(n):
                    if cast_i[0] >= len(moe_casts):
                        return
                    dst, src, e = moe_casts[cast_i[0]]
                    O.dma("pool", dst, src, (), [bmoe_w[e]], "mcast%d" % (cast_i[0] % 8))
                    cast_i[0] += 1

            O.memset("pool", Vc[:, :, :, HD:HD + 1], 1.0, [bVones])
            O.memset("pool", onesB[:], 1.0, [bconst])
            O.memset("pool", qT[:], 0.0, [bqT])

            chunks = [(b, c) for b in range(NB) for c in range(NCH)]

            norm_x = {}

            def stage_norm_dma(g, i):
                b, c = chunks[g]
                tok0 = b * S + c * 512
                xt, bxt, chn = xring.next()
                O.dma("sp", xt[:], x_d[tok0 + i * 128: tok0 + (i + 1) * 128, :], (), [bxt], chn)
                norm_x[(g, i)] = (xt, bxt)

            def stage_norm_compute(g, i):
                xt, bxt = norm_x.pop((g, i))
                S_.op("dve", lambda e: e.scalar_tensor_tensor(out=xn[i][:], in0=xt[:], scalar=1.0, in1=xt[:],
                                                               op0=ALU.mult, op1=ALU.mult, accum_out=ss[:, i:i + 1]),
                      [bxt], [bxn[i], bss])
                O.act(lnv[:, i:i + 1], ss[:, i:i + 1], AF.Ln, [bss], [brstd], bias=EPS, scale=1.0 / D)
                O.act(rstd[:, i:i + 1], lnv[:, i:i + 1], AF.Exp, [brstd], [brstd], scale=-0.5)
                O.ts("pool", xn[i][:], xt[:], rstd[:, i:i + 1], None, ALU.mult, None, [bxt, brstd], [bxn[i]])

            def stage_norm(g):
                for i in range(4):
                    stage_norm_dma(g, i)
                    stage_norm_compute(g, i)

            def stage_tr(g):
                b, c = chunks[g]
                hT, bhT = hTs[g % 2]
                for kc in range(KC):
                    tp, btp = Gring.next()
                    tpb = tp[:, :].bitcast(BF16)
                    for i in range(4):
                        O.tr(tpb[:, i * 128:(i + 1) * 128], xn[i][:, kc * 128:(kc + 1) * 128], identB[:],
                             [bxn[i], bconst], [btp])
                    O.ts("dve", hT[:, kc, :], tpb[:, 0:512], A1[:, b, kc:kc + 1], B1[:, b, kc:kc + 1],
                         ALU.mult, ALU.add, [btp, bconst], [bhT])

            def stage_proj(g):
                b, c = chunks[g]
                hT, bhT = hTs[g % 2]
                pf, bpf = Gring.next()
                for i in range(4):
                    for kc in range(KC):
                        O.mm(pf[:, i * NH:(i + 1) * NH], hT[:, kc, i * 128:(i + 1) * 128], wfB[:, kc, :],
                             kc == 0, kc == KC - 1, [bconst, bhT], [bpf])
                O.tt("dve", zf[:, :].rearrange("p (i h) -> p i h", h=NH),
                     pf[:, 0:32].rearrange("p (i h) -> p i h", h=NH),
                     bfg[:, None, :].broadcast_to([128, 4, NH]), ALU.add, [bpf, bconst], [bzf])
                O.act(spf[:], zf[:], AF.Exp, [bzf], [bspf], scale=-1.0)
                O.act(spf[:], spf[:], AF.Ln, [bspf], [bspf], bias=1.0)
                for which in range(2):
                    si = load_seg((3 + which) * 512)
                    w8 = v8(wslot[si])
                    for hp in range(4):
                        pt, bpt = Gring.next()
                        for kc in range(KC):
                            O.mm(pt[:, :], w8[:, kc, hp * 128:(hp + 1) * 128], hT[:, kc, :], kc == 0,
                                 kc == KC - 1, [bslot[si], bhT], [bpt])
                        O.act(sq[:], pt[:, :], AF.Square, [bpt], [bsq])
                        pm, bpm = Gring.next()
                        O.mm(pm[:, :], bonesB[:], sq[:], True, True, [bconst, bsq], [bpm])
                        ri = rsring.next()
                        O.act(rsb[ri][:], pm[:, :], AF.Ln, [bpm], [brsb[ri]], bias=EPS)
                        O.act(rsb[ri][:], rsb[ri][:], AF.Exp, [brsb[ri]], [brsb[ri]], scale=-0.5)
                        if which == 0:
                            O.stt(qT[0:64, 2 * hp, :], pt[0:64, :], qkws[0:64, 0:1], rsb[ri][0:64, :], ALU.mult, ALU.mult,
                                  [bpt, bconst, brsb[ri]], [bqT])
                            O.stt(qT[64:128, 2 * hp + 1, :], pt[64:128, :], qkws[64:128, 0:1], rsb[ri][64:128, :],
                                  ALU.mult, ALU.mult, [bpt, bconst, brsb[ri]], [bqT])
                        else:
                            O.stt(Kc[:, hp, c * 512:(c + 1) * 512], pt[:, :], qkws[:, 1:2], rsb[ri][:],
                                  ALU.mult, ALU.mult, [bpt, bconst, brsb[ri]], [bKc[c]])
                si = load_seg(5 * 512)
                w8 = v8(wslot[si])
                for i in range(4):
                    pt, bpt = Gring.next()
                    for kc in range(KC):
                        O.mm(pt[:, :], hT[:, kc, i * 128:(i + 1) * 128], w8[:, kc, :], kc == 0, kc == KC - 1,
                             [bslot[si], bhT], [bpt])
                    O.copy("dve", Vc[:, c * 4 + i, :, 0:HD], pt[:, :].rearrange("p (h d) -> p h d", h=NH),
                           [bpt], [bVc[c]])

                for i in range(4):
                    blk = c * 4 + i
                    pg, bpg = Gring.next()
                    if blk == 0:
                        O.mm(pg[:, 0:NH], triF, spf[:, i * NH:(i + 1) * NH], True, True, [bconst, bspf], [bpg])
                    else:
                        O.mm(pg[:, 0:NH], triF, spf[:, i * NH:(i + 1) * NH], True, False, [bconst, bspf], [bpg])
                        O.mm(pg[:, 0:NH], elastF, Gall[:, blk - 1, :], False, True, [bconst, bGall[blk - 1]], [bpg])
                    O.copy("dve", Gall[:, blk, :], pg[:, 0:NH], [bpg], [bGall[blk]])
                pg, bpg = Gring.next()
                O.mm(pg[:, 0:NH], elastF, Gall[:, c * 4 + 1, :], True, True, [bconst, bGall[c * 4 + 1]], [bpg])
                O.copy("dve", Gmid[:], pg[:, 0:NH], [bpg], [bGmid])
                nkb = 4 * c + 4
                O.tt("dve", nbias[:, 0:nkb, :], Gall[:, 0:nkb, :], Gmid[:, None, :].broadcast_to([128, nkb, NH]),
                     ALU.subtract, [bGmid] + bGall[0:nkb], [bnbias])

            def conv_units(g, ring):
                b, c = chunks[g]
                hT, bhT = hTs[g % 2]
                units = []
                segslot = {}

                def seg_unit(seg, j, dst, bdst):
                    def fn():
                        if j == 0:
                            segslot[seg] = load_seg(seg * 512)
                        si = segslot[seg]
                        w8 = v8(wslot[si])
                        pt, bpt = ring.next()
                        for kc in range(KC):
                            O.mm(pt[:, :], w8[:, kc, j * 128:(j + 1) * 128], hT[:, kc, :], kc == 0, kc == KC - 1,
                                 [bslot[si], bhT], [bpt])
                            if kc == 3:
                                yield
                        O.copy("dve", dst[:, j, :], pt[:, :], [bpt], [bdst])
                        yield
                    return fn

                def conv_unit(j):
                    def fn():
                        if j == 0:
                            segslot[2] = load_seg(2 * 512)
                        si = segslot[2]
                        w8 = v8(wslot[si])
                        pt, bpt = ring.next()
                        for kc in range(KC):
                            O.mm(pt[:, :], w8[:, kc, j * 128:(j + 1) * 128], hT[:, kc, :], kc == 0, kc == KC - 1,
                                 [bslot[si], bhT], [bpt])
                            if kc == 3:
                                yield
                        if c == 0:
                            O.memset("dve", ub[j][:, 0:2], 0.0, [bub[j]])
                        else:
                            O.copy("dve", ub[j][:, 0:2], ub[j][:, 512:514], [bub[j]], [bub[j]])
                        O.tt("dve", ub[j][:, 2:514], pt[:, :], xin_sb[:, j, :], ALU.mult, [bpt, bxin], [bub[j]])
                        yield
                        pc, bpc = ring.next()
                        for tap in range(3):
                            O.mm(pc[:, :], diagW[:, j * 3 + tap, :], ub[j][:, tap:tap + 512], tap == 0, tap == 2,
                                 [bconst, bub[j]], [bpc])
                        O.tt("dve", yaT[:, j, :], pc[:, :], b_sb[:, j, :], ALU.mult, [bpc, bbsb], [byaT])
                        yield
                    return fn
                for j in range(4):
                    units.append(seg_unit(0, j, xin_sb, bxin))
                for j in range(4):
                    units.append(seg_unit(1, j, b_sb, bbsb))
                for j in range(4):
                    units.append(conv_unit(j))

                def steps():
                    for u in units:
                        for _ in u():
                            yield
                return steps()

            def stage_attn(g):
                b, c = chunks[g]
                nkb = 4 * c + 4

                def finalize(h, Ob, bOb):
                    hp, jj = divmod(h, 2)
                    lo = jj * 64
                    ri = rlring.next()
                    O.act(rl[ri][64:65, :], Ob[64:65, :], AF.Ln, [bOb], [brl[ri]])
                    O.act(rlb[ri][64:65, :], rl[ri][64:65, :], AF.Exp, [brl[ri]], [brlb[ri]], scale=-1.0)
                    pbc, bpbc = Bring.next()
                    O.mm(pbc[0:64, :], onesB[64:65, 0:64], rlb[ri][64:65, :], True, True, [bconst, brlb[ri]], [bpbc])
                    O.copy("dve", bcs[:, :], pbc[0:64, :], [bpbc], [bbcs])
                    O.tt("dve", ybT[lo:lo + 64, hp, :], Ob[0:64, :], bcs[:, :], ALU.mult, [bOb, bbcs], [bybT])

                prev_fin = None
                npairs = nkb // 2
                cunits = conv_units(g, Bring)
                items = [(h, p) for h in range(NH) for p in range(npairs)]
                spp = -(-28 // len(items))
                Obs = {}

                def qkpair(idx):
                    h, p = items[idx]
                    hp = h // 2
                    res = []
                    for t in range(2):
                        kb = 2 * p + t
                        d = kb - 4 * c
                        q0 = max(d, 0) * 128
                        Sb, bSb = Spairs[idx % 2][t]
                        O.mm(Sb[:, 0:512 - q0], Kc[:, hp, kb * 128:(kb + 1) * 128],
                             qT[:, h, q0:512], True, True, [bKc[kb // 4], bqT], [bSb])
                        res.append((Sb, bSb, q0, d, kb))
                    return res

                pend = [qkpair(i) for i in range(min(2, len(items)))]
                for idx, (h, p) in enumerate(items):
                    if p == 0:
                        Obs[h] = Oring.next()
                    Ob, bOb = Obs[h]
                    cur = pend.pop(0)
                    pts = []
                    for (Sb, bSb, q0, d, kb) in cur:
                        N = 512 - q0
                        Pt, bPt = PTring.next()
                        O.act(Pt[:, 0:N], Sb[:, 0:N], AF.Exp, [bSb, bnbias], [bPt], bias=nbias[:, kb, h:h + 1])
                        if d >= 0:
                            O.tt("pool", Pt[:, 0:128], Pt[:, 0:128], triB[:], ALU.mult, [bPt, bconst], [bPt])
                        pts.append((Pt, bPt, q0, N, kb))
                    allp = [x[1] for x in pts]
                    for (Pt, bPt, q0, N, kb) in pts:
                        O.mm(Ob[0:HD + 1, q0:512], Vc[:, kb, h, :], Pt[:, 0:N], kb == 0, kb == nkb - 1,
                             [bVc[kb // 4], bVones] + allp, [bOb])
                    if idx + 2 < len(items):
                        pend.append(qkpair(idx + 2))
                    if p == 0 and prev_fin is not None:
                        finalize(*prev_fin)
                        prev_fin = None
                    for _ in range(spp):
                        next(cunits, None)
                    if p == npairs - 1:
                        prev_fin = (h, Ob, bOb)
                        if g + 1 < len(chunks):
                            if 1 <= h <= 4:
                                stage_norm_compute(g + 1, h - 1)
                            if h <= 3:
                                stage_norm_dma(g + 1, h)
                        if h >= 1 and h <= 6:
                            issue_casts(1)
                finalize(*prev_fin)
                for _ in cunits:
                    pass

            def stage_post(g):
                b, c = chunks[g]
                tok0 = b * S + c * 512
                hT, bhT = hTs[g % 2]
                s_oc = slots.next()
                O.dma("sp", v4(wslot[s_oc]), woc_v, [b_winbf], [bslot[s_oc]], "wsl%d" % s_oc)
                s_oa = slots.next()
                O.dma("sp", v4(wslot[s_oa]), woa_v, [b_winbf], [bslot[s_oa]], "wsl%d" % s_oa)
                woc4, woa4 = v4(wslot[s_oc]), v4(wslot[s_oa])
                for jp in range(4):
                    sg = slots.next()
                    while sg in (s_oc, s_oa):
                        sg = slots.next()
                    g8 = v8(wslot[sg])
                    O.dman("sp", [(g8[:, :, 0:256], win_v[:, :, 3080 + jp * 256: 3080 + (jp + 1) * 256]),
                                  (g8[:, :, 256:512], win_v[:, :, 4104 + jp * 256: 4104 + (jp + 1) * 256])],
                           [b_winbf], [bslot[sg]], "wsl%d" % sg)
                    for j2 in range(2):
                        jf = jp * 2 + j2
                        pgc, bpgc = Gring.next()
                        for kc in range(KC):
                            O.mm(pgc[:, :], g8[:, kc, j2 * 128:(j2 + 1) * 128], hT[:, kc, :], kc == 0,
                                 kc == KC - 1, [bslot[sg], bhT], [bpgc])
                        thc, bthc = thring.next()
                        O.act(thc[:], pgc[:, :], AF.Tanh, [bpgc], [bthc], scale=0.5)
                        pa, bpa = Gring.next()
                        for kc in range(4):
                            O.mm(pa[:, :], woc4[:, kc, jf * 128:(jf + 1) * 128], yaT[:, kc, :], kc == 0, kc == 3,
                                 [bslot[s_oc], byaT], [bpa])
                        pga, bpga = Gring.next()
                        for kc in range(KC):
                            O.mm(pga[:, :], g8[:, kc, 256 + j2 * 128: 256 + (j2 + 1) * 128], hT[:, kc, :], kc == 0,
                                 kc == KC - 1, [bslot[sg], bhT], [bpga])
                        tha, btha = thring.next()
                        O.act(tha[:], pga[:, :], AF.Tanh, [bpga], [btha], scale=0.5)
                        t1, bt1 = t12ring.next()
                        O.stt(t1[:], thc[:], 1.0, pa[:, :], ALU.add, ALU.mult, [bthc, bpa], [bt1])
                        pb_, bpb_ = Gring.next()
                        for kc in range(4):
                            O.mm(pb_[:, :], woa4[:, kc, jf * 128:(jf + 1) * 128], ybT[:, kc, :], kc == 0, kc == 3,
                                 [bslot[s_oa], bybT], [bpb_])
                        t2, bt2 = t12ring.next()
                        O.stt(t2[:], tha[:], 1.0, pb_[:, :], ALU.add, ALU.mult, [btha, bpb_], [bt2])
                        O.tt("pool", mT[:, jf, :], t1[:], t2[:], ALU.add, [bt1, bt2], [bmT])
                if b == 0 and c == 0:
                    dtap("hT", hT[:], [bhT], [128, KC, 512], BF16)
                    dtap("yaT", yaT[:], [byaT], [128, 4, 512], BF16)
                    dtap("qT", qT[:], [bqT], [128, NH, 512], BF16)
                    dtap("Kc", Kc[:, :, 0:512], [bKc[0]], [128, 4, 512], BF16)
                    dtap("Vc", Vc[:, 0:4, :, :], [bVc[0], bVones], [128, 4, NH, HD + 1], BF16)
                    dtap("Gall", Gall[:, 0:4, :], bGall[0:4], [128, 4, NH], F32)
                    dtap("nbias", nbias[:, 0:4, :], [bnbias], [128, 4, NH], F32)
                    dtap("ybT", ybT[:], [bybT], [128, 4, 512], BF16)
                    dtap("mT", mT[:], [bmT], [128, KC, 512], BF16)
                s_o = []
                for n in range(2):
                    so = slots.next()
                    O.dma("sp", v8(wslot[so]), wos_bf[b].rearrange("(kc p) n -> p kc n", p=128)[:, :, n * 512:(n + 1) * 512],
                          [b_wos], [bslot[so]], "wsl%d" % so)
                    s_o.append(so)
                for i in range(4):
                    xt, bxt, chn = xring.next()
                    O.dma("sp", xt[:], x_d[tok0 + i * 128: tok0 + (i + 1) * 128, :], (), [bxt], chn)
                    x1t, bx1t, x1ch = x1ring.next()
                    for n in range(2):
                        po, bpo = Gring.next()
                        w8 = v8(wslot[s_o[n]])
                        for kc in range(KC):
                            O.mm(po[:, :], mT[:, kc, i * 128:(i + 1) * 128], w8[:, kc, :], kc == 0, kc == KC - 1,
                                 [bslot[s_o[n]], bmT], [bpo])
                        O.tt("dve", x1t[:, n * 512:(n + 1) * 512], po[:, :], xt[:, n * 512:(n + 1) * 512], ALU.add,
                             [bpo, bxt], [bx1t])
                    ti = (tok0 // 128) + i
                    O.dma("pool", out_d[ti * 128:(ti + 1) * 128, :], x1t[:], [bx1t], [b_outd[ti]], x1ch)

            NG = len(chunks)
            nzf = (NST_ * 512) // 1024
            zf_i = [0]
            bXs0 = Buf("Xs_zero")

            def issue_zero(n):
                for _ in range(n):
                    if zf_i[0] >= nzf:
                        return
                    r0 = zf_i[0] * 1024
                    O.dma("act", Xs[r0:r0 + 1024, :], zeros_d[:, :], (), [bXs0], "zf%d" % (zf_i[0] % 4))
                    zf_i[0] += 1
            zper = -(-nzf // NG)
            stage_norm(0)
            stage_tr(0)
            for g in range(NG):
                issue_zero(zper)
                stage_proj(g)
                stage_attn(g)
                if g + 1 < NG:
                    stage_tr(g + 1)
                stage_post(g)
            issue_casts(len(moe_casts))
            issue_zero(nzf)
            S_.finish()

        if not run_b:
            return nc
        NTT = NTOK // 128
        NST = (2 * NTOK) // 512 + NE
        NSLOTS = NST * 512
        Ybuf = nc.dram_tensor("Ybuf", [NSLOTS, D], BF16).ap()
        I32 = mybir.dt.int32
        sloti = sbt(top, "sloti", [128, NTT * 2], I32)
        wk = sbt(top, "wk", [128, NTT * 2], F32)
        widx = sbt(top, "widx", [128, NST], I32)
        g2bc = sbt(top, "g2bc_t", [128, NB, D], F32)

        def bcast_rows(O, dst, src_fn, ring, bufs_in, bdst, diag, bdiag):
            k = 0
            for b in range(NB):
                for half in range(2):
                    pb_, bb_ = ring.next()
                    for q in range(4):
                        kc = half * 4 + q
                        dg, bdg = diag[k % 2], bdiag[k % 2]
                        k += 1
                        O.ts("dve", dg[:], identF, src_fn(b, kc), None, ALU.mult, None, bufs_in, [bdg])
                        O.mm(pb_[:, q * 128:(q + 1) * 128], onesF, dg[:], True, True, [bconst, bdg], [bb_])
                    O.copy("dve", dst[:, b, half * 512:(half + 1) * 512], pb_[:, :], [bb_], [bdst])

        with contextlib.ExitStack() as ph:
            S_ = Sched(nc, "B1")
            tap_state["S"] = S_
            O = Ops(S_)
            H2all = sbt(ph, "H2all", [128, NTT, D], BF16)
            bH2 = [Buf("H2_%d" % i) for i in range(NTT)]
            OH1 = sbt(ph, "OH1", [128, NTT, NE], F32)
            OH2 = sbt(ph, "OH2", [128, NTT, NE], F32)
            bOH = [Buf("OH%d" % i) for i in range(NTT)]
            A2row = sbt(ph, "A2row", [128, NB, D], F32)
            B2row = sbt(ph, "B2row", [128, NB, D], F32)
            bA2r = Buf("A2row")
            bg2 = Buf("g2bc")
            diag = [sbt(ph, "bdiag%d" % i, [128, 128], F32) for i in range(2)]
            bdiag = [Buf("bdiag%d" % i) for i in range(2)]
            xr = [sbt(ph, "bxr%d" % i, [128, D], F32) for i in range(2)]
            xring = Ring([(xr[i], Buf("bxr%d" % i), "bxr%d" % i) for i in range(2)])
            tmpf = [sbt(ph, "tmpf%d" % i, [128, D], F32) for i in range(1)]
            tmpring = Ring([(tmpf[i], Buf("tmpf%d" % i)) for i in range(1)])
            Ar = [sbt(ph, "Ar%d" % i, [128, NE], F32) for i in range(2)]
            Aring = Ring([(Ar[i], Buf("Ar%d" % i)) for i in range(2)])
            ss = sbt(ph, "bss", [128, 4], F32); bss = Buf("bss")
            lnv = sbt(ph, "blnv", [128, 4], F32)
            rstd = sbt(ph, "brstd", [128, 4], F32); brstd = Buf("brstd")
            h2T = sbt(ph, "h2T", [128, KC, 512], BF16); bh2T = Buf("h2T")
            rb = {n: sbt(ph, "rb_" + n, [128, w], F32) for n, w in
                  (("lg", 144), ("gmax", 4), ("ohg", 16), ("sh", 16), ("ex", 16), ("sume", 4), ("psel", 4), ("pen", 16),
                   ("lem", 128), ("m8", 32), ("d21", 4), ("e2", 4), ("den", 4), ("rden", 4))}
            brt_ = Buf("rt")
            cntf = sbt(ph, "cntf", [128, NE], F32)
            padi = sbt(ph, "padi", [128, NE], I32)
            padf = sbt(ph, "padf", [128, NE], F32)
            base = sbt(ph, "base", [128, NE], F32)
            jv = sbt(ph, "jv_sb", [128, NST + 1], F32)
            texpf = sbt(ph, "texpf", [128, NST], F32)
            Rsum = sbt(ph, "Rsum", [128, NE], F32)
            Tt = sbt(ph, "Tt", [128, NE], F32)
            Tm = sbt(ph, "Tm", [128, NE], F32)
            slotf = sbt(ph, "slotf", [128, NTT * 2], F32)
            bcnt = Buf("cnt"); bRsum = Buf("Rsum"); bT = Buf("T"); bslot = Buf("slot")
            Dring = Ring([(psum[i], pbuf[i]) for i in range(8)])
            striF = constsF[:, 5, :]
            striB = sbt(ph, "striB", [128, 128], BF16)
            onesBb = sbt(ph, "onesBb", [128, 128], BF16)
            bstri = Buf("striB")

            bcast_rows(O, g2bc, lambda b, kc: modT[:, 40 + kc, b:b + 1], Dring, [bconst], bg2, diag, bdiag)
            bcast_rows(O, A2row, lambda b, kc: A2[:, b, kc:kc + 1], Dring, [bconst], bA2r, diag, bdiag)
            bcast_rows(O, B2row, lambda b, kc: B2[:, b, kc:kc + 1], Dring, [bconst], bA2r, diag, bdiag)
            O.dma("sp", jv[:], jv_d[:, :], (), [bcnt], "jv")

            TRring = Ring([(psum[i], pbuf[i]) for i in range(6)])
            PRring = Ring([(psum[i], pbuf[i]) for i in (6, 7)])

            def b1_norm(scn):
                    b = (scn * 512) // S
                    for i in range(4):
                        ti = scn * 4 + i
                        xt, bxt, chn = xring.next()
                        O.dma("sp", xt[:], out_d[ti * 128:(ti + 1) * 128, :], [b_outd[ti]], [bxt], chn)
                        O.act(H2all[:, ti, :], xt[:], AF.Square, [bxt], [bH2[ti], bss], accum=ss[:, i:i + 1])
                        O.act(lnv[:, i:i + 1], ss[:, i:i + 1], AF.Ln, [bss], [brstd], bias=EPS, scale=1.0 / D)
                        O.act(rstd[:, i:i + 1], lnv[:, i:i + 1], AF.Exp, [brstd], [brstd], scale=-0.5)
                        tf, btf = tmpring.next()
                        O.stt(tf[:], xt[:], rstd[:, i:i + 1], A2row[:, b, :], ALU.mult, ALU.mult, [bxt, brstd, bA2r], [btf])
                        O.tt("pool", H2all[:, ti, :], tf[:], B2row[:, b, :], ALU.add, [btf, bA2r], [bH2[ti]])

            def b1_tr(scn):
                    for kc in range(KC):
                        tp, btp = TRring.next()
                        tpb = tp[:, :].bitcast(BF16)
                        for i in range(4):
                            ti = scn * 4 + i
                            O.tr(tpb[:, i * 128:(i + 1) * 128], H2all[:, ti, kc * 128:(kc + 1) * 128], identB[:],
                                 [bH2[ti], bconst], [btp])
                        O.act(h2T[:, kc, :], tpb[:, 0:512], AF.Copy, [btp], [bh2T])

            def b1_router_mm(scn):
                    pr, bpr = PRring.next()
                    for i in range(4):
                        for kc in range(KC):
                            O.mm(pr[:, i * 36:(i + 1) * 36], h2T[:, kc, i * 128:(i + 1) * 128], wrB[:, kc, :], kc == 0,
                                 kc == KC - 1, [bh2T, bconst], [bpr])
                    return pr, bpr

            def b1_router_math(scn, pr, bpr):
                    R = [brt_]
                    BIG = 1.0e30
                    t0 = scn * 4
                    lg3 = rb["lg"][:, :].rearrange("p (t c) -> p t c", t=4)
                    O.tt("dve", lg3, pr[:, 0:144].rearrange("p (t c) -> p t c", t=4),
                         brt[:, None, :].broadcast_to([128, 4, 36]), ALU.add, [bpr, bconst], R)
                    S_.op("dve", lambda e, lg3=lg3: e.tensor_reduce(out=rb["gmax"][:], in_=lg3[:, :, 0:4], axis=AX.X,
                                                                   op=ALU.max), R, R)
                    gm_b = rb["gmax"][:, :, None].broadcast_to([128, 4, 4])
                    O.tt("dve", rb["ohg"][:, :].rearrange("p (t g) -> p t g", t=4), lg3[:, :, 0:4], gm_b, ALU.is_equal, R, R)
                    O.tt("dve", rb["sh"][:, :].rearrange("p (t g) -> p t g", t=4), lg3[:, :, 0:4], gm_b, ALU.subtract, R, R)
                    O.act(rb["ex"][:], rb["sh"][:], AF.Exp, R, R)
                    S_.op("dve", lambda e: e.tensor_reduce(out=rb["sume"][:], in_=rb["ex"][:, :].rearrange("p (t g) -> p t g", t=4),
                                                          axis=AX.X, op=ALU.add), R, R)
                    S_.op("dve", lambda e: e.reciprocal(out=rb["psel"][:], in_=rb["sume"][:]), R, R)
                    O.ts("dve", rb["pen"][:], rb["ohg"][:], BIG, -BIG, ALU.mult, ALU.add, R, R)
                    O.tt("dve", rb["lem"][:, :].rearrange("p (t g e) -> p t g e", t=4, g=4),
                         lg3[:, :, 4:36].rearrange("p t (g e) -> p t g e", g=4),
                         rb["pen"][:, :].rearrange("p (t g) -> p t g", t=4)[:, :, :, None].broadcast_to([128, 4, 4, 8]),
                         ALU.add, R, R)
                    for i in range(4):
                        S_.op("dve", lambda e, i=i: e.max(out=rb["m8"][:, i * 8:(i + 1) * 8], in_=rb["lem"][:, i * 32:(i + 1) * 32]), R, R)
                    m83 = rb["m8"][:, :].rearrange("p (t k) -> p t k", t=4)
                    lem3 = rb["lem"][:, :].rearrange("p (t e) -> p t e", t=4)
                    O.tt("dve", OH1[:, t0:t0 + 4, :], lem3, m83[:, :, 0:1].broadcast_to([128, 4, NE]), ALU.is_equal,
                         R, bOH[t0:t0 + 4])
                    O.tt("dve", OH2[:, t0:t0 + 4, :], lem3, m83[:, :, 1:2].broadcast_to([128, 4, NE]), ALU.is_equal,
                         R, bOH[t0:t0 + 4])
                    O.tt("dve", rb["d21"][:, :, None], m83[:, :, 1:2], m83[:, :, 0:1], ALU.subtract, R, R)
                    O.act(rb["e2"][:], rb["d21"][:], AF.Exp, R, R)
                    O.ts("dve", rb["den"][:], rb["e2"][:], 1.0, None, ALU.add, None, R, R)
                    S_.op("dve", lambda e: e.reciprocal(out=rb["rden"][:], in_=rb["den"][:]), R, R)
                    wk3 = wk[:, t0 * 2:(t0 + 4) * 2].rearrange("p (t k) -> p t k", k=2)
                    O.tt("dve", wk3[:, :, 0:1], rb["rden"][:, :, None], rb["psel"][:, :, None], ALU.mult, R, [bslot])
                    O.tt("dve", wk3[:, :, 1:2], wk3[:, :, 0:1], rb["e2"][:, :, None], ALU.mult, R + [bslot], [bslot])


            NSCN = NTOK // 512
            prs = {}
            for scn in range(NSCN):
                b1_norm(scn)
                if scn > 0:
                    b1_router_math(scn - 1, *prs.pop(scn - 1))
                b1_tr(scn)
                prs[scn] = b1_router_mm(scn)
            b1_router_math(NSCN - 1, *prs.pop(NSCN - 1))

            Ab = h2T[:, :, :].rearrange("p k n -> p (k n)")[:, 0:NTT * NE].rearrange("p (t e) -> p t e", e=NE)
            O.tt("dve", Ab, OH1[:, :, :], OH2[:, :, :], ALU.add, bOH, [bh2T])
            O.copy("dve", striB[:], striF, [bconst], [bstri])
            O.memset("dve", onesBb[:], 1.0, [bstri])
            pc_, bpc_ = Dring.next()
            for ti in range(NTT):
                O.mm(pc_[:, 0:NE], onesBb[:], Ab[:, ti, :], ti == 0, ti == NTT - 1, [bstri, bh2T], [bpc_])
            O.ts("dve", cntf[:], pc_[:, 0:NE], 511.0, None, ALU.add, None, [bpc_], [bcnt])
            O.copy("dve", padi[:], cntf[:], [bcnt], [bcnt])
            O.ts("dve", padi[:], padi[:], 9, 9, ALU.arith_shift_right, ALU.logical_shift_left, [bcnt], [bcnt])
            O.copy("dve", padf[:], padi[:], [bcnt], [bcnt])
            O.memset("dve", base[:, 0:1], 0.0, [bcnt])
            for e in range(1, NE):
                O.tt("dve", base[:, e:e + 1], base[:, e - 1:e], padf[:, e - 1:e], ALU.add, [bcnt], [bcnt])
            O.memset("dve", texpf[:], -1.0, [bcnt])
            for e in range(NE):
                O.stt(texpf[:], jv[:, 0:NST], base[:, e:e + 1], texpf[:], ALU.is_ge, ALU.add, [bcnt], [bcnt])
            O.ts("dve", texpf[:], texpf[:], 128.0, jv[:, NST:NST + 1], ALU.mult, ALU.add, [bcnt], [bcnt])
            O.copy("dve", widx[:], texpf[:], [bcnt], [bconst])
            TPB = 16 if NTT >= 16 else NTT
            NBK = NTT // TPB
            Abk = rb["lem"][:, :].bitcast(BF16)[:, 0:NBK * NE].rearrange("p (b e) -> p b e", e=NE)
            for bk in range(NBK):
                def red(e, bk=bk):
                    with nc.allow_low_precision(reason="exact small integer counts"):
                        return e.tensor_reduce(
                            out=Abk[:, bk, :], in_=Ab[:, bk * TPB:(bk + 1) * TPB, :].rearrange("p t e -> p e t"),
                            axis=AX.X, op=ALU.add)
                S_.op("dve", red, [bh2T], [brt_])
            tfT, btfT = tmpring.next()
            for bk in range(NBK):
                pk, bpk = Dring.next()
                for tl in range(TPB):
                    ti = bk * TPB + tl
                    reg = pk[:, tl * NE:(tl + 1) * NE]
                    last = (tl == 0 and bk == 0)
                    O.mm(reg, striB[:], Ab[:, ti, :], True, last, [bstri, bh2T], [bpk])
                    srcs = [Ab[:, bk * TPB + tj, :] for tj in range(tl)] + [Abk[:, bj, :] for bj in range(bk)]
                    for n_, src in enumerate(srcs):
                        O.mm(reg, onesBb[:], src, False, n_ == len(srcs) - 1, [bstri, bh2T, brt_], [bpk])
                W = TPB * NE
                Tb = tfT[:, 0:W].rearrange("p (t e) -> p t e", e=NE)
                Tm = tfT[:, 512:512 + W].rearrange("p (t e) -> p t e", e=NE)
                O.tt("dve", Tb, pk[:, 0:W].rearrange("p (t e) -> p t e", e=NE),
                     base[:, None, :].broadcast_to([128, TPB, NE]), ALU.add, [bpk, bcnt], [btfT])
                for k2, OHk in ((0, OH1), (1, OH2)):
                    O.tt("dve", Tm, Tb, OHk[:, bk * TPB:(bk + 1) * TPB, :], ALU.mult, [btfT] + bOH[bk * TPB:(bk + 1) * TPB], [btfT])
                    S_.op("dve", lambda e, bk=bk, k2=k2, Tm=Tm: e.tensor_reduce(
                        out=slotf[:, bk * TPB * 2:(bk + 1) * TPB * 2].rearrange("p (t k) -> p t k", k=2)[:, :, k2],
                        in_=Tm, axis=AX.X, op=ALU.add), [btfT], [bslot])
            O.copy("dve", sloti[:], slotf[:], [bslot], [bslot])
            dtap("slotf", slotf[:], [bslot], [128, NTT * 2], F32)
            dtap("cntf", cntf[:], [bcnt], [128, NE], F32)
            dtap("base", base[:], [bcnt], [128, NE], F32)
            dtap("wk", wk[:], [bslot], [128, NTT * 2], F32)
            for ti in range(NTT):
                for k2 in range(2):
                    col = ti * 2 + k2
                    S_.op("pool", lambda e, ti=ti, col=col: e.indirect_dma_start(
                        out=Xs[:, :], out_offset=bass.IndirectOffsetOnAxis(ap=sloti[:, col:col + 1], axis=0),
                        in_=H2all[:, ti, :], in_offset=None), [bslot, bH2[ti]], (), chan="sc%d" % (col % 8))
            S_.finish()

        with contextlib.ExitStack() as ph:
            S_ = Sched(nc, "B2")
            tap_state["S"] = S_
            O = Ops(S_)
            NWS = 3
            wsl = [sbt(ph, "ews%d" % i, [128, 3, 2048], BF16) for i in range(NWS)]
            bwsl = [Buf("ews%d" % i) for i in range(NWS)]
            wring = Ring(list(range(NWS)))
            xs = [sbt(ph, "xs%d" % i, [128, D], BF16) for i in range(8)]
            xsring = Ring([(xs[i], Buf("xs%d" % i), "xs%d" % i) for i in range(8)])
            hTs2 = [(sbt(ph, "ehT%d" % i, [128, KC, 512], BF16), Buf("ehT%d" % i)) for i in range(2)]
            aT = [sbt(ph, "aT%d" % i, [128, 2, 512], BF16) for i in range(2)]
            baT = [Buf("aT%d" % i) for i in range(2)]
            sgt = [sbt(ph, "sgt%d" % i, [128, 512], BF16) for i in range(2)]
            sgring = Ring([(sgt[i], Buf("sgt%d" % i)) for i in range(2)])
            yt = [sbt(ph, "yt%d" % i, [128, D], BF16) for i in range(3)]
            yring = Ring([(yt[i], Buf("yt%d" % i), "yst%d" % i) for i in range(3)])
            GUring = Ring([(psum[i], pbuf[i]) for i in range(4)])
            Dring = Ring([(psum[i], pbuf[i]) for i in range(4, 8)])

            def load_weights(j):
                wi = wring.next()
                S_.op("pool", lambda e: [
                    e.indirect_dma_start(out=wsl[wi][:, 0:2, :].rearrange("p a n -> p (a n)"), out_offset=None,
                                         in_=W1[:, :],
                                         in_offset=bass.IndirectOffsetOnAxis(ap=widx[:, j:j + 1], axis=0)),
                    e.indirect_dma_start(out=wsl[wi][:, 2, :], out_offset=None, in_=W2[:, :],
                                         in_offset=bass.IndirectOffsetOnAxis(ap=widx[:, j:j + 1], axis=0))],
                    [bconst], [bwsl[wi]], chan="ews%d" % wi, ndma=2)
                return wi

            def do_down(args):
                j, wi, ai = args
                wdv = wsl[wi][:, 2, :].rearrange("p (f n) -> p f n", f=2)
                for i in range(4):
                    y_, by_, ych = yring.next()
                    for n in range(2):
                        pd, bpd = Dring.next()
                        for f in range(2):
                            O.mm(pd[:, :], aT[ai][:, f, i * 128:(i + 1) * 128], wdv[:, f, n * 512:(n + 1) * 512],
                                 f == 0, f == 1, [baT[ai], bwsl[wi]], [bpd])
                        if n == 0:
                            O.act(y_[:, 0:512], pd[:, :], AF.Copy, [bpd], [by_])
                        else:
                            O.copy("dve", y_[:, 512:1024], pd[:, :], [bpd], [by_])
                    r0 = j * 512 + i * 128
                    O.dma("act", Ybuf[r0:r0 + 128, :], y_[:], [by_], (), ych)

            def load_tr(j):
                hT2, bhT2 = hTs2[j % 2]
                xts = []
                for i in range(4):
                    x_, bx_, xch = xsring.next()
                    r0 = j * 512 + i * 128
                    O.dma("sp", x_[:], Xs[r0:r0 + 128, :], (), [bx_], xch)
                    xts.append((x_, bx_))
                for kc in range(KC):
                    tp, btp = Dring.next()
                    for i in range(4):
                        O.mm(tp[:, i * 128:(i + 1) * 128], xts[i][0][:, kc * 128:(kc + 1) * 128], identB[:], True, True,
                             [xts[i][1], bconst], [btp])
                    if kc % 2 == 0:
                        O.copy("dve", hT2[:, kc, :], tp[:, :], [btp], [bhT2])
                    else:
                        O.act(hT2[:, kc, :], tp[:, :], AF.Copy, [btp], [bhT2])

            pend_down = None
            wis = {0: load_weights(0)}
            load_tr(0)
            for j in range(NST):
                wi = wis.pop(j)
                hT2, bhT2 = hTs2[j % 2]
                if j + 1 < NST:
                    wis[j + 1] = load_weights(j + 1)
                    load_tr(j + 1)
                wgv = wsl[wi][:, 0, :].rearrange("p (k n) -> p k n", k=KC)
                wuv = wsl[wi][:, 1, :].rearrange("p (k n) -> p k n", k=KC)
                ai = j % 2
                for f in range(2):
                    pg, bpg = GUring.next()
                    pu, bpu = GUring.next()
                    for kc in range(KC):
                        O.mm(pg[:, :], wgv[:, kc, f * 128:(f + 1) * 128], hT2[:, kc, :], kc == 0, kc == KC - 1,
                             [bwsl[wi], bhT2], [bpg])
                    for kc in range(KC):
                        O.mm(pu[:, :], wuv[:, kc, f * 128:(f + 1) * 128], hT2[:, kc, :], kc == 0, kc == KC - 1,
                             [bwsl[wi], bhT2], [bpu])
                    sg_, bsg_ = sgring.next()
                    O.act(sg_[:], pg[:, :], AF.Silu, [bpg], [bsg_])
                    O.tt("dve", aT[ai][:, f, :], sg_[:], pu[:, :], ALU.mult, [bsg_, bpu], [baT[ai]])
                if pend_down is not None:
                    do_down(pend_down)
                pend_down = (j, wi, ai)
            do_down(pend_down)

            S_.fence("pool", "yst")
            y1r = [sbt(ph, "y1r%d" % i, [128, D], BF16) for i in range(2)]
            y2r = [sbt(ph, "y2r%d" % i, [128, D], BF16) for i in range(2)]
            ygring = Ring([(y1r[i], y2r[i], Buf("yg%d" % i), "yg%d" % i) for i in range(2)])
            xr2 = [sbt(ph, "fxr%d" % i, [128, D], F32) for i in range(2)]
            xring2 = Ring([(xr2[i], Buf("fxr%d" % i), "fxr%d" % i) for i in range(2)])
            tf2 = [sbt(ph, "tf2_%d" % i, [128, D], F32) for i in range(3)]
            tring2 = Ring([(tf2[i], Buf("tf2_%d" % i)) for i in range(3)])
            otile = [sbt(ph, "otile%d" % i, [128, D], F32) for i in range(2)]
            oring = Ring([(otile[i], Buf("otile%d" % i), "ost%d" % i) for i in range(2)])
            for ti in range(NTT):
                b = (ti * 128) // S
                y1, y2, byg, gch = ygring.next()
                S_.op("pool", lambda e, ti=ti, y1=y1, y2=y2: [
                    e.indirect_dma_start(out=y1[:, :], out_offset=None, in_=Ybuf[:, :],
                                         in_offset=bass.IndirectOffsetOnAxis(ap=sloti[:, ti * 2:ti * 2 + 1], axis=0)),
                    e.indirect_dma_start(out=y2[:, :], out_offset=None, in_=Ybuf[:, :],
                                         in_offset=bass.IndirectOffsetOnAxis(ap=sloti[:, ti * 2 + 1:ti * 2 + 2], axis=0))],
                    [bconst], [byg], chan=gch, ndma=2)
                xt, bxt, chn = xring2.next()
                O.dma("sp", xt[:], out_d[ti * 128:(ti + 1) * 128, :], [b_outd[ti]], [bxt], chn)
                tf, btf = tring2.next()
                O.act(tf[:], y1[:], AF.Copy, [byg, bconst], [btf], scale=wk[:, ti * 2:ti * 2 + 1])
                O.stt(tf[:], y2[:], wk[:, ti * 2 + 1:ti * 2 + 2], tf[:], ALU.mult, ALU.add, [byg, bconst, btf], [btf])
                O.tt("dve", tf[:], tf[:], g2bc[:, b, :], ALU.mult, [btf, bconst], [btf])
                ot, bot, och = oring.next()
                O.tt("dve", ot[:], tf[:], xt[:], ALU.add, [btf, bxt], [bot])
                O.dma("act", out_d[ti * 128:(ti + 1) * 128, :], ot[:], [bot], [b_outd[ti]], och)
            S_.finish()
    return nc


def _consts():
    c = np.zeros((128, 6, 128), np.float32)
    c[:, 0, :] = np.eye(128, dtype=np.float32)
    c[:, 1, :] = np.triu(np.ones((128, 128), np.float32))
    c[127, 2, :] = 1.0
    c[0:64, 3, 0:64] = 1.0 / 64
    c[64:128, 3, 64:128] = 1.0 / 64
    c[:, 4, :] = 1.0
    c[:, 5, :] = np.triu(np.ones((128, 128), np.float32), k=1)
    return c


def _jv(nst):
    j = np.zeros((128, nst + 1), np.float32)
    j[:, 0:nst] = 512.0 * np.arange(nst, dtype=np.float32)[None, :]
    j[:, nst] = np.arange(128, dtype=np.float32)
    return j


def _fm(v):
    return np.ascontiguousarray(np.asarray(v, np.float32).reshape(-1, 128).T)


def make_in_maps(inputs, n_cores, NB, S):
    f = lambda a: np.ascontiguousarray(np.asarray(a, np.float32))
    x = f(inputs["x"])
    c = f(inputs["c"])
    shared = {
        "w_ada": f(inputs["w_ada"][0]),
        "b_adaT": _fm(inputs["b_ada"][0]),
        "n1wT": _fm(inputs["norm1_w"][0]),
        "n2wT": _fm(inputs["norm2_w"][0]),
        "w_in": f(inputs["w_in"][0]),
        "bfg": f(np.broadcast_to(np.asarray(inputs["b_forget"][0], np.float32)[None, :], (128, NH))),
        "convT": np.ascontiguousarray(np.asarray(inputs["conv_w"][0], np.float32).reshape(3, 4, 128).transpose(2, 1, 0)),
        "qkw": np.ascontiguousarray(np.stack([np.tile(np.asarray(inputs["q_norm_w"][0], np.float32), 2),
                                              np.tile(np.asarray(inputs["k_norm_w"][0], np.float32), 2)], axis=1)),
        "w_oc": f(inputs["w_out_conv"][0]),
        "w_oa": f(inputs["w_out_attn"][0]),
        "w_o": f(inputs["w_o"][0]),
        "w_r": np.ascontiguousarray(np.concatenate([np.asarray(inputs["w_router_group"][0], np.float32),
                                                    np.asarray(inputs["w_router_expert"][0], np.float32)], axis=1)),
        "b_r": f(np.broadcast_to(np.concatenate([np.asarray(inputs["b_router_group"][0], np.float32),
                                                 np.asarray(inputs["b_router_expert"][0], np.float32)])[None, :], (128, 36))),
        "w_gate": f(inputs["w_gate"][0]),
        "w_up": f(inputs["w_up"][0]),
        "w_down": f(inputs["w_down"][0]),
        "consts": _consts(),
        "zeros_bf": np.zeros((1024, D), dtype=ml_dtypes.bfloat16),
        "jv": _jv((2 * NB * S) // 512 + NE),
    }
    maps = []
    for core in range(n_cores):
        bs = slice(core * NB, (core + 1) * NB)
        m = dict(shared)
        m["x"] = np.ascontiguousarray(x[bs].reshape(NB * S, D))
        cb = c[bs]
        m["cT"] = np.ascontiguousarray(cb.reshape(NB, KC, 128).transpose(2, 1, 0))
        maps.append(m)
    return maps


def kernel(**inputs):
    x = np.asarray(inputs["x"])
    B, S, _ = x.shape
    n_cores = 8
    NB = B // n_cores
    nc = build_nc(NB, S)
    in_maps = make_in_maps(inputs, n_cores, NB, S)
    res = run_bass_kernel_spmd(nc, in_maps, core_ids=list(range(n_cores)))
    outs = [np.asarray(r["out"]).reshape(NB, S, D) for r in res.results]
    return np.concatenate(outs, axis=0).astype(np.float32)
```

```python
import contextlib
import numpy as np
import ml_dtypes
import concourse.bass as bass
import concourse.mybir as mybir
from concourse.bass_utils import run_bass_kernel_spmd
from concourse.alu_op_type import AluOpType as ALU

F32 = mybir.dt.float32
BF16 = mybir.dt.bfloat16
AF = mybir.ActivationFunctionType
AX = mybir.AxisListType

D = 1024
KC = 8
NH = 8
HD = 64
NE = 32
FE = 256
IN_COLS = 5128
EPS = 1e-6
COMPUTE = ("pe", "act", "dve", "pool")


class Buf:
    __slots__ = ("name", "w", "r")

    ALL = []

    def __init__(self, name):
        self.name = name
        self.w = None
        self.r = []
        Buf.ALL.append(self)


class Sched:
    def __init__(self, nc, tag):
        self.nc = nc
        self.tag = tag
        self.streams = {k: [] for k in ("pe", "act", "dve", "pool", "sp")}
        self.cnt = {}
        self.waited = {}
        for b in Buf.ALL:
            b.w = None
            b.r = []

    def _deps(self, eng, reads, writes):
        deps = []
        for b in reads:
            if b.w is not None:
                deps.append(b.w)
        for b in writes:
            if b.w is not None:
                deps.append(b.w)
            deps.extend(b.r)
        out = {}
        for (sk, val, e2) in deps:
            if eng == "pe" and e2 == "pe":
                continue
            if self.waited.get((eng, sk), 0) >= val:
                continue
            if out.get(sk, 0) < val:
                out[sk] = val
        for sk, val in out.items():
            self.waited[(eng, sk)] = val
        return list(out.items())

    def op(self, eng, fn, reads=(), writes=(), chan=None, ndma=1):
        reads = [b for b in reads if b is not None]
        writes = [b for b in writes if b is not None]
        waits = self._deps(eng, reads, writes)
        if chan is not None:
            sk = ("dma", chan)
            prev = self.cnt.get(sk, 0)
            if prev > 0 and self.waited.get((eng, sk), 0) < prev:
                waits.append((sk, prev))
                self.waited[(eng, sk)] = prev
            val = prev + 16 * ndma
            inc = 16
        else:
            sk = ("eng", eng)
            val = self.cnt.get(sk, 0) + 1
            inc = 1
        self.cnt[sk] = val
        ev = (sk, val, eng if chan is None else "dma")
        self.streams[eng].append((waits, fn, sk, inc))
        for b in writes:
            b.w = ev
            b.r = []
        for b in reads:
            if b not in writes:
                b.r.append(ev)
        return ev

    def fence(self, eng, prefix):
        waits = []
        for sk, v in self.cnt.items():
            if sk[0] == "dma" and str(sk[1]).startswith(prefix) and self.waited.get((eng, sk), 0) < v:
                waits.append((sk, v))
                self.waited[(eng, sk)] = v
        self.streams[eng].append((waits, None, None, 0))

    def finish(self):
        waits = [(sk, v) for sk, v in self.cnt.items() if sk[0] == "dma"]
        self.streams["sp"].append((waits, None, None, 0))
        nc = self.nc
        with contextlib.ExitStack() as st:
            sems = {}
            for i, sk in enumerate(self.cnt.keys()):
                sems[sk] = st.enter_context(nc.semaphore("%s_s%d" % (self.tag, i)))
            block = st.enter_context(nc.Block())

            def run(stream):
                def body(e):
                    for (waits, fn, sk, inc) in stream:
                        for (wsk, wval) in waits:
                            e.wait_ge(sems[wsk], wval)
                        if fn is None:
                            continue
                        r = fn(e)
                        if isinstance(r, (list, tuple)):
                            for ins in r:
                                ins.then_inc(sems[sk], inc)
                        else:
                            r.then_inc(sems[sk], inc)
                return body

            for name, attr in (("pe", "tensor"), ("act", "scalar"), ("dve", "vector"),
                               ("pool", "gpsimd"), ("sp", "sync")):
                if self.streams[name]:
                    getattr(block, attr)(run(self.streams[name]))


class Ring:
    def __init__(self, items):
        self.items = items
        self.i = 0

    def next(self):
        it = self.items[self.i % len(self.items)]
        self.i += 1
        return it


class Ops:
    def __init__(self, S):
        self.S = S

    def mm(self, out, lhsT, rhs, start, stop, reads, writes):
        self.S.op("pe", lambda e: e.matmul(out, lhsT=lhsT, rhs=rhs, start=start, stop=stop),
                  reads, writes)

    def tr(self, out, in_, ident, reads, writes):
        self.S.op("pe", lambda e: e.transpose(out, in_, ident), reads, writes)

    def act(self, out, in_, func, reads, writes, bias=None, scale=None, accum=None):
        kw = {}
        if bias is not None:
            kw["bias"] = bias
        if scale is not None:
            kw["scale"] = scale
        if accum is not None:
            kw["accum_out"] = accum
        self.S.op("act", lambda e: e.activation(out=out, in_=in_, func=func, **kw), reads, writes)

    def ts(self, eng, out, in0, s1, s2, op0, op1, reads, writes):
        if op1 is None:
            self.S.op(eng, lambda e: e.tensor_scalar(out=out, in0=in0, scalar1=s1, scalar2=None, op0=op0),
                      reads, writes)
        else:
            self.S.op(eng, lambda e: e.tensor_scalar(out=out, in0=in0, scalar1=s1, scalar2=s2, op0=op0, op1=op1),
                      reads, writes)

    def tt(self, eng, out, in0, in1, op, reads, writes):
        self.S.op(eng, lambda e: e.tensor_tensor(out=out, in0=in0, in1=in1, op=op), reads, writes)

    def stt(self, out, in0, scalar, in1, op0, op1, reads, writes):
        self.S.op("dve", lambda e: e.scalar_tensor_tensor(out=out, in0=in0, scalar=scalar, in1=in1,
                                                          op0=op0, op1=op1), reads, writes)

    def copy(self, eng, out, in_, reads, writes):
        self.S.op(eng, lambda e: e.tensor_copy(out=out, in_=in_), reads, writes)

    def memset(self, eng, ap, val, writes):
        self.S.op(eng, lambda e: e.memset(ap, val), (), writes)

    def dma(self, q, out, in_, reads, writes, chan):
        self.S.op(q, lambda e: e.dma_start(out=out, in_=in_), reads, writes, chan=chan)

    def dman(self, q, pairs, reads, writes, chan):
        self.S.op(q, lambda e: [e.dma_start(out=o, in_=i) for (o, i) in pairs], reads, writes, chan=chan,
                  ndma=len(pairs))


def build_nc(NB, S, debug=False, run_b=True, taps=()):
    assert S % 512 == 0
    NCH = S // 512
    NBLK = S // 128
    NTOK = NB * S
    TB = min(2048, S)
    nc = bass.Bass("TRN2", target_bir_lowering=False)
    Buf.ALL = []

    def din(name, shape, dt=F32):
        return nc.dram_tensor(name, list(shape), dt, kind="ExternalInput").ap()

    x_d = din("x", [NTOK, D])
    cT_d = din("cT", [128, KC, NB])
    wada_d = din("w_ada", [D, 6 * D])
    badaT_d = din("b_adaT", [128, 48])
    n1w_d = din("n1wT", [128, KC])
    n2w_d = din("n2wT", [128, KC])
    win_d = din("w_in", [D, IN_COLS])
    bfg_d = din("bfg", [128, NH])
    convT_d = din("convT", [128, 4, 3])
    qkw_d = din("qkw", [128, 2])
    woc_d = din("w_oc", [512, D])
    woa_d = din("w_oa", [512, D])
    wo_d = din("w_o", [D, D])
    wr_d = din("w_r", [D, 36])
    br_d = din("b_r", [128, 36])
    wg_d = din("w_gate", [NE, D, FE])
    wu_d = din("w_up", [NE, D, FE])
    wd_d = din("w_down", [NE, FE, D])
    consts_d = din("consts", [128, 6, 128])
    jv_d = din("jv", [128, (2 * NTOK) // 512 + NE + 1])
    zeros_d = din("zeros_bf", [1024, D], BF16)
    out_d = nc.dram_tensor("out", [NTOK, D], F32, kind="ExternalOutput").ap()

    win_bf = nc.dram_tensor("win_bf", [D, IN_COLS], BF16).ap()
    woc_bf = nc.dram_tensor("woc_bf", [512, D], BF16).ap()
    woa_bf = nc.dram_tensor("woa_bf", [512, D], BF16).ap()
    wos_bf = nc.dram_tensor("wos_bf", [NB, D, D], BF16).ap()
    W1 = nc.dram_tensor("W1_bf", [NE * 128, 4096], BF16).ap()
    W2 = nc.dram_tensor("W2_bf", [NE * 128, 2048], BF16).ap()

    NST_ = (2 * NTOK) // 512 + NE
    Xs = nc.dram_tensor("Xs", [NST_ * 512, D], BF16).ap()
    dbg_outs = {}
    tap_state = {"S": None}

    def dtap(name, ap, bufs, shape, dt):
        if name not in taps or name in dbg_outs:
            return
        t = nc.dram_tensor("dbg_" + name, list(shape), dt, kind="ExternalOutput").ap()
        dbg_outs[name] = t
        tap_state["S"].op("sp", lambda e: e.dma_start(out=t, in_=ap), bufs, (), chan="dbg_" + name)

    with contextlib.ExitStack() as top:
        def sbt(ctx, name, shape, dt):
            return ctx.enter_context(nc.sbuf_tensor(name, list(shape), dt))

        constsF = sbt(top, "constsF", [128, 6, 128], F32)
        identF = constsF[:, 0, :]
        triF = constsF[:, 1, :]
        elastF = constsF[:, 2, :]
        onesF = constsF[:, 4, :]
        identB = sbt(top, "identB", [128, 128], BF16)
        triB = sbt(top, "triB", [128, 128], BF16)
        bonesB = sbt(top, "bonesB", [128, 128], BF16)
        diagW = sbt(top, "diagW", [128, 12, 128], BF16)
        modT = sbt(top, "modT", [128, 48, NB], F32)
        A1 = sbt(top, "A1", [128, NB, KC], F32)
        B1 = sbt(top, "B1", [128, NB, KC], F32)
        A2 = sbt(top, "A2", [128, NB, KC], F32)
        B2 = sbt(top, "B2", [128, NB, KC], F32)
        qkws = sbt(top, "qkws", [128, 2], F32)
        bfg = sbt(top, "bfg_sb", [128, NH], F32)
        wfB = sbt(top, "wfB", [128, KC, NH], BF16)
        wrB = sbt(top, "wrB", [128, KC, 36], BF16)
        brt = sbt(top, "brt", [128, 36], F32)
        psum = [top.enter_context(nc.psum_tensor("ps%d" % i, [128, 512], F32)) for i in range(8)]
        pbuf = [Buf("ps%d" % i) for i in range(8)]
        bconst = Buf("consts")
        bmoe_w = [Buf("moew%d" % e) for e in range(NE)]
        b_winbf = Buf("winbf")
        b_wos = Buf("wosbf")
        b_outd = [Buf("out%d" % i) for i in range(NTOK // 128)]

        with contextlib.ExitStack() as ph:
            S_ = Sched(nc, "P")
            tap_state["S"] = S_
            O = Ops(S_)
            cT = sbt(ph, "cT_sb", [128, KC, NB], F32)
            sc = sbt(ph, "sc_sb", [128, KC, NB], F32)
            th = sbt(ph, "th_sb", [128, KC, NB], F32)
            badaT = sbt(ph, "badaT_sb", [128, 48], F32)
            n1w = sbt(ph, "n1w_sb", [128, KC], F32)
            n2w = sbt(ph, "n2w_sb", [128, KC], F32)
            convT = sbt(ph, "convT_sb", [128, 4, 3], F32)
            qkw = sbt(ph, "qkw_sb", [128, 2], F32)
            wfF = sbt(ph, "wfF", [128, KC, NH], F32)
            wrF = sbt(ph, "wrF", [128, KC, 36], F32)
            wa = [sbt(ph, "wa%d" % i, [128, KC, 768], F32) for i in range(2)]
            bwa = [Buf("wa%d" % i) for i in range(2)]
            woF = sbt(ph, "woF", [128, KC, D], F32)
            g1bc = sbt(ph, "g1bc", [128, NB, D], F32)
            diag = [sbt(ph, "diag%d" % i, [128, 128], F32) for i in range(2)]
            bdiag = [Buf("diag%d" % i) for i in range(2)]
            wtmp = [sbt(ph, "wtmp%d" % i, [128, D], BF16) for i in range(2)]
            bwtmp = [Buf("wtmp%d" % i) for i in range(2)]
            b_small = Buf("small_in")
            b_sc = Buf("sc")
            b_woF = Buf("woF")
            b_g1 = Buf("g1bc")
            b_modps = pbuf[0]

            ci = 0
            for r in range(8):
                O.dma("pool", win_bf[r * 128:(r + 1) * 128, :], win_d[r * 128:(r + 1) * 128, :], (), [b_winbf],
                      "cast%d" % (ci % 8)); ci += 1
            for r in range(4):
                O.dma("pool", woc_bf[r * 128:(r + 1) * 128, :], woc_d[r * 128:(r + 1) * 128, :], (), [b_winbf],
                      "cast%d" % (ci % 8)); ci += 1
                O.dma("pool", woa_bf[r * 128:(r + 1) * 128, :], woa_d[r * 128:(r + 1) * 128, :], (), [b_winbf],
                      "cast%d" % (ci % 8)); ci += 1

            nsm = [0]
            for dst, src in ((constsF[:], consts_d[:, :, :]), (cT[:], cT_d[:, :, :]), (badaT[:], badaT_d[:, :]),
                             (n1w[:], n1w_d[:, :]), (n2w[:], n2w_d[:, :]), (convT[:], convT_d[:, :, :]),
                             (qkw[:], qkw_d[:, :]), (bfg[:], bfg_d[:, :]), (brt[:], br_d[:, :])):
                O.dma("sp", dst, src, (), [b_small], "small%d" % nsm[0]); nsm[0] += 1
            O.dma("sp", wfF[:], win_d.rearrange("(kc p) n -> p kc n", p=128)[:, :, 3072:3080], (), [b_small], "smallA")
            O.dma("sp", wrF[:], wr_d.rearrange("(kc p) n -> p kc n", p=128), (), [b_small], "smallB")
            O.dma("sp", woF[:], wo_d.rearrange("(kc p) n -> p kc n", p=128), (), [b_woF], "woF")

            O.act(th[:], cT[:], AF.Tanh, [b_small], [b_sc], scale=0.5)
            O.stt(sc[:], th[:], 1.0, cT[:], ALU.add, ALU.mult, [b_small, b_sc], [b_sc])
            O.ts("dve", sc[:], sc[:], 0.5, None, ALU.mult, None, [b_sc], [b_sc])
            O.copy("dve", identB[:], identF, [b_small], [bconst])
            O.copy("dve", triB[:], triF, [b_small], [bconst])
            O.copy("dve", bonesB[:], constsF[:, 3, :], [b_small], [bconst])
            O.copy("dve", wfB[:], wfF[:], [b_small], [bconst])
            O.copy("dve", wrB[:], wrF[:], [b_small], [bconst])
            for j in range(4):
                for tap in range(3):
                    O.ts("dve", diagW[:, j * 3 + tap, :], identF, convT[:, j, tap:tap + 1], None, ALU.mult, None,
                         [b_small], [bconst])
            O.ts("dve", qkws[:, 0:1], qkw[:, 0:1], HD ** -0.5, None, ALU.mult, None, [b_small], [bconst])
            O.copy("dve", qkws[:, 1:2], qkw[:, 1:2], [b_small], [bconst])

            wada_v = wada_d.rearrange("(kc p) n -> p kc n", p=128)
            modps = psum[0]
            for g in range(8):
                w_, bw_ = wa[g % 2], bwa[g % 2]
                O.dma("sp", w_[:], wada_v[:, :, g * 768:(g + 1) * 768], (), [bw_], "wa%d" % (g % 2))
                for jj in range(6):
                    col = g * 6 + jj
                    for kc in range(KC):
                        O.mm(modps[:, col * NB:(col + 1) * NB], w_[:, kc, jj * 128:(jj + 1) * 128], sc[:, kc, :],
                             kc == 0, kc == KC - 1, [bw_, b_sc], [b_modps])
            O.tt("dve", modT[:], modps[:, 0:48 * NB].rearrange("p (a b) -> p a b", b=NB),
                 badaT[:, :, None].broadcast_to([128, 48, NB]), ALU.add, [b_modps, b_small], [bconst])
            for b in range(NB):
                O.stt(A1[:, b, :], modT[:, 8:16, b], 1.0, n1w[:], ALU.add, ALU.mult, [bconst, b_small], [bconst])
                O.copy("dve", B1[:, b, :], modT[:, 0:8, b], [bconst], [bconst])
                O.stt(A2[:, b, :], modT[:, 32:40, b], 1.0, n2w[:], ALU.add, ALU.mult, [bconst, b_small], [bconst])
                O.copy("dve", B2[:, b, :], modT[:, 24:32, b], [bconst], [bconst])
            k = 0
            for b in range(NB):
                for half in range(2):
                    pb_, bb_ = psum[1 + half], pbuf[1 + half]
                    for q in range(4):
                        kc = half * 4 + q
                        dg, bdg = diag[k % 2], bdiag[k % 2]
                        k += 1
                        O.ts("dve", dg[:], identF, modT[:, 16 + kc, b:b + 1], 0.5, ALU.mult, ALU.mult,
                             [bconst, b_small], [bdg])
                        O.mm(pb_[:, q * 128:(q + 1) * 128], onesF, dg[:], True, True, [b_small, bdg], [bb_])
                    O.copy("dve", g1bc[:, b, half * 512:(half + 1) * 512], pb_[:, :], [bb_], [b_g1])
            k = 0
            for b in range(NB):
                for kc in range(KC):
                    wt, bwt = wtmp[k % 2], bwtmp[k % 2]
                    k += 1
                    O.tt("pool", wt[:], woF[:, kc, :], g1bc[:, b, :], ALU.mult, [b_woF, b_g1], [bwt])
                    O.dma("sp", wos_bf[b, kc * 128:(kc + 1) * 128, :], wt[:], [bwt], [b_wos], "wos%d" % (k % 2))
            dtap("modT", modT[:], [bconst], [128, 48, NB], F32)
            dtap("A1", A1[:], [bconst], [128, NB, KC], F32)
            dtap("g1bc", g1bc[:], [b_g1], [128, NB, D], F32)
            S_.finish()

        with contextlib.ExitStack() as ph:
            S_ = Sched(nc, "A")
            tap_state["S"] = S_
            O = Ops(S_)
            Kc = sbt(ph, "Kc", [128, 4, S], BF16)
            Vc = sbt(ph, "Vc", [128, NBLK, NH, HD + 1], BF16)
            Gall = sbt(ph, "Gall", [128, NBLK, NH], F32)
            nbias = sbt(ph, "nbias", [128, NBLK, NH], F32)
            bKc = [Buf("Kc%d" % i) for i in range(NCH)]
            bVc = [Buf("Vc%d" % i) for i in range(NCH)]
            bVones = Buf("Vones")
            bGall = [Buf("Gall%d" % i) for i in range(NBLK)]
            bnbias = Buf("nbias")
            NSLOT = 4
            wslot = [sbt(ph, "wslot%d" % i, [128, 4096], BF16) for i in range(NSLOT)]
            bslot = [Buf("wslot%d" % i) for i in range(NSLOT)]
            slots = Ring(list(range(NSLOT)))
            xr = [sbt(ph, "xr%d" % i, [128, D], F32) for i in range(2)]
            xring = Ring([(xr[i], Buf("xr%d" % i), "xr%d" % i) for i in range(2)])
            onesB = sbt(ph, "onesB", [128, 64], BF16)
            rlb = [sbt(ph, "rlb%d" % i, [128, 512], BF16) for i in range(2)]
            brlb = [Buf("rlb%d" % i) for i in range(2)]
            x1ts = [sbt(ph, "x1t%d" % i, [128, D], F32) for i in range(2)]
            x1ring = Ring([(x1ts[i], Buf("x1t%d" % i), "x1st%d" % i) for i in range(2)])
            xn = [sbt(ph, "xn%d" % i, [128, D], BF16) for i in range(4)]
            bxn = [Buf("xn%d" % i) for i in range(4)]
            ss = sbt(ph, "ss", [128, 4], F32); bss = Buf("ss")
            lnv = sbt(ph, "lnv", [128, 4], F32)
            rstd = sbt(ph, "rstd", [128, 4], F32); brstd = Buf("rstd")
            hTs = [(sbt(ph, "hT%d" % i, [128, KC, 512], BF16), Buf("hT%d" % i)) for i in range(2)]
            xin_sb = sbt(ph, "xin_sb", [128, 4, 512], BF16); bxin = Buf("xin")
            b_sb = sbt(ph, "b_sb", [128, 4, 512], BF16); bbsb = Buf("bsb")
            ub = [sbt(ph, "ub%d" % j, [128, 514], BF16) for j in range(4)]
            bub = [Buf("ub%d" % j) for j in range(4)]
            yaT = sbt(ph, "yaT", [128, 4, 512], BF16); byaT = Buf("yaT")
            sq = sbt(ph, "sq", [128, 512], BF16); bsq = Buf("sq")
            rsb = [sbt(ph, "rsb%d" % i, [128, 512], F32) for i in range(1)]
            brsb = [Buf("rsb%d" % i) for i in range(1)]
            rsring = Ring(list(range(1)))
            qT = sbt(ph, "qTz", [128, NH, 512], BF16); bqT = Buf("qT")
            PT = [sbt(ph, "PT%d" % i, [128, 512], BF16) for i in range(4)]
            PTring = Ring([(PT[i], Buf("PT%d" % i)) for i in range(4)])
            zf = sbt(ph, "zf", [128, 32], F32); bzf = Buf("zf")
            spf = sbt(ph, "spf", [128, 32], F32); bspf = Buf("spf")
            Gmid = sbt(ph, "Gmid", [128, NH], F32); bGmid = Buf("Gmid")
            rl = [sbt(ph, "rl%d" % i, [128, 512], F32) for i in range(1)]
            brl = [Buf("rl0"), Buf("rl0b")]
            rl = [rl[0], rl[0]]
            brl = [brl[0], brl[0]]
            rlring = Ring(list(range(2)))
            bcs = sbt(ph, "bcs", [64, 512], F32); bbcs = Buf("bcs")
            ybT = sbt(ph, "ybT", [128, 4, 512], BF16); bybT = Buf("ybT")
            thr = [sbt(ph, "thr%d" % i, [128, 512], BF16) for i in range(2)]
            thring = Ring([(thr[i], Buf("thr%d" % i)) for i in range(2)])
            t12 = [sbt(ph, "t12_%d" % i, [128, 512], BF16) for i in range(2)]
            t12ring = Ring([(t12[i], Buf("t12_%d" % i)) for i in range(2)])
            mT = sbt(ph, "mT", [128, KC, 512], BF16); bmT = Buf("mT")

            Gring = Ring([(psum[i], pbuf[i]) for i in range(6)])
            Bring = Ring([(psum[i], pbuf[i]) for i in range(2)])
            Spairs = [[(psum[3], pbuf[3]), (psum[4], pbuf[4])], [(psum[5], pbuf[5]), (psum[2], pbuf[2])]]
            Oring = Ring([(psum[i], pbuf[i]) for i in (6, 7)])
            win_v = win_bf.rearrange("(kc p) n -> p kc n", p=128)
            woc_v = woc_bf.rearrange("(kc p) n -> p kc n", p=128)
            woa_v = woa_bf.rearrange("(kc p) n -> p kc n", p=128)

            def v8(t):
                return t[:, :].rearrange("p (k n) -> p k n", k=8)

            def v4(t):
                return t[:, :].rearrange("p (k n) -> p k n", k=4)

            def load_seg(c0, ncol=512):
                si = slots.next()
                O.dma("sp", v8(wslot[si])[:, :, 0:ncol], win_v[:, :, c0:c0 + ncol], [b_winbf], [bslot[si]],
                      "wsl%d" % si)
                return si

            moe_casts = []
            for e in range(NE):
                rows = slice(e * 128, (e + 1) * 128)
                moe_casts.append((W1[rows, 0:2048].rearrange("p (k n) -> p k n", k=KC),
                                  wg_d[e].rearrange("(kc p) n -> p kc n", p=128), e))
                moe_casts.append((W1[rows, 2048:4096].rearrange("p (k n) -> p k n", k=KC),
                                  wu_d[e].rearrange("(kc p) n -> p kc n", p=128), e))
                moe_casts.append((W2[rows, :].rearrange("p (f n) -> p f n", f=2),
                                  wd_d[e].rearrange("(f p) n -> p f n", p=128), e))
            n_total_chunks = NB * NCH
            per_chunk = -(-len(moe_casts) // n_total_chunks)
            cast_i = [0]

            def issue_casts(n):
                for _ in range(n):
                    if cast_i[0] >= len(moe_casts):
                        return
                    dst, src, e = moe_casts[cast_i[0]]
                    O.dma("pool", dst, src, (), [bmoe_w[e]], "mcast%d" % (cast_i[0] % 8))
                    cast_i[0] += 1

            O.memset("pool", Vc[:, :, :, HD:HD + 1], 1.0, [bVones])
            O.memset("pool", onesB[:], 1.0, [bconst])
            O.memset("pool", qT[:], 0.0, [bqT])

            chunks = [(b, c) for b in range(NB) for c in range(NCH)]

            norm_x = {}

            def stage_norm_dma(g, i):
                b, c = chunks[g]
                tok0 = b * S + c * 512
                xt, bxt, chn = xring.next()
                O.dma("sp", xt[:], x_d[tok0 + i * 128: tok0 + (i + 1) * 128, :], (), [bxt], chn)
                norm_x[(g, i)] = (xt, bxt)

            def stage_norm_compute(g, i):
                xt, bxt = norm_x.pop((g, i))
                O.act(xn[i][:], xt[:], AF.Square, [bxt], [bxn[i], bss], accum=ss[:, i:i + 1])
                O.act(lnv[:, i:i + 1], ss[:, i:i + 1], AF.Ln, [bss], [brstd], bias=EPS, scale=1.0 / D)
                O.act(rstd[:, i:i + 1], lnv[:, i:i + 1], AF.Exp, [brstd], [brstd], scale=-0.5)
                O.ts("pool", xn[i][:], xt[:], rstd[:, i:i + 1], None, ALU.mult, None, [bxt, brstd], [bxn[i]])

            def stage_norm(g):
                for i in range(4):
                    stage_norm_dma(g, i)
                    stage_norm_compute(g, i)

            def stage_tr(g):
                b, c = chunks[g]
                hT, bhT = hTs[g % 2]
                for kc in range(KC):
                    tp, btp = Gring.next()
                    tpb = tp[:, :].bitcast(BF16)
                    for i in range(4):
                        O.tr(tpb[:, i * 128:(i + 1) * 128], xn[i][:, kc * 128:(kc + 1) * 128], identB[:],
                             [bxn[i], bconst], [btp])
                    O.ts("dve", hT[:, kc, :], tpb[:, 0:512], A1[:, b, kc:kc + 1], B1[:, b, kc:kc + 1],
                         ALU.mult, ALU.add, [btp, bconst], [bhT])

            def stage_proj(g):
                b, c = chunks[g]
                hT, bhT = hTs[g % 2]
                pf, bpf = Gring.next()
                for i in range(4):
                    for kc in range(KC):
                        O.mm(pf[:, i * NH:(i + 1) * NH], hT[:, kc, i * 128:(i + 1) * 128], wfB[:, kc, :],
                             kc == 0, kc == KC - 1, [bconst, bhT], [bpf])
                O.tt("dve", zf[:, :].rearrange("p (i h) -> p i h", h=NH),
                     pf[:, 0:32].rearrange("p (i h) -> p i h", h=NH),
                     bfg[:, None, :].broadcast_to([128, 4, NH]), ALU.add, [bpf, bconst], [bzf])
                O.act(spf[:], zf[:], AF.Exp, [bzf], [bspf], scale=-1.0)
                O.act(spf[:], spf[:], AF.Ln, [bspf], [bspf], bias=1.0)
                for which in range(2):
                    si = load_seg((3 + which) * 512)
                    w8 = v8(wslot[si])
                    for hp in range(4):
                        pt, bpt = Gring.next()
                        for kc in range(KC):
                            O.mm(pt[:, :], w8[:, kc, hp * 128:(hp + 1) * 128], hT[:, kc, :], kc == 0,
                                 kc == KC - 1, [bslot[si], bhT], [bpt])
                        O.act(sq[:], pt[:, :], AF.Square, [bpt], [bsq])
                        pm, bpm = Gring.next()
                        O.mm(pm[:, :], bonesB[:], sq[:], True, True, [bconst, bsq], [bpm])
                        ri = rsring.next()
                        O.act(rsb[ri][:], pm[:, :], AF.Ln, [bpm], [brsb[ri]], bias=EPS)
                        O.act(rsb[ri][:], rsb[ri][:], AF.Exp, [brsb[ri]], [brsb[ri]], scale=-0.5)
                        if which == 0:
                            O.stt(qT[0:64, 2 * hp, :], pt[0:64, :], qkws[0:64, 0:1], rsb[ri][0:64, :], ALU.mult, ALU.mult,
                                  [bpt, bconst, brsb[ri]], [bqT])
                            O.stt(qT[64:128, 2 * hp + 1, :], pt[64:128, :], qkws[64:128, 0:1], rsb[ri][64:128, :],
                                  ALU.mult, ALU.mult, [bpt, bconst, brsb[ri]], [bqT])
                        else:
                            O.stt(Kc[:, hp, c * 512:(c + 1) * 512], pt[:, :], qkws[:, 1:2], rsb[ri][:],
                                  ALU.mult, ALU.mult, [bpt, bconst, brsb[ri]], [bKc[c]])
                si = load_seg(5 * 512)
                w8 = v8(wslot[si])
                for i in range(4):
                    pt, bpt = Gring.next()
                    for kc in range(KC):
                        O.mm(pt[:, :], hT[:, kc, i * 128:(i + 1) * 128], w8[:, kc, :], kc == 0, kc == KC - 1,
                             [bslot[si], bhT], [bpt])
                    O.copy("dve", Vc[:, c * 4 + i, :, 0:HD], pt[:, :].rearrange("p (h d) -> p h d", h=NH),
                           [bpt], [bVc[c]])

                for i in range(4):
                    blk = c * 4 + i
                    pg, bpg = Gring.next()
                    if blk == 0:
                        O.mm(pg[:, 0:NH], triF, spf[:, i * NH:(i + 1) * NH], True, True, [bconst, bspf], [bpg])
                    else:
                        O.mm(pg[:, 0:NH], triF, spf[:, i * NH:(i + 1) * NH], True, False, [bconst, bspf], [bpg])
                        O.mm(pg[:, 0:NH], elastF, Gall[:, blk - 1, :], False, True, [bconst, bGall[blk - 1]], [bpg])
                    O.copy("dve", Gall[:, blk, :], pg[:, 0:NH], [bpg], [bGall[blk]])
                pg, bpg = Gring.next()
                O.mm(pg[:, 0:NH], elastF, Gall[:, c * 4 + 1, :], True, True, [bconst, bGall[c * 4 + 1]], [bpg])
                O.copy("dve", Gmid[:], pg[:, 0:NH], [bpg], [bGmid])
                nkb = 4 * c + 4
                O.tt("dve", nbias[:, 0:nkb, :], Gall[:, 0:nkb, :], Gmid[:, None, :].broadcast_to([128, nkb, NH]),
                     ALU.subtract, [bGmid] + bGall[0:nkb], [bnbias])

            def conv_units(g, ring):
                b, c = chunks[g]
                hT, bhT = hTs[g % 2]
                units = []
                segslot = {}

                def seg_unit(seg, j, dst, bdst):
                    def fn():
                        if j == 0:
                            segslot[seg] = load_seg(seg * 512)
                        si = segslot[seg]
                        w8 = v8(wslot[si])
                        pt, bpt = ring.next()
                        for kc in range(KC):
                            O.mm(pt[:, :], w8[:, kc, j * 128:(j + 1) * 128], hT[:, kc, :], kc == 0, kc == KC - 1,
                                 [bslot[si], bhT], [bpt])
                            if kc == 3:
                                yield
                        O.copy("dve", dst[:, j, :], pt[:, :], [bpt], [bdst])
                        yield
                    return fn

                def conv_unit(j):
                    def fn():
                        if j == 0:
                            segslot[2] = load_seg(2 * 512)
                        si = segslot[2]
                        w8 = v8(wslot[si])
                        pt, bpt = ring.next()
                        for kc in range(KC):
                            O.mm(pt[:, :], w8[:, kc, j * 128:(j + 1) * 128], hT[:, kc, :], kc == 0, kc == KC - 1,
                                 [bslot[si], bhT], [bpt])
                            if kc == 3:
                                yield
                        if c == 0:
                            O.memset("dve", ub[j][:, 0:2], 0.0, [bub[j]])
                        else:
                            O.copy("dve", ub[j][:, 0:2], ub[j][:, 512:514], [bub[j]], [bub[j]])
                        O.tt("dve", ub[j][:, 2:514], pt[:, :], xin_sb[:, j, :], ALU.mult, [bpt, bxin], [bub[j]])
                        yield
                        pc, bpc = ring.next()
                        for tap in range(3):
                            O.mm(pc[:, :], diagW[:, j * 3 + tap, :], ub[j][:, tap:tap + 512], tap == 0, tap == 2,
                                 [bconst, bub[j]], [bpc])
                        O.tt("dve", yaT[:, j, :], pc[:, :], b_sb[:, j, :], ALU.mult, [bpc, bbsb], [byaT])
                        yield
                    return fn
                for j in range(4):
                    units.append(seg_unit(0, j, xin_sb, bxin))
                for j in range(4):
                    units.append(seg_unit(1, j, b_sb, bbsb))
                for j in range(4):
                    units.append(conv_unit(j))

                def steps():
                    for u in units:
                        for _ in u():
                            yield
                return steps()

            def stage_attn(g):
                b, c = chunks[g]
                nkb = 4 * c + 4

                def finalize(h, Ob, bOb):
                    hp, jj = divmod(h, 2)
                    lo = jj * 64
                    ri = rlring.next()
                    O.act(rl[ri][64:65, :], Ob[64:65, :], AF.Ln, [bOb], [brl[ri]])
                    O.act(rlb[ri][64:65, :], rl[ri][64:65, :], AF.Exp, [brl[ri]], [brlb[ri]], scale=-1.0)
                    pbc, bpbc = Bring.next()
                    O.mm(pbc[0:64, :], onesB[64:65, 0:64], rlb[ri][64:65, :], True, True, [bconst, brlb[ri]], [bpbc])
                    O.copy("dve", bcs[:, :], pbc[0:64, :], [bpbc], [bbcs])
                    O.tt("dve", ybT[lo:lo + 64, hp, :], Ob[0:64, :], bcs[:, :], ALU.mult, [bOb, bbcs], [bybT])

                prev_fin = None
                npairs = nkb // 2
                cunits = conv_units(g, Bring)
                items = [(h, p) for h in range(NH) for p in range(npairs)]
                spp = -(-28 // len(items))
                Obs = {}

                def qkpair(idx):
                    h, p = items[idx]
                    hp = h // 2
                    res = []
                    for t in range(2):
                        kb = 2 * p + t
                        d = kb - 4 * c
                        q0 = max(d, 0) * 128
                        Sb, bSb = Spairs[idx % 2][t]
                        O.mm(Sb[:, 0:512 - q0], Kc[:, hp, kb * 128:(kb + 1) * 128],
                             qT[:, h, q0:512], True, True, [bKc[kb // 4], bqT], [bSb])
                        res.append((Sb, bSb, q0, d, kb))
                    return res

                pend = [qkpair(i) for i in range(min(2, len(items)))]
                for idx, (h, p) in enumerate(items):
                    if p == 0:
                        Obs[h] = Oring.next()
                    Ob, bOb = Obs[h]
                    cur = pend.pop(0)
                    pts = []
                    for (Sb, bSb, q0, d, kb) in cur:
                        N = 512 - q0
                        Pt, bPt = PTring.next()
                        O.act(Pt[:, 0:N], Sb[:, 0:N], AF.Exp, [bSb, bnbias], [bPt], bias=nbias[:, kb, h:h + 1])
                        if d >= 0:
                            O.tt("pool", Pt[:, 0:128], Pt[:, 0:128], triB[:], ALU.mult, [bPt, bconst], [bPt])
                        pts.append((Pt, bPt, q0, N, kb))
                    allp = [x[1] for x in pts]
                    for (Pt, bPt, q0, N, kb) in pts:
                        O.mm(Ob[0:HD + 1, q0:512], Vc[:, kb, h, :], Pt[:, 0:N], kb == 0, kb == nkb - 1,
                             [bVc[kb // 4], bVones] + allp, [bOb])
                    if idx + 2 < len(items):
                        pend.append(qkpair(idx + 2))
                    if p == 0 and prev_fin is not None:
                        finalize(*prev_fin)
                        prev_fin = None
                    for _ in range(spp):
                        next(cunits, None)
                    if p == npairs - 1:
                        prev_fin = (h, Ob, bOb)
                        if g + 1 < len(chunks):
                            if 1 <= h <= 4:
                                stage_norm_compute(g + 1, h - 1)
                            if h <= 3:
                                stage_norm_dma(g + 1, h)
                        if h >= 1 and h <= 6:
                            issue_casts(1)
                finalize(*prev_fin)
                for _ in cunits:
                    pass

            def stage_post(g):
                b, c = chunks[g]
                tok0 = b * S + c * 512
                hT, bhT = hTs[g % 2]
                s_oc = slots.next()
                O.dma("sp", v4(wslot[s_oc]), woc_v, [b_winbf], [bslot[s_oc]], "wsl%d" % s_oc)
                s_oa = slots.next()
                O.dma("sp", v4(wslot[s_oa]), woa_v, [b_winbf], [bslot[s_oa]], "wsl%d" % s_oa)
                woc4, woa4 = v4(wslot[s_oc]), v4(wslot[s_oa])
                for jp in range(4):
                    sg = slots.next()
                    while sg in (s_oc, s_oa):
                        sg = slots.next()
                    g8 = v8(wslot[sg])
                    O.dman("sp", [(g8[:, :, 0:256], win_v[:, :, 3080 + jp * 256: 3080 + (jp + 1) * 256]),
                                  (g8[:, :, 256:512], win_v[:, :, 4104 + jp * 256: 4104 + (jp + 1) * 256])],
                           [b_winbf], [bslot[sg]], "wsl%d" % sg)
                    for j2 in range(2):
                        jf = jp * 2 + j2
                        pgc, bpgc = Gring.next()
                        for kc in range(KC):
                            O.mm(pgc[:, :], g8[:, kc, j2 * 128:(j2 + 1) * 128], hT[:, kc, :], kc == 0,
                                 kc == KC - 1, [bslot[sg], bhT], [bpgc])
                        thc, bthc = thring.next()
                        O.act(thc[:], pgc[:, :], AF.Tanh, [bpgc], [bthc], scale=0.5)
                        pa, bpa = Gring.next()
                        for kc in range(4):
                            O.mm(pa[:, :], woc4[:, kc, jf * 128:(jf + 1) * 128], yaT[:, kc, :], kc == 0, kc == 3,
                                 [bslot[s_oc], byaT], [bpa])
                        pga, bpga = Gring.next()
                        for kc in range(KC):
                            O.mm(pga[:, :], g8[:, kc, 256 + j2 * 128: 256 + (j2 + 1) * 128], hT[:, kc, :], kc == 0,
                                 kc == KC - 1, [bslot[sg], bhT], [bpga])
                        tha, btha = thring.next()
                        O.act(tha[:], pga[:, :], AF.Tanh, [bpga], [btha], scale=0.5)
                        t1, bt1 = t12ring.next()
                        O.stt(t1[:], thc[:], 1.0, pa[:, :], ALU.add, ALU.mult, [bthc, bpa], [bt1])
                        pb_, bpb_ = Gring.next()
                        for kc in range(4):
                            O.mm(pb_[:, :], woa4[:, kc, jf * 128:(jf + 1) * 128], ybT[:, kc, :], kc == 0, kc == 3,
                                 [bslot[s_oa], bybT], [bpb_])
                        t2, bt2 = t12ring.next()
                        O.stt(t2[:], tha[:], 1.0, pb_[:, :], ALU.add, ALU.mult, [btha, bpb_], [bt2])
                        O.tt("pool", mT[:, jf, :], t1[:], t2[:], ALU.add, [bt1, bt2], [bmT])
                if b == 0 and c == 0:
                    dtap("hT", hT[:], [bhT], [128, KC, 512], BF16)
                    dtap("yaT", yaT[:], [byaT], [128, 4, 512], BF16)
                    dtap("qT", qT[:], [bqT], [128, NH, 512], BF16)
                    dtap("Kc", Kc[:, :, 0:512], [bKc[0]], [128, 4, 512], BF16)
                    dtap("Vc", Vc[:, 0:4, :, :], [bVc[0], bVones], [128, 4, NH, HD + 1], BF16)
                    dtap("Gall", Gall[:, 0:4, :], bGall[0:4], [128, 4, NH], F32)
                    dtap("nbias", nbias[:, 0:4, :], [bnbias], [128, 4, NH], F32)
                    dtap("ybT", ybT[:], [bybT], [128, 4, 512], BF16)
                    dtap("mT", mT[:], [bmT], [128, KC, 512], BF16)
                s_o = []
                for n in range(2):
                    so = slots.next()
                    O.dma("sp", v8(wslot[so]), wos_bf[b].rearrange("(kc p) n -> p kc n", p=128)[:, :, n * 512:(n + 1) * 512],
                          [b_wos], [bslot[so]], "wsl%d" % so)
                    s_o.append(so)
                for i in range(4):
                    xt, bxt, chn = xring.next()
                    O.dma("sp", xt[:], x_d[tok0 + i * 128: tok0 + (i + 1) * 128, :], (), [bxt], chn)
                    x1t, bx1t, x1ch = x1ring.next()
                    for n in range(2):
                        po, bpo = Gring.next()
                        w8 = v8(wslot[s_o[n]])
                        for kc in range(KC):
                            O.mm(po[:, :], mT[:, kc, i * 128:(i + 1) * 128], w8[:, kc, :], kc == 0, kc == KC - 1,
                                 [bslot[s_o[n]], bmT], [bpo])
                        O.tt("dve", x1t[:, n * 512:(n + 1) * 512], po[:, :], xt[:, n * 512:(n + 1) * 512], ALU.add,
                             [bpo, bxt], [bx1t])
                    ti = (tok0 // 128) + i
                    O.dma("pool", out_d[ti * 128:(ti + 1) * 128, :], x1t[:], [bx1t], [b_outd[ti]], x1ch)

            NG = len(chunks)
            nzf = (NST_ * 512) // 1024
            zf_i = [0]
            bXs0 = Buf("Xs_zero")

            def issue_zero(n):
                for _ in range(n):
                    if zf_i[0] >= nzf:
                        return
                    r0 = zf_i[0] * 1024
                    O.dma("act", Xs[r0:r0 + 1024, :], zeros_d[:, :], (), [bXs0], "zf%d" % (zf_i[0] % 4))
                    zf_i[0] += 1
            zper = -(-nzf // NG)
            stage_norm(0)
            stage_tr(0)
            for g in range(NG):
                issue_zero(zper)
                stage_proj(g)
                stage_attn(g)
                if g + 1 < NG:
                    stage_tr(g + 1)
                stage_post(g)
            issue_casts(len(moe_casts))
            issue_zero(nzf)
            S_.finish()

        if not run_b:
            return nc
        NTT = NTOK // 128
        NST = (2 * NTOK) // 512 + NE
        NSLOTS = NST * 512
        Ybuf = nc.dram_tensor("Ybuf", [NSLOTS, D], BF16).ap()
        I32 = mybir.dt.int32
        sloti = sbt(top, "sloti", [128, NTT * 2], I32)
        wk = sbt(top, "wk", [128, NTT * 2], F32)
        widx = sbt(top, "widx", [128, NST], I32)
        g2bc = sbt(top, "g2bc_t", [128, NB, D], F32)

        def bcast_rows(O, dst, src_fn, ring, bufs_in, bdst, diag, bdiag):
            k = 0
            for b in range(NB):
                for half in range(2):
                    pb_, bb_ = ring.next()
                    for q in range(4):
                        kc = half * 4 + q
                        dg, bdg = diag[k % 2], bdiag[k % 2]
                        k += 1
                        O.ts("dve", dg[:], identF, src_fn(b, kc), None, ALU.mult, None, bufs_in, [bdg])
                        O.mm(pb_[:, q * 128:(q + 1) * 128], onesF, dg[:], True, True, [bconst, bdg], [bb_])
                    O.copy("dve", dst[:, b, half * 512:(half + 1) * 512], pb_[:, :], [bb_], [bdst])

        with contextlib.ExitStack() as ph:
            S_ = Sched(nc, "B1")
            tap_state["S"] = S_
            O = Ops(S_)
            H2all = sbt(ph, "H2all", [128, NTT, D], BF16)
            bH2 = [Buf("H2_%d" % i) for i in range(NTT)]
            OH1 = sbt(ph, "OH1", [128, NTT, NE], F32)
            OH2 = sbt(ph, "OH2", [128, NTT, NE], F32)
            bOH = [Buf("OH%d" % i) for i in range(NTT)]
            A2row = sbt(ph, "A2row", [128, NB, D], F32)
            B2row = sbt(ph, "B2row", [128, NB, D], F32)
            bA2r = Buf("A2row")
            bg2 = Buf("g2bc")
            diag = [sbt(ph, "bdiag%d" % i, [128, 128], F32) for i in range(2)]
            bdiag = [Buf("bdiag%d" % i) for i in range(2)]
            xr = [sbt(ph, "bxr%d" % i, [128, D], F32) for i in range(2)]
            xring = Ring([(xr[i], Buf("bxr%d" % i), "bxr%d" % i) for i in range(2)])
            tmpf = [sbt(ph, "tmpf%d" % i, [128, D], F32) for i in range(1)]
            tmpring = Ring([(tmpf[i], Buf("tmpf%d" % i)) for i in range(1)])
            Ar = [sbt(ph, "Ar%d" % i, [128, NE], F32) for i in range(2)]
            Aring = Ring([(Ar[i], Buf("Ar%d" % i)) for i in range(2)])
            ss = sbt(ph, "bss", [128, 4], F32); bss = Buf("bss")
            lnv = sbt(ph, "blnv", [128, 4], F32)
            rstd = sbt(ph, "brstd", [128, 4], F32); brstd = Buf("brstd")
            h2T = sbt(ph, "h2T", [128, KC, 512], BF16); bh2T = Buf("h2T")
            rb = {n: sbt(ph, "rb_" + n, [128, w], F32) for n, w in
                  (("lg", 144), ("gmax", 4), ("ohg", 16), ("sh", 16), ("ex", 16), ("sume", 4), ("psel", 4), ("pen", 16),
                   ("lem", 128), ("m8", 32), ("d21", 4), ("e2", 4), ("den", 4), ("rden", 4))}
            brt_ = Buf("rt")
            cntf = sbt(ph, "cntf", [128, NE], F32)
            padi = sbt(ph, "padi", [128, NE], I32)
            padf = sbt(ph, "padf", [128, NE], F32)
            base = sbt(ph, "base", [128, NE], F32)
            jv = sbt(ph, "jv_sb", [128, NST + 1], F32)
            texpf = sbt(ph, "texpf", [128, NST], F32)
            Rsum = sbt(ph, "Rsum", [128, NE], F32)
            Tt = sbt(ph, "Tt", [128, NE], F32)
            Tm = sbt(ph, "Tm", [128, NE], F32)
            slotf = sbt(ph, "slotf", [128, NTT * 2], F32)
            bcnt = Buf("cnt"); bRsum = Buf("Rsum"); bT = Buf("T"); bslot = Buf("slot")
            Dring = Ring([(psum[i], pbuf[i]) for i in range(8)])
            striF = constsF[:, 5, :]
            striB = sbt(ph, "striB", [128, 128], BF16)
            onesBb = sbt(ph, "onesBb", [128, 128], BF16)
            bstri = Buf("striB")

            bcast_rows(O, g2bc, lambda b, kc: modT[:, 40 + kc, b:b + 1], Dring, [bconst], bg2, diag, bdiag)
            bcast_rows(O, A2row, lambda b, kc: A2[:, b, kc:kc + 1], Dring, [bconst], bA2r, diag, bdiag)
            bcast_rows(O, B2row, lambda b, kc: B2[:, b, kc:kc + 1], Dring, [bconst], bA2r, diag, bdiag)
            O.dma("sp", jv[:], jv_d[:, :], (), [bcnt], "jv")

            TRring = Ring([(psum[i], pbuf[i]) for i in range(6)])
            PRring = Ring([(psum[i], pbuf[i]) for i in (6, 7)])

            def b1_norm(scn):
                    b = (scn * 512) // S
                    for i in range(4):
                        ti = scn * 4 + i
                        xt, bxt, chn = xring.next()
                        O.dma("sp", xt[:], out_d[ti * 128:(ti + 1) * 128, :], [b_outd[ti]], [bxt], chn)
                        O.act(H2all[:, ti, :], xt[:], AF.Square, [bxt], [bH2[ti], bss], accum=ss[:, i:i + 1])
                        O.act(lnv[:, i:i + 1], ss[:, i:i + 1], AF.Ln, [bss], [brstd], bias=EPS, scale=1.0 / D)
                        O.act(rstd[:, i:i + 1], lnv[:, i:i + 1], AF.Exp, [brstd], [brstd], scale=-0.5)
                        tf, btf = tmpring.next()
                        O.stt(tf[:], xt[:], rstd[:, i:i + 1], A2row[:, b, :], ALU.mult, ALU.mult, [bxt, brstd, bA2r], [btf])
                        O.tt("pool", H2all[:, ti, :], tf[:], B2row[:, b, :], ALU.add, [btf, bA2r], [bH2[ti]])

            def b1_tr(scn):
                    for kc in range(KC):
                        tp, btp = TRring.next()
                        tpb = tp[:, :].bitcast(BF16)
                        for i in range(4):
                            ti = scn * 4 + i
                            O.tr(tpb[:, i * 128:(i + 1) * 128], H2all[:, ti, kc * 128:(kc + 1) * 128], identB[:],
                                 [bH2[ti], bconst], [btp])
                        O.act(h2T[:, kc, :], tpb[:, 0:512], AF.Copy, [btp], [bh2T])

            def b1_router_mm(scn):
                    pr, bpr = PRring.next()
                    for i in range(4):
                        for kc in range(KC):
                            O.mm(pr[:, i * 36:(i + 1) * 36], h2T[:, kc, i * 128:(i + 1) * 128], wrB[:, kc, :], kc == 0,
                                 kc == KC - 1, [bh2T, bconst], [bpr])
                    return pr, bpr

            def b1_router_math(scn, pr, bpr):
                    R = [brt_]
                    BIG = 1.0e30
                    t0 = scn * 4
                    lg3 = rb["lg"][:, :].rearrange("p (t c) -> p t c", t=4)
                    O.tt("dve", lg3, pr[:, 0:144].rearrange("p (t c) -> p t c", t=4),
                         brt[:, None, :].broadcast_to([128, 4, 36]), ALU.add, [bpr, bconst], R)
                    S_.op("dve", lambda e, lg3=lg3: e.tensor_reduce(out=rb["gmax"][:], in_=lg3[:, :, 0:4], axis=AX.X,
                                                                   op=ALU.max), R, R)
                    gm_b = rb["gmax"][:, :, None].broadcast_to([128, 4, 4])
                    O.tt("dve", rb["ohg"][:, :].rearrange("p (t g) -> p t g", t=4), lg3[:, :, 0:4], gm_b, ALU.is_equal, R, R)
                    O.tt("dve", rb["sh"][:, :].rearrange("p (t g) -> p t g", t=4), lg3[:, :, 0:4], gm_b, ALU.subtract, R, R)
                    O.act(rb["ex"][:], rb["sh"][:], AF.Exp, R, R)
                    S_.op("dve", lambda e: e.tensor_reduce(out=rb["sume"][:], in_=rb["ex"][:, :].rearrange("p (t g) -> p t g", t=4),
                                                          axis=AX.X, op=ALU.add), R, R)
                    S_.op("dve", lambda e: e.reciprocal(out=rb["psel"][:], in_=rb["sume"][:]), R, R)
                    O.ts("dve", rb["pen"][:], rb["ohg"][:], BIG, -BIG, ALU.mult, ALU.add, R, R)
                    O.tt("dve", rb["lem"][:, :].rearrange("p (t g e) -> p t g e", t=4, g=4),
                         lg3[:, :, 4:36].rearrange("p t (g e) -> p t g e", g=4),
                         rb["pen"][:, :].rearrange("p (t g) -> p t g", t=4)[:, :, :, None].broadcast_to([128, 4, 4, 8]),
                         ALU.add, R, R)
                    for i in range(4):
                        S_.op("dve", lambda e, i=i: e.max(out=rb["m8"][:, i * 8:(i + 1) * 8], in_=rb["lem"][:, i * 32:(i + 1) * 32]), R, R)
                    m83 = rb["m8"][:, :].rearrange("p (t k) -> p t k", t=4)
                    lem3 = rb["lem"][:, :].rearrange("p (t e) -> p t e", t=4)
                    O.tt("dve", OH1[:, t0:t0 + 4, :], lem3, m83[:, :, 0:1].broadcast_to([128, 4, NE]), ALU.is_equal,
                         R, bOH[t0:t0 + 4])
                    O.tt("dve", OH2[:, t0:t0 + 4, :], lem3, m83[:, :, 1:2].broadcast_to([128, 4, NE]), ALU.is_equal,
                         R, bOH[t0:t0 + 4])
                    O.tt("dve", rb["d21"][:, :, None], m83[:, :, 1:2], m83[:, :, 0:1], ALU.subtract, R, R)
                    O.act(rb["e2"][:], rb["d21"][:], AF.Exp, R, R)
                    O.ts("dve", rb["den"][:], rb["e2"][:], 1.0, None, ALU.add, None, R, R)
                    S_.op("dve", lambda e: e.reciprocal(out=rb["rden"][:], in_=rb["den"][:]), R, R)
                    wk3 = wk[:, t0 * 2:(t0 + 4) * 2].rearrange("p (t k) -> p t k", k=2)
                    O.tt("dve", wk3[:, :, 0:1], rb["rden"][:, :, None], rb["psel"][:, :, None], ALU.mult, R, [bslot])
                    O.tt("dve", wk3[:, :, 1:2], wk3[:, :, 0:1], rb["e2"][:, :, None], ALU.mult, R + [bslot], [bslot])


            NSCN = NTOK // 512
            prs = {}
            for scn in range(NSCN):
                b1_norm(scn)
                if scn > 0:
                    b1_router_math(scn - 1, *prs.pop(scn - 1))
                b1_tr(scn)
                prs[scn] = b1_router_mm(scn)
            b1_router_math(NSCN - 1, *prs.pop(NSCN - 1))

            Ab = h2T[:, :, :].rearrange("p k n -> p (k n)")[:, 0:NTT * NE].rearrange("p (t e) -> p t e", e=NE)
            O.tt("dve", Ab, OH1[:, :, :], OH2[:, :, :], ALU.add, bOH, [bh2T])
            O.copy("dve", striB[:], striF, [bconst], [bstri])
            O.memset("dve", onesBb[:], 1.0, [bstri])
            pc_, bpc_ = Dring.next()
            for ti in range(NTT):
                O.mm(pc_[:, 0:NE], onesBb[:], Ab[:, ti, :], ti == 0, ti == NTT - 1, [bstri, bh2T], [bpc_])
            O.ts("dve", cntf[:], pc_[:, 0:NE], 511.0, None, ALU.add, None, [bpc_], [bcnt])
            O.copy("dve", padi[:], cntf[:], [bcnt], [bcnt])
            O.ts("dve", padi[:], padi[:], 9, 9, ALU.arith_shift_right, ALU.logical_shift_left, [bcnt], [bcnt])
            O.copy("dve", padf[:], padi[:], [bcnt], [bcnt])
            O.memset("dve", base[:, 0:1], 0.0, [bcnt])
            for e in range(1, NE):
                O.tt("dve", base[:, e:e + 1], base[:, e - 1:e], padf[:, e - 1:e], ALU.add, [bcnt], [bcnt])
            O.memset("dve", texpf[:], -1.0, [bcnt])
            for e in range(NE):
                O.stt(texpf[:], jv[:, 0:NST], base[:, e:e + 1], texpf[:], ALU.is_ge, ALU.add, [bcnt], [bcnt])
            O.ts("dve", texpf[:], texpf[:], 128.0, jv[:, NST:NST + 1], ALU.mult, ALU.add, [bcnt], [bcnt])
            O.copy("dve", widx[:], texpf[:], [bcnt], [bconst])
            TPB = 16 if NTT >= 16 else NTT
            NBK = NTT // TPB
            Abk = rb["lem"][:, :].bitcast(BF16)[:, 0:NBK * NE].rearrange("p (b e) -> p b e", e=NE)
            for bk in range(NBK):
                def red(e, bk=bk):
                    with nc.allow_low_precision(reason="exact small integer counts"):
                        return e.tensor_reduce(
                            out=Abk[:, bk, :], in_=Ab[:, bk * TPB:(bk + 1) * TPB, :].rearrange("p t e -> p e t"),
                            axis=AX.X, op=ALU.add)
                S_.op("dve", red, [bh2T], [brt_])
            tfT, btfT = tmpring.next()
            for bk in range(NBK):
                pk, bpk = Dring.next()
                for tl in range(TPB):
                    ti = bk * TPB + tl
                    reg = pk[:, tl * NE:(tl + 1) * NE]
                    last = (tl == 0 and bk == 0)
                    O.mm(reg, striB[:], Ab[:, ti, :], True, last, [bstri, bh2T], [bpk])
                    srcs = [Ab[:, bk * TPB + tj, :] for tj in range(tl)] + [Abk[:, bj, :] for bj in range(bk)]
                    for n_, src in enumerate(srcs):
                        O.mm(reg, onesBb[:], src, False, n_ == len(srcs) - 1, [bstri, bh2T, brt_], [bpk])
                W = TPB * NE
                Tb = tfT[:, 0:W].rearrange("p (t e) -> p t e", e=NE)
                Tm = tfT[:, 512:512 + W].rearrange("p (t e) -> p t e", e=NE)
                O.tt("dve", Tb, pk[:, 0:W].rearrange("p (t e) -> p t e", e=NE),
                     base[:, None, :].broadcast_to([128, TPB, NE]), ALU.add, [bpk, bcnt], [btfT])
                for k2, OHk in ((0, OH1), (1, OH2)):
                    O.tt("dve", Tm, Tb, OHk[:, bk * TPB:(bk + 1) * TPB, :], ALU.mult, [btfT] + bOH[bk * TPB:(bk + 1) * TPB], [btfT])
                    S_.op("dve", lambda e, bk=bk, k2=k2, Tm=Tm: e.tensor_reduce(
                        out=slotf[:, bk * TPB * 2:(bk + 1) * TPB * 2].rearrange("p (t k) -> p t k", k=2)[:, :, k2],
                        in_=Tm, axis=AX.X, op=ALU.add), [btfT], [bslot])
            O.copy("dve", sloti[:], slotf[:], [bslot], [bslot])
            dtap("slotf", slotf[:], [bslot], [128, NTT * 2], F32)
            dtap("cntf", cntf[:], [bcnt], [128, NE], F32)
            dtap("base", base[:], [bcnt], [128, NE], F32)
            dtap("wk", wk[:], [bslot], [128, NTT * 2], F32)
            for ti in range(NTT):
                for k2 in range(2):
                    col = ti * 2 + k2
                    S_.op("pool", lambda e, ti=ti, col=col: e.indirect_dma_start(
                        out=Xs[:, :], out_offset=bass.IndirectOffsetOnAxis(ap=sloti[:, col:col + 1], axis=0),
                        in_=H2all[:, ti, :], in_offset=None), [bslot, bH2[ti]], (), chan="sc%d" % (col % 8))
            S_.finish()

        with contextlib.ExitStack() as ph:
            S_ = Sched(nc, "B2")
            tap_state["S"] = S_
            O = Ops(S_)
            NWS = 4
            wsl = [sbt(ph, "ews%d" % i, [128, 3, 2048], BF16) for i in range(NWS)]
            bwsl = [Buf("ews%d" % i) for i in range(NWS)]
            wring = Ring(list(range(NWS)))
            xs = [sbt(ph, "xs%d" % i, [128, D], BF16) for i in range(8)]
            xsring = Ring([(xs[i], Buf("xs%d" % i), "xs%d" % i) for i in range(8)])
            hTs2 = [(sbt(ph, "ehT%d" % i, [128, KC, 512], BF16), Buf("ehT%d" % i)) for i in range(2)]
            aT = [sbt(ph, "aT%d" % i, [128, 2, 512], BF16) for i in range(2)]
            baT = [Buf("aT%d" % i) for i in range(2)]
            sgt = [sbt(ph, "sgt%d" % i, [128, 512], BF16) for i in range(2)]
            sgring = Ring([(sgt[i], Buf("sgt%d" % i)) for i in range(2)])
            yt = [sbt(ph, "yt%d" % i, [128, D], BF16) for i in range(4)]
            yring = Ring([(yt[i], Buf("yt%d" % i), "yst%d" % i) for i in range(4)])
            GUring = Ring([(psum[i], pbuf[i]) for i in range(4)])
            Dring = Ring([(psum[i], pbuf[i]) for i in range(4, 8)])

            def load_weights(j):
                wi = wring.next()
                S_.op("pool", lambda e: [
                    e.indirect_dma_start(out=wsl[wi][:, 0:2, :].rearrange("p a n -> p (a n)"), out_offset=None,
                                         in_=W1[:, :],
                                         in_offset=bass.IndirectOffsetOnAxis(ap=widx[:, j:j + 1], axis=0)),
                    e.indirect_dma_start(out=wsl[wi][:, 2, :], out_offset=None, in_=W2[:, :],
                                         in_offset=bass.IndirectOffsetOnAxis(ap=widx[:, j:j + 1], axis=0))],
                    [bconst], [bwsl[wi]], chan="ews%d" % wi, ndma=2)
                return wi

            def do_down(args):
                j, wi, ai = args
                wdv = wsl[wi][:, 2, :].rearrange("p (f n) -> p f n", f=2)
                for i in range(4):
                    y_, by_, ych = yring.next()
                    for n in range(2):
                        pd, bpd = Dring.next()
                        for f in range(2):
                            O.mm(pd[:, :], aT[ai][:, f, i * 128:(i + 1) * 128], wdv[:, f, n * 512:(n + 1) * 512],
                                 f == 0, f == 1, [baT[ai], bwsl[wi]], [bpd])
                        if n == 0:
                            O.act(y_[:, 0:512], pd[:, :], AF.Copy, [bpd], [by_])
                        else:
                            O.copy("dve", y_[:, 512:1024], pd[:, :], [bpd], [by_])
                    r0 = j * 512 + i * 128
                    O.dma("act", Ybuf[r0:r0 + 128, :], y_[:], [by_], (), ych)

            def load_tr(j):
                hT2, bhT2 = hTs2[j % 2]
                xts = []
                for i in range(4):
                    x_, bx_, xch = xsring.next()
                    r0 = j * 512 + i * 128
                    O.dma("sp", x_[:], Xs[r0:r0 + 128, :], (), [bx_], xch)
                    xts.append((x_, bx_))
                for kc in range(KC):
                    tp, btp = Dring.next()
                    for i in range(4):
                        O.mm(tp[:, i * 128:(i + 1) * 128], xts[i][0][:, kc * 128:(kc + 1) * 128], identB[:], True, True,
                             [xts[i][1], bconst], [btp])
                    if kc % 2 == 0:
                        O.copy("dve", hT2[:, kc, :], tp[:, :], [btp], [bhT2])
                    else:
                        O.act(hT2[:, kc, :], tp[:, :], AF.Copy, [btp], [bhT2])

            pend_down = None
            wis = {0: load_weights(0)}
            load_tr(0)
            for j in range(NST):
                wi = wis.pop(j)
                hT2, bhT2 = hTs2[j % 2]
                if j + 1 < NST:
                    wis[j + 1] = load_weights(j + 1)
                    load_tr(j + 1)
                wgv = wsl[wi][:, 0, :].rearrange("p (k n) -> p k n", k=KC)
                wuv = wsl[wi][:, 1, :].rearrange("p (k n) -> p k n", k=KC)
                ai = j % 2
                for f in range(2):
                    pg, bpg = GUring.next()
                    pu, bpu = GUring.next()
                    for kc in range(KC):
                        O.mm(pg[:, :], wgv[:, kc, f * 128:(f + 1) * 128], hT2[:, kc, :], kc == 0, kc == KC - 1,
                             [bwsl[wi], bhT2], [bpg])
                    for kc in range(KC):
                        O.mm(pu[:, :], wuv[:, kc, f * 128:(f + 1) * 128], hT2[:, kc, :], kc == 0, kc == KC - 1,
                             [bwsl[wi], bhT2], [bpu])
                    sg_, bsg_ = sgring.next()
                    O.act(sg_[:], pg[:, :], AF.Silu, [bpg], [bsg_])
                    O.tt("dve", aT[ai][:, f, :], sg_[:], pu[:, :], ALU.mult, [bsg_, bpu], [baT[ai]])
                if pend_down is not None:
                    do_down(pend_down)
                pend_down = (j, wi, ai)
            do_down(pend_down)

            S_.fence("pool", "yst")
            y1r = [sbt(ph, "y1r%d" % i, [128, D], BF16) for i in range(4)]
            y2r = [sbt(ph, "y2r%d" % i, [128, D], BF16) for i in range(4)]
            ygring = Ring([(y1r[i], y2r[i], Buf("yg%d" % i), "yg%d" % i) for i in range(4)])
            xr2 = [sbt(ph, "fxr%d" % i, [128, D], F32) for i in range(4)]
            xring2 = Ring([(xr2[i], Buf("fxr%d" % i), "fxr%d" % i) for i in range(4)])
            tf2 = [sbt(ph, "tf2_%d" % i, [128, D], F32) for i in range(3)]
            tring2 = Ring([(tf2[i], Buf("tf2_%d" % i)) for i in range(3)])
            otile = [sbt(ph, "otile%d" % i, [128, D], F32) for i in range(3)]
            oring = Ring([(otile[i], Buf("otile%d" % i), "ost%d" % i) for i in range(3)])
            for ti in range(NTT):
                b = (ti * 128) // S
                y1, y2, byg, gch = ygring.next()
                S_.op("pool", lambda e, ti=ti, y1=y1, y2=y2: [
                    e.indirect_dma_start(out=y1[:, :], out_offset=None, in_=Ybuf[:, :],
                                         in_offset=bass.IndirectOffsetOnAxis(ap=sloti[:, ti * 2:ti * 2 + 1], axis=0)),
                    e.indirect_dma_start(out=y2[:, :], out_offset=None, in_=Ybuf[:, :],
                                         in_offset=bass.IndirectOffsetOnAxis(ap=sloti[:, ti * 2 + 1:ti * 2 + 2], axis=0))],
                    [bconst], [byg], chan=gch, ndma=2)
                xt, bxt, chn = xring2.next()
                O.dma("sp", xt[:], out_d[ti * 128:(ti + 1) * 128, :], [b_outd[ti]], [bxt], chn)
                tf, btf = tring2.next()
                O.act(tf[:], y1[:], AF.Copy, [byg, bconst], [btf], scale=wk[:, ti * 2:ti * 2 + 1])
                O.stt(tf[:], y2[:], wk[:, ti * 2 + 1:ti * 2 + 2], tf[:], ALU.mult, ALU.add, [byg, bconst, btf], [btf])
                O.tt("dve", tf[:], tf[:], g2bc[:, b, :], ALU.mult, [btf, bconst], [btf])
                ot, bot, och = oring.next()
                O.tt("dve", ot[:], tf[:], xt[:], ALU.add, [btf, bxt], [bot])
                O.dma("act", out_d[ti * 128:(ti + 1) * 128, :], ot[:], [bot], [b_outd[ti]], och)
            S_.finish()
    return nc


def _consts():
    c = np.zeros((128, 6, 128), np.float32)
    c[:, 0, :] = np.eye(128, dtype=np.float32)
    c[:, 1, :] = np.triu(np.ones((128, 128), np.float32))
    c[127, 2, :] = 1.0
    c[0:64, 3, 0:64] = 1.0 / 64
    c[64:128, 3, 64:128] = 1.0 / 64
    c[:, 4, :] = 1.0
    c[:, 5, :] = np.triu(np.ones((128, 128), np.float32), k=1)
    return c


def _jv(nst):
    j = np.zeros((128, nst + 1), np.float32)
    j[:, 0:nst] = 512.0 * np.arange(nst, dtype=np.float32)[None, :]
    j[:, nst] = np.arange(128, dtype=np.float32)
    return j


def _fm(v):
    return np.ascontiguousarray(np.asarray(v, np.float32).reshape(-1, 128).T)


def make_in_maps(inputs, n_cores, NB, S):
    f = lambda a: np.ascontiguousarray(np.asarray(a, np.float32))
    x = f(inputs["x"])
    c = f(inputs["c"])
    shared = {
        "w_ada": f(inputs["w_ada"][0]),
        "b_adaT": _fm(inputs["b_ada"][0]),
        "n1wT": _fm(inputs["norm1_w"][0]),
        "n2wT": _fm(inputs["norm2_w"][0]),
        "w_in": f(inputs["w_in"][0]),
        "bfg": f(np.broadcast_to(np.asarray(inputs["b_forget"][0], np.float32)[None, :], (128, NH))),
        "convT": np.ascontiguousarray(np.asarray(inputs["conv_w"][0], np.float32).reshape(3, 4, 128).transpose(2, 1, 0)),
        "qkw": np.ascontiguousarray(np.stack([np.tile(np.asarray(inputs["q_norm_w"][0], np.float32), 2),
                                              np.tile(np.asarray(inputs["k_norm_w"][0], np.float32), 2)], axis=1)),
        "w_oc": f(inputs["w_out_conv"][0]),
        "w_oa": f(inputs["w_out_attn"][0]),
        "w_o": f(inputs["w_o"][0]),
        "w_r": np.ascontiguousarray(np.concatenate([np.asarray(inputs["w_router_group"][0], np.float32),
                                                    np.asarray(inputs["w_router_expert"][0], np.float32)], axis=1)),
        "b_r": f(np.broadcast_to(np.concatenate([np.asarray(inputs["b_router_group"][0], np.float32),
                                                 np.asarray(inputs["b_router_expert"][0], np.float32)])[None, :], (128, 36))),
        "w_gate": f(inputs["w_gate"][0]),
        "w_up": f(inputs["w_up"][0]),
        "w_down": f(inputs["w_down"][0]),
        "consts": _consts(),
        "zeros_bf": np.zeros((1024, D), dtype=ml_dtypes.bfloat16),
        "jv": _jv((2 * NB * S) // 512 + NE),
    }
    maps = []
    for core in range(n_cores):
        bs = slice(core * NB, (core + 1) * NB)
        m = dict(shared)
        m["x"] = np.ascontiguousarray(x[bs].reshape(NB * S, D))
        cb = c[bs]
        m["cT"] = np.ascontiguousarray(cb.reshape(NB, KC, 128).transpose(2, 1, 0))
        maps.append(m)
    return maps


def kernel(**inputs):
    x = np.asarray(inputs["x"])
    B, S, _ = x.shape
    n_cores = 8
    NB = B // n_cores
    nc = build_nc(NB, S)
    in_maps = make_in_maps(inputs, n_cores, NB, S)
    res = run_bass_kernel_spmd(nc, in_maps, core_ids=list(range(n_cores)))
    outs = [np.asarray(r["out"]).reshape(NB, S, D) for r in res.results]
    return np.concatenate(outs, axis=0).astype(np.float32)
```

```python
import contextlib
import numpy as np
import ml_dtypes
import concourse.bass as bass
import concourse.mybir as mybir
from concourse.bass_utils import run_bass_kernel_spmd
from concourse.alu_op_type import AluOpType as ALU

F32 = mybir.dt.float32
BF16 = mybir.dt.bfloat16
AF = mybir.ActivationFunctionType
AX = mybir.AxisListType

D = 1024
KC = 8
NH = 8
HD = 64
NE = 32
FE = 256
IN_COLS = 5128
EPS = 1e-6
COMPUTE = ("pe", "act", "dve", "pool")


class Buf:
    __slots__ = ("name", "w", "r")

    ALL = []

    def __init__(self, name):
        self.name = name
        self.w = None
        self.r = []
        Buf.ALL.append(self)


class Sched:
    def __init__(self, nc, tag):
        self.nc = nc
        self.tag = tag
        self.streams = {k: [] for k in ("pe", "act", "dve", "pool", "sp")}
        self.cnt = {}
        self.waited = {}
        for b in Buf.ALL:
            b.w = None
            b.r = []

    def _deps(self, eng, reads, writes):
        deps = []
        for b in reads:
            if b.w is not None:
                deps.append(b.w)
        for b in writes:
            if b.w is not None:
                deps.append(b.w)
            deps.extend(b.r)
        out = {}
        for (sk, val, e2) in deps:
            if eng == "pe" and e2 == "pe":
                continue
            if self.waited.get((eng, sk), 0) >= val:
                continue
            if out.get(sk, 0) < val:
                out[sk] = val
        for sk, val in out.items():
            self.waited[(eng, sk)] = val
        return list(out.items())

    def op(self, eng, fn, reads=(), writes=(), chan=None, ndma=1):
        reads = [b for b in reads if b is not None]
        writes = [b for b in writes if b is not None]
        waits = self._deps(eng, reads, writes)
        if chan is not None:
            sk = ("dma", chan)
            prev = self.cnt.get(sk, 0)
            if prev > 0 and self.waited.get((eng, sk), 0) < prev:
                waits.append((sk, prev))
                self.waited[(eng, sk)] = prev
            val = prev + 16 * ndma
            inc = 16
        else:
            sk = ("eng", eng)
            val = self.cnt.get(sk, 0) + 1
            inc = 1
        self.cnt[sk] = val
        ev = (sk, val, eng if chan is None else "dma")
        self.streams[eng].append((waits, fn, sk, inc))
        for b in writes:
            b.w = ev
            b.r = []
        for b in reads:
            if b not in writes:
                b.r.append(ev)
        return ev

    def fence(self, eng, prefix):
        waits = []
        for sk, v in self.cnt.items():
            if sk[0] == "dma" and str(sk[1]).startswith(prefix) and self.waited.get((eng, sk), 0) < v:
                waits.append((sk, v))
                self.waited[(eng, sk)] = v
        self.streams[eng].append((waits, None, None, 0))

    def finish(self):
        waits = [(sk, v) for sk, v in self.cnt.items() if sk[0] == "dma"]
        self.streams["sp"].append((waits, None, None, 0))
        nc = self.nc
        with contextlib.ExitStack() as st:
            sems = {}
            for i, sk in enumerate(self.cnt.keys()):
                sems[sk] = st.enter_context(nc.semaphore("%s_s%d" % (self.tag, i)))
            block = st.enter_context(nc.Block())

            def run(stream):
                def body(e):
                    for (waits, fn, sk, inc) in stream:
                        for (wsk, wval) in waits:
                            e.wait_ge(sems[wsk], wval)
                        if fn is None:
                            continue
                        r = fn(e)
                        if isinstance(r, (list, tuple)):
                            for ins in r:
                                ins.then_inc(sems[sk], inc)
                        else:
                            r.then_inc(sems[sk], inc)
                return body

            for name, attr in (("pe", "tensor"), ("act", "scalar"), ("dve", "vector"),
                               ("pool", "gpsimd"), ("sp", "sync")):
                if self.streams[name]:
                    getattr(block, attr)(run(self.streams[name]))


class Ring:
    def __init__(self, items):
        self.items = items
        self.i = 0

    def next(self):
        it = self.items[self.i % len(self.items)]
        self.i += 1
        return it


class Ops:
    def __init__(self, S):
        self.S = S

    def mm(self, out, lhsT, rhs, start, stop, reads, writes):
        self.S.op("pe", lambda e: e.matmul(out, lhsT=lhsT, rhs=rhs, start=start, stop=stop),
                  reads, writes)

    def tr(self, out, in_, ident, reads, writes):
        self.S.op("pe", lambda e: e.transpose(out, in_, ident), reads, writes)

    def act(self, out, in_, func, reads, writes, bias=None, scale=None, accum=None):
        kw = {}
        if bias is not None:
            kw["bias"] = bias
        if scale is not None:
            kw["scale"] = scale
        if accum is not None:
            kw["accum_out"] = accum
        self.S.op("act", lambda e: e.activation(out=out, in_=in_, func=func, **kw), reads, writes)

    def ts(self, eng, out, in0, s1, s2, op0, op1, reads, writes):
        if op1 is None:
            self.S.op(eng, lambda e: e.tensor_scalar(out=out, in0=in0, scalar1=s1, scalar2=None, op0=op0),
                      reads, writes)
        else:
            self.S.op(eng, lambda e: e.tensor_scalar(out=out, in0=in0, scalar1=s1, scalar2=s2, op0=op0, op1=op1),
                      reads, writes)

    def tt(self, eng, out, in0, in1, op, reads, writes):
        self.S.op(eng, lambda e: e.tensor_tensor(out=out, in0=in0, in1=in1, op=op), reads, writes)

    def stt(self, out, in0, scalar, in1, op0, op1, reads, writes):
        self.S.op("dve", lambda e: e.scalar_tensor_tensor(out=out, in0=in0, scalar=scalar, in1=in1,
                                                          op0=op0, op1=op1), reads, writes)

    def copy(self, eng, out, in_, reads, writes):
        self.S.op(eng, lambda e: e.tensor_copy(out=out, in_=in_), reads, writes)

    def memset(self, eng, ap, val, writes):
        self.S.op(eng, lambda e: e.memset(ap, val), (), writes)

    def dma(self, q, out, in_, reads, writes, chan):
        self.S.op(q, lambda e: e.dma_start(out=out, in_=in_), reads, writes, chan=chan)

    def dman(self, q, pairs, reads, writes, chan):
        self.S.op(q, lambda e: [e.dma_start(out=o, in_=i) for (o, i) in pairs], reads, writes, chan=chan,
                  ndma=len(pairs))


def build_nc(NB, S, debug=False, run_b=True, taps=()):
    assert S % 512 == 0
    NCH = S // 512
    NBLK = S // 128
    NTOK = NB * S
    TB = min(2048, S)
    nc = bass.Bass("TRN2", target_bir_lowering=False)
    Buf.ALL = []

    def din(name, shape, dt=F32):
        return nc.dram_tensor(name, list(shape), dt, kind="ExternalInput").ap()

    x_d = din("x", [NTOK, D])
    cT_d = din("cT", [128, KC, NB])
    wada_d = din("w_ada", [D, 6 * D])
    badaT_d = din("b_adaT", [128, 48])
    n1w_d = din("n1wT", [128, KC])
    n2w_d = din("n2wT", [128, KC])
    win_d = din("w_in", [D, IN_COLS])
    bfg_d = din("bfg", [128, NH])
    convT_d = din("convT", [128, 4, 3])
    qkw_d = din("qkw", [128, 2])
    woc_d = din("w_oc", [512, D])
    woa_d = din("w_oa", [512, D])
    wo_d = din("w_o", [D, D])
    wr_d = din("w_r", [D, 36])
    br_d = din("b_r", [128, 36])
    wg_d = din("w_gate", [NE, D, FE])
    wu_d = din("w_up", [NE, D, FE])
    wd_d = din("w_down", [NE, FE, D])
    consts_d = din("consts", [128, 6, 128])
    jv_d = din("jv", [128, (2 * NTOK) // 512 + NE + 1])
    zeros_d = din("zeros_bf", [1024, D], BF16)
    out_d = nc.dram_tensor("out", [NTOK, D], F32, kind="ExternalOutput").ap()

    win_bf = nc.dram_tensor("win_bf", [D, IN_COLS], BF16).ap()
    woc_bf = nc.dram_tensor("woc_bf", [512, D], BF16).ap()
    woa_bf = nc.dram_tensor("woa_bf", [512, D], BF16).ap()
    wos_bf = nc.dram_tensor("wos_bf", [NB, D, D], BF16).ap()
    W1 = nc.dram_tensor("W1_bf", [NE * 128, 4096], BF16).ap()
    W2 = nc.dram_tensor("W2_bf", [NE * 128, 2048], BF16).ap()

    NST_ = (2 * NTOK) // 512 + NE
    Xs = nc.dram_tensor("Xs", [NST_ * 512, D], BF16).ap()
    dbg_outs = {}
    tap_state = {"S": None}

    def dtap(name, ap, bufs, shape, dt):
        if name not in taps or name in dbg_outs:
            return
        t = nc.dram_tensor("dbg_" + name, list(shape), dt, kind="ExternalOutput").ap()
        dbg_outs[name] = t
        tap_state["S"].op("sp", lambda e: e.dma_start(out=t, in_=ap), bufs, (), chan="dbg_" + name)

    with contextlib.ExitStack() as top:
        def sbt(ctx, name, shape, dt):
            return ctx.enter_context(nc.sbuf_tensor(name, list(shape), dt))

        constsF = sbt(top, "constsF", [128, 6, 128], F32)
        identF = constsF[:, 0, :]
        triF = constsF[:, 1, :]
        elastF = constsF[:, 2, :]
        onesF = constsF[:, 4, :]
        identB = sbt(top, "identB", [128, 128], BF16)
        triB = sbt(top, "triB", [128, 128], BF16)
        bonesB = sbt(top, "bonesB", [128, 128], BF16)
        diagW = sbt(top, "diagW", [128, 12, 128], BF16)
        modT = sbt(top, "modT", [128, 48, NB], F32)
        A1 = sbt(top, "A1", [128, NB, KC], F32)
        B1 = sbt(top, "B1", [128, NB, KC], F32)
        A2 = sbt(top, "A2", [128, NB, KC], F32)
        B2 = sbt(top, "B2", [128, NB, KC], F32)
        qkws = sbt(top, "qkws", [128, 2], F32)
        bfg = sbt(top, "bfg_sb", [128, NH], F32)
        wfB = sbt(top, "wfB", [128, KC, NH], BF16)
        wrB = sbt(top, "wrB", [128, KC, 36], BF16)
        brt = sbt(top, "brt", [128, 36], F32)
        psum = [top.enter_context(nc.psum_tensor("ps%d" % i, [128, 512], F32)) for i in range(8)]
        pbuf = [Buf("ps%d" % i) for i in range(8)]
        bconst = Buf("consts")
        bmoe_w = [Buf("moew%d" % e) for e in range(NE)]
        b_winbf = Buf("winbf")
        b_wos = Buf("wosbf")
        b_outd = [Buf("out%d" % i) for i in range(NTOK // 128)]

        with contextlib.ExitStack() as ph:
            S_ = Sched(nc, "P")
            tap_state["S"] = S_
            O = Ops(S_)
            cT = sbt(ph, "cT_sb", [128, KC, NB], F32)
            sc = sbt(ph, "sc_sb", [128, KC, NB], F32)
            th = sbt(ph, "th_sb", [128, KC, NB], F32)
            badaT = sbt(ph, "badaT_sb", [128, 48], F32)
            n1w = sbt(ph, "n1w_sb", [128, KC], F32)
            n2w = sbt(ph, "n2w_sb", [128, KC], F32)
            convT = sbt(ph, "convT_sb", [128, 4, 3], F32)
            qkw = sbt(ph, "qkw_sb", [128, 2], F32)
            wfF = sbt(ph, "wfF", [128, KC, NH], F32)
            wrF = sbt(ph, "wrF", [128, KC, 36], F32)
            wa = [sbt(ph, "wa%d" % i, [128, KC, 768], F32) for i in range(2)]
            bwa = [Buf("wa%d" % i) for i in range(2)]
            woF = sbt(ph, "woF", [128, KC, D], F32)
            g1bc = sbt(ph, "g1bc", [128, NB, D], F32)
            diag = [sbt(ph, "diag%d" % i, [128, 128], F32) for i in range(2)]
            bdiag = [Buf("diag%d" % i) for i in range(2)]
            wtmp = [sbt(ph, "wtmp%d" % i, [128, D], BF16) for i in range(2)]
            bwtmp = [Buf("wtmp%d" % i) for i in range(2)]
            b_small = Buf("small_in")
            b_sc = Buf("sc")
            b_woF = Buf("woF")
            b_g1 = Buf("g1bc")
            b_modps = pbuf[0]

            ci = 0
            for r in range(8):
                O.dma("pool", win_bf[r * 128:(r + 1) * 128, :], win_d[r * 128:(r + 1) * 128, :], (), [b_winbf],
                      "cast%d" % (ci % 8)); ci += 1
            for r in range(4):
                O.dma("pool", woc_bf[r * 128:(r + 1) * 128, :], woc_d[r * 128:(r + 1) * 128, :], (), [b_winbf],
                      "cast%d" % (ci % 8)); ci += 1
                O.dma("pool", woa_bf[r * 128:(r + 1) * 128, :], woa_d[r * 128:(r + 1) * 128, :], (), [b_winbf],
                      "cast%d" % (ci % 8)); ci += 1

            nsm = [0]
            for dst, src in ((constsF[:], consts_d[:, :, :]), (cT[:], cT_d[:, :, :]), (badaT[:], badaT_d[:, :]),
                             (n1w[:], n1w_d[:, :]), (n2w[:], n2w_d[:, :]), (convT[:], convT_d[:, :, :]),
                             (qkw[:], qkw_d[:, :]), (bfg[:], bfg_d[:, :]), (brt[:], br_d[:, :])):
                O.dma("sp", dst, src, (), [b_small], "small%d" % nsm[0]); nsm[0] += 1
            O.dma("sp", wfF[:], win_d.rearrange("(kc p) n -> p kc n", p=128)[:, :, 3072:3080], (), [b_small], "smallA")
            O.dma("sp", wrF[:], wr_d.rearrange("(kc p) n -> p kc n", p=128), (), [b_small], "smallB")
            O.dma("sp", woF[:], wo_d.rearrange("(kc p) n -> p kc n", p=128), (), [b_woF], "woF")

            O.act(th[:], cT[:], AF.Tanh, [b_small], [b_sc], scale=0.5)
            O.stt(sc[:], th[:], 1.0, cT[:], ALU.add, ALU.mult, [b_small, b_sc], [b_sc])
            O.ts("dve", sc[:], sc[:], 0.5, None, ALU.mult, None, [b_sc], [b_sc])
            O.copy("dve", identB[:], identF, [b_small], [bconst])
            O.copy("dve", triB[:], triF, [b_small], [bconst])
            O.copy("dve", bonesB[:], constsF[:, 3, :], [b_small], [bconst])
            O.copy("dve", wfB[:], wfF[:], [b_small], [bconst])
            O.copy("dve", wrB[:], wrF[:], [b_small], [bconst])
            for j in range(4):
                for tap in range(3):
                    O.ts("dve", diagW[:, j * 3 + tap, :], identF, convT[:, j, tap:tap + 1], None, ALU.mult, None,
                         [b_small], [bconst])
            O.ts("dve", qkws[:, 0:1], qkw[:, 0:1], HD ** -0.5, None, ALU.mult, None, [b_small], [bconst])
            O.copy("dve", qkws[:, 1:2], qkw[:, 1:2], [b_small], [bconst])

            wada_v = wada_d.rearrange("(kc p) n -> p kc n", p=128)
            modps = psum[0]
            for g in range(8):
                w_, bw_ = wa[g % 2], bwa[g % 2]
                O.dma("sp", w_[:], wada_v[:, :, g * 768:(g + 1) * 768], (), [bw_], "wa%d" % (g % 2))
                for jj in range(6):
                    col = g * 6 + jj
                    for kc in range(KC):
                        O.mm(modps[:, col * NB:(col + 1) * NB], w_[:, kc, jj * 128:(jj + 1) * 128], sc[:, kc, :],
                             kc == 0, kc == KC - 1, [bw_, b_sc], [b_modps])
            O.tt("dve", modT[:], modps[:, 0:48 * NB].rearrange("p (a b) -> p a b", b=NB),
                 badaT[:, :, None].broadcast_to([128, 48, NB]), ALU.add, [b_modps, b_small], [bconst])
            for b in range(NB):
                O.stt(A1[:, b, :], modT[:, 8:16, b], 1.0, n1w[:], ALU.add, ALU.mult, [bconst, b_small], [bconst])
                O.copy("dve", B1[:, b, :], modT[:, 0:8, b], [bconst], [bconst])
                O.stt(A2[:, b, :], modT[:, 32:40, b], 1.0, n2w[:], ALU.add, ALU.mult, [bconst, b_small], [bconst])
                O.copy("dve", B2[:, b, :], modT[:, 24:32, b], [bconst], [bconst])
            k = 0
            for b in range(NB):
                for half in range(2):
                    pb_, bb_ = psum[1 + half], pbuf[1 + half]
                    for q in range(4):
                        kc = half * 4 + q
                        dg, bdg = diag[k % 2], bdiag[k % 2]
                        k += 1
                        O.ts("dve", dg[:], identF, modT[:, 16 + kc, b:b + 1], 0.5, ALU.mult, ALU.mult,
                             [bconst, b_small], [bdg])
                        O.mm(pb_[:, q * 128:(q + 1) * 128], onesF, dg[:], True, True, [b_small, bdg], [bb_])
                    O.copy("dve", g1bc[:, b, half * 512:(half + 1) * 512], pb_[:, :], [bb_], [b_g1])
            k = 0
            for b in range(NB):
                for kc in range(KC):
                    wt, bwt = wtmp[k % 2], bwtmp[k % 2]
                    k += 1
                    O.tt("pool", wt[:], woF[:, kc, :], g1bc[:, b, :], ALU.mult, [b_woF, b_g1], [bwt])
                    O.dma("sp", wos_bf[b, kc * 128:(kc + 1) * 128, :], wt[:], [bwt], [b_wos], "wos%d" % (k % 2))
            dtap("modT", modT[:], [bconst], [128, 48, NB], F32)
            dtap("A1", A1[:], [bconst], [128, NB, KC], F32)
            dtap("g1bc", g1bc[:], [b_g1], [128, NB, D], F32)
            S_.finish()

        with contextlib.ExitStack() as ph:
            S_ = Sched(nc, "A")
            tap_state["S"] = S_
            O = Ops(S_)
            Kc = sbt(ph, "Kc", [128, 4, S], BF16)
            Vc = sbt(ph, "Vc", [128, NBLK, NH, HD + 1], BF16)
            Gall = sbt(ph, "Gall", [128, NBLK, NH], F32)
            nbias = sbt(ph, "nbias", [128, NBLK, NH], F32)
            bKc = [Buf("Kc%d" % i) for i in range(NCH)]
            bVc = [Buf("Vc%d" % i) for i in range(NCH)]
            bVones = Buf("Vones")
            bGall = [Buf("Gall%d" % i) for i in range(NBLK)]
            bnbias = Buf("nbias")
            NSLOT = 4
            wslot = [sbt(ph, "wslot%d" % i, [128, 4096], BF16) for i in range(NSLOT)]
            bslot = [Buf("wslot%d" % i) for i in range(NSLOT)]
            slots = Ring(list(range(NSLOT)))
            xr = [sbt(ph, "xr%d" % i, [128, D], F32) for i in range(2)]
            xring = Ring([(xr[i], Buf("xr%d" % i), "xr%d" % i) for i in range(2)])
            onesB = sbt(ph, "onesB", [128, 64], BF16)
            rlb = [sbt(ph, "rlb%d" % i, [128, 512], BF16) for i in range(2)]
            brlb = [Buf("rlb%d" % i) for i in range(2)]
            x1ts = [sbt(ph, "x1t%d" % i, [128, D], F32) for i in range(2)]
            x1ring = Ring([(x1ts[i], Buf("x1t%d" % i), "x1st%d" % i) for i in range(2)])
            xn = [sbt(ph, "xn%d" % i, [128, D], BF16) for i in range(4)]
            bxn = [Buf("xn%d" % i) for i in range(4)]
            ss = sbt(ph, "ss", [128, 4], F32); bss = Buf("ss")
            lnv = sbt(ph, "lnv", [128, 4], F32)
            rstd = sbt(ph, "rstd", [128, 4], F32); brstd = Buf("rstd")
            hTs = [(sbt(ph, "hT%d" % i, [128, KC, 512], BF16), Buf("hT%d" % i)) for i in range(2)]
            xin_sb = sbt(ph, "xin_sb", [128, 4, 512], BF16); bxin = Buf("xin")
            b_sb = sbt(ph, "b_sb", [128, 4, 512], BF16); bbsb = Buf("bsb")
            ub = [sbt(ph, "ub%d" % j, [128, 514], BF16) for j in range(4)]
            bub = [Buf("ub%d" % j) for j in range(4)]
            yaT = sbt(ph, "yaT", [128, 4, 512], BF16); byaT = Buf("yaT")
            sq = sbt(ph, "sq", [128, 512], BF16); bsq = Buf("sq")
            rsb = [sbt(ph, "rsb%d" % i, [128, 512], F32) for i in range(1)]
            brsb = [Buf("rsb%d" % i) for i in range(1)]
            rsring = Ring(list(range(1)))
            qT = sbt(ph, "qTz", [128, NH, 512], BF16); bqT = Buf("qT")
            PT = [sbt(ph, "PT%d" % i, [128, 512], BF16) for i in range(4)]
            PTring = Ring([(PT[i], Buf("PT%d" % i)) for i in range(4)])
            zf = sbt(ph, "zf", [128, 32], F32); bzf = Buf("zf")
            spf = sbt(ph, "spf", [128, 32], F32); bspf = Buf("spf")
            Gmid = sbt(ph, "Gmid", [128, NH], F32); bGmid = Buf("Gmid")
            rl = [sbt(ph, "rl%d" % i, [128, 512], F32) for i in range(1)]
            brl = [Buf("rl0"), Buf("rl0b")]
            rl = [rl[0], rl[0]]
            brl = [brl[0], brl[0]]
            rlring = Ring(list(range(2)))
            bcs = sbt(ph, "bcs", [64, 512], F32); bbcs = Buf("bcs")
            ybT = sbt(ph, "ybT", [128, 4, 512], BF16); bybT = Buf("ybT")
            thr = [sbt(ph, "thr%d" % i, [128, 512], BF16) for i in range(2)]
            thring = Ring([(thr[i], Buf("thr%d" % i)) for i in range(2)])
            t12 = [sbt(ph, "t12_%d" % i, [128, 512], BF16) for i in range(2)]
            t12ring = Ring([(t12[i], Buf("t12_%d" % i)) for i in range(2)])
            mT = sbt(ph, "mT", [128, KC, 512], BF16); bmT = Buf("mT")

            Gring = Ring([(psum[i], pbuf[i]) for i in range(6)])
            Bring = Ring([(psum[i], pbuf[i]) for i in range(2)])
            Spairs = [[(psum[3], pbuf[3]), (psum[4], pbuf[4])], [(psum[5], pbuf[5]), (psum[2], pbuf[2])]]
            Oring = Ring([(psum[i], pbuf[i]) for i in (6, 7)])
            win_v = win_bf.rearrange("(kc p) n -> p kc n", p=128)
            woc_v = woc_bf.rearrange("(kc p) n -> p kc n", p=128)
            woa_v = woa_bf.rearrange("(kc p) n -> p kc n", p=128)

            def v8(t):
                return t[:, :].rearrange("p (k n) -> p k n", k=8)

            def v4(t):
                return t[:, :].rearrange("p (k n) -> p k n", k=4)

            def load_seg(c0, ncol=512):
                si = slots.next()
                O.dma("sp", v8(wslot[si])[:, :, 0:ncol], win_v[:, :, c0:c0 + ncol], [b_winbf], [bslot[si]],
                      "wsl%d" % si)
                return si

            moe_casts = []
            for e in range(NE):
                rows = slice(e * 128, (e + 1) * 128)
                moe_casts.append((W1[rows, 0:2048].rearrange("p (k n) -> p k n", k=KC),
                                  wg_d[e].rearrange("(kc p) n -> p kc n", p=128), e))
                moe_casts.append((W1[rows, 2048:4096].rearrange("p (k n) -> p k n", k=KC),
                                  wu_d[e].rearrange("(kc p) n -> p kc n", p=128), e))
                moe_casts.append((W2[rows, :].rearrange("p (f n) -> p f n", f=2),
                                  wd_d[e].rearrange("(f p) n -> p f n", p=128), e))
            n_total_chunks = NB * NCH
            per_chunk = -(-len(moe_casts) // n_total_chunks)
            cast_i = [0]

            def issue_casts(n):
                for _ in range(n):
                    if cast_i[0] >= len(moe_casts):
                        return
                    dst, src, e = moe_casts[cast_i[0]]
                    O.dma("pool", dst, src, (), [bmoe_w[e]], "mcast%d" % (cast_i[0] % 8))
                    cast_i[0] += 1

            O.memset("pool", Vc[:, :, :, HD:HD + 1], 1.0, [bVones])
            O.memset("pool", onesB[:], 1.0, [bconst])
            O.memset("pool", qT[:], 0.0, [bqT])

            chunks = [(b, c) for b in range(NB) for c in range(NCH)]

            norm_x = {}

            def stage_norm_dma(g, i):
                b, c = chunks[g]
                tok0 = b * S + c * 512
                xt, bxt, chn = xring.next()
                O.dma("sp", xt[:], x_d[tok0 + i * 128: tok0 + (i + 1) * 128, :], (), [bxt], chn)
                norm_x[(g, i)] = (xt, bxt)

            def stage_norm_compute(g, i):
                xt, bxt = norm_x.pop((g, i))
                O.act(xn[i][:], xt[:], AF.Square, [bxt], [bxn[i], bss], accum=ss[:, i:i + 1])
                O.act(lnv[:, i:i + 1], ss[:, i:i + 1], AF.Ln, [bss], [brstd], bias=EPS, scale=1.0 / D)
                O.act(rstd[:, i:i + 1], lnv[:, i:i + 1], AF.Exp, [brstd], [brstd], scale=-0.5)
                O.ts("pool", xn[i][:], xt[:], rstd[:, i:i + 1], None, ALU.mult, None, [bxt, brstd], [bxn[i]])

            def stage_norm(g):
                for i in range(4):
                    stage_norm_dma(g, i)
                    stage_norm_compute(g, i)

            def stage_tr(g):
                b, c = chunks[g]
                hT, bhT = hTs[g % 2]
                for kc in range(KC):
                    tp, btp = Gring.next()
                    tpb = tp[:, :].bitcast(BF16)
                    for i in range(4):
                        O.tr(tpb[:, i * 128:(i + 1) * 128], xn[i][:, kc * 128:(kc + 1) * 128], identB[:],
                             [bxn[i], bconst], [btp])
                    O.ts("dve", hT[:, kc, :], tpb[:, 0:512], A1[:, b, kc:kc + 1], B1[:, b, kc:kc + 1],
                         ALU.mult, ALU.add, [btp, bconst], [bhT])

            def stage_proj(g):
                b, c = chunks[g]
                hT, bhT = hTs[g % 2]
                pf, bpf = Gring.next()
                for i in range(4):
                    for kc in range(KC):
                        O.mm(pf[:, i * NH:(i + 1) * NH], hT[:, kc, i * 128:(i + 1) * 128], wfB[:, kc, :],
                             kc == 0, kc == KC - 1, [bconst, bhT], [bpf])
                O.tt("dve", zf[:, :].rearrange("p (i h) -> p i h", h=NH),
                     pf[:, 0:32].rearrange("p (i h) -> p i h", h=NH),
                     bfg[:, None, :].broadcast_to([128, 4, NH]), ALU.add, [bpf, bconst], [bzf])
                O.act(spf[:], zf[:], AF.Exp, [bzf], [bspf], scale=-1.0)
                O.act(spf[:], spf[:], AF.Ln, [bspf], [bspf], bias=1.0)
                for which in range(2):
                    si = load_seg((3 + which) * 512)
                    w8 = v8(wslot[si])
                    for hp in range(4):
                        pt, bpt = Gring.next()
                        for kc in range(KC):
                            O.mm(pt[:, :], w8[:, kc, hp * 128:(hp + 1) * 128], hT[:, kc, :], kc == 0,
                                 kc == KC - 1, [bslot[si], bhT], [bpt])
                        O.act(sq[:], pt[:, :], AF.Square, [bpt], [bsq])
                        pm, bpm = Gring.next()
                        O.mm(pm[:, :], bonesB[:], sq[:], True, True, [bconst, bsq], [bpm])
                        ri = rsring.next()
                        O.act(rsb[ri][:], pm[:, :], AF.Ln, [bpm], [brsb[ri]], bias=EPS)
                        O.act(rsb[ri][:], rsb[ri][:], AF.Exp, [brsb[ri]], [brsb[ri]], scale=-0.5)
                        if which == 0:
                            O.stt(qT[0:64, 2 * hp, :], pt[0:64, :], qkws[0:64, 0:1], rsb[ri][0:64, :], ALU.mult, ALU.mult,
                                  [bpt, bconst, brsb[ri]], [bqT])
                            O.stt(qT[64:128, 2 * hp + 1, :], pt[64:128, :], qkws[64:128, 0:1], rsb[ri][64:128, :],
                                  ALU.mult, ALU.mult, [bpt, bconst, brsb[ri]], [bqT])
                        else:
                            O.stt(Kc[:, hp, c * 512:(c + 1) * 512], pt[:, :], qkws[:, 1:2], rsb[ri][:],
                                  ALU.mult, ALU.mult, [bpt, bconst, brsb[ri]], [bKc[c]])
                si = load_seg(5 * 512)
                w8 = v8(wslot[si])
                for i in range(4):
                    pt, bpt = Gring.next()
                    for kc in range(KC):
                        O.mm(pt[:, :], hT[:, kc, i * 128:(i + 1) * 128], w8[:, kc, :], kc == 0, kc == KC - 1,
                             [bslot[si], bhT], [bpt])
                    O.copy("dve", Vc[:, c * 4 + i, :, 0:HD], pt[:, :].rearrange("p (h d) -> p h d", h=NH),
                           [bpt], [bVc[c]])

                for i in range(4):
                    blk = c * 4 + i
                    pg, bpg = Gring.next()
                    if blk == 0:
                        O.mm(pg[:, 0:NH], triF, spf[:, i * NH:(i + 1) * NH], True, True, [bconst, bspf], [bpg])
                    else:
                        O.mm(pg[:, 0:NH], triF, spf[:, i * NH:(i + 1) * NH], True, False, [bconst, bspf], [bpg])
                        O.mm(pg[:, 0:NH], elastF, Gall[:, blk - 1, :], False, True, [bconst, bGall[blk - 1]], [bpg])
                    O.copy("dve", Gall[:, blk, :], pg[:, 0:NH], [bpg], [bGall[blk]])
                pg, bpg = Gring.next()
                O.mm(pg[:, 0:NH], elastF, Gall[:, c * 4 + 1, :], True, True, [bconst, bGall[c * 4 + 1]], [bpg])
                O.copy("dve", Gmid[:], pg[:, 0:NH], [bpg], [bGmid])
                nkb = 4 * c + 4
                O.tt("dve", nbias[:, 0:nkb, :], Gall[:, 0:nkb, :], Gmid[:, None, :].broadcast_to([128, nkb, NH]),
                     ALU.subtract, [bGmid] + bGall[0:nkb], [bnbias])

            def conv_units(g, ring):
                b, c = chunks[g]
                hT, bhT = hTs[g % 2]
                units = []
                segslot = {}

                def seg_unit(seg, j, dst, bdst):
                    def fn():
                        if j == 0:
                            segslot[seg] = load_seg(seg * 512)
                        si = segslot[seg]
                        w8 = v8(wslot[si])
                        pt, bpt = ring.next()
                        for kc in range(KC):
                            O.mm(pt[:, :], w8[:, kc, j * 128:(j + 1) * 128], hT[:, kc, :], kc == 0, kc == KC - 1,
                                 [bslot[si], bhT], [bpt])
                            if kc == 3:
                                yield
                        O.copy("dve", dst[:, j, :], pt[:, :], [bpt], [bdst])
                        yield
                    return fn

                def conv_unit(j):
                    def fn():
                        if j == 0:
                            segslot[2] = load_seg(2 * 512)
                        si = segslot[2]
                        w8 = v8(wslot[si])
                        pt, bpt = ring.next()
                        for kc in range(KC):
                            O.mm(pt[:, :], w8[:, kc, j * 128:(j + 1) * 128], hT[:, kc, :], kc == 0, kc == KC - 1,
                                 [bslot[si], bhT], [bpt])
                            if kc == 3:
                                yield
                        if c == 0:
                            O.memset("dve", ub[j][:, 0:2], 0.0, [bub[j]])
                        else:
                            O.copy("dve", ub[j][:, 0:2], ub[j][:, 512:514], [bub[j]], [bub[j]])
                        O.tt("dve", ub[j][:, 2:514], pt[:, :], xin_sb[:, j, :], ALU.mult, [bpt, bxin], [bub[j]])
                        yield
                        pc, bpc = ring.next()
                        for tap in range(3):
                            O.mm(pc[:, :], diagW[:, j * 3 + tap, :], ub[j][:, tap:tap + 512], tap == 0, tap == 2,
                                 [bconst, bub[j]], [bpc])
                        O.tt("dve", yaT[:, j, :], pc[:, :], b_sb[:, j, :], ALU.mult, [bpc, bbsb], [byaT])
                        yield
                    return fn
                for j in range(4):
                    units.append(seg_unit(0, j, xin_sb, bxin))
                for j in range(4):
                    units.append(seg_unit(1, j, b_sb, bbsb))
                for j in range(4):
                    units.append(conv_unit(j))

                def steps():
                    for u in units:
                        for _ in u():
                            yield
                return steps()

            def stage_attn(g):
                b, c = chunks[g]
                nkb = 4 * c + 4

                def finalize(h, Ob, bOb):
                    hp, jj = divmod(h, 2)
                    lo = jj * 64
                    ri = rlring.next()
                    O.act(rl[ri][64:65, :], Ob[64:65, :], AF.Ln, [bOb], [brl[ri]])
                    O.act(rlb[ri][64:65, :], rl[ri][64:65, :], AF.Exp, [brl[ri]], [brlb[ri]], scale=-1.0)
                    pbc, bpbc = Bring.next()
                    O.mm(pbc[0:64, :], onesB[64:65, 0:64], rlb[ri][64:65, :], True, True, [bconst, brlb[ri]], [bpbc])
                    O.copy("dve", bcs[:, :], pbc[0:64, :], [bpbc], [bbcs])
                    O.tt("dve", ybT[lo:lo + 64, hp, :], Ob[0:64, :], bcs[:, :], ALU.mult, [bOb, bbcs], [bybT])

                prev_fin = None
                npairs = nkb // 2
                cunits = conv_units(g, Bring)
                items = [(h, p) for h in range(NH) for p in range(npairs)]
                spp = -(-28 // len(items))
                Obs = {}

                def qkpair(idx):
                    h, p = items[idx]
                    hp = h // 2
                    res = []
                    for t in range(2):
                        kb = 2 * p + t
                        d = kb - 4 * c
                        q0 = max(d, 0) * 128
                        Sb, bSb = Spairs[idx % 2][t]
                        O.mm(Sb[:, 0:512 - q0], Kc[:, hp, kb * 128:(kb + 1) * 128],
                             qT[:, h, q0:512], True, True, [bKc[kb // 4], bqT], [bSb])
                        res.append((Sb, bSb, q0, d, kb))
                    return res

                pend = [qkpair(i) for i in range(min(2, len(items)))]
                for idx, (h, p) in enumerate(items):
                    if p == 0:
                        Obs[h] = Oring.next()
                    Ob, bOb = Obs[h]
                    cur = pend.pop(0)
                    pts = []
                    for (Sb, bSb, q0, d, kb) in cur:
                        N = 512 - q0
                        Pt, bPt = PTring.next()
                        O.act(Pt[:, 0:N], Sb[:, 0:N], AF.Exp, [bSb, bnbias], [bPt], bias=nbias[:, kb, h:h + 1])
                        if d >= 0:
                            O.tt("pool", Pt[:, 0:128], Pt[:, 0:128], triB[:], ALU.mult, [bPt, bconst], [bPt])
                        pts.append((Pt, bPt, q0, N, kb))
                    allp = [x[1] for x in pts]
                    for (Pt, bPt, q0, N, kb) in pts:
                        O.mm(Ob[0:HD + 1, q0:512], Vc[:, kb, h, :], Pt[:, 0:N], kb == 0, kb == nkb - 1,
                             [bVc[kb // 4], bVones] + allp, [bOb])
                    if idx + 2 < len(items):
                        pend.append(qkpair(idx + 2))
                    if p == 0 and prev_fin is not None:
                        finalize(*prev_fin)
                        prev_fin = None
                    for _ in range(spp):
                        next(cunits, None)
                    if p == npairs - 1:
                        prev_fin = (h, Ob, bOb)
                        if g + 1 < len(chunks):
                            if 1 <= h <= 4:
                                stage_norm_compute(g + 1, h - 1)
                            if h <= 3:
                                stage_norm_dma(g + 1, h)
                        if h >= 1 and h <= 6:
                            issue_casts(1)
                finalize(*prev_fin)
                for _ in cunits:
                    pass

            def stage_post(g):
                b, c = chunks[g]
                tok0 = b * S + c * 512
                hT, bhT = hTs[g % 2]
                s_oc = slots.next()
                O.dma("sp", v4(wslot[s_oc]), woc_v, [b_winbf], [bslot[s_oc]], "wsl%d" % s_oc)
                s_oa = slots.next()
                O.dma("sp", v4(wslot[s_oa]), woa_v, [b_winbf], [bslot[s_oa]], "wsl%d" % s_oa)
                woc4, woa4 = v4(wslot[s_oc]), v4(wslot[s_oa])
                for jp in range(4):
                    sg = slots.next()
                    while sg in (s_oc, s_oa):
                        sg = slots.next()
                    g8 = v8(wslot[sg])
                    O.dman("sp", [(g8[:, :, 0:256], win_v[:, :, 3080 + jp * 256: 3080 + (jp + 1) * 256]),
                                  (g8[:, :, 256:512], win_v[:, :, 4104 + jp * 256: 4104 + (jp + 1) * 256])],
                           [b_winbf], [bslot[sg]], "wsl%d" % sg)
                    for j2 in range(2):
                        jf = jp * 2 + j2
                        pgc, bpgc = Gring.next()
                        for kc in range(KC):
                            O.mm(pgc[:, :], g8[:, kc, j2 * 128:(j2 + 1) * 128], hT[:, kc, :], kc == 0,
                                 kc == KC - 1, [bslot[sg], bhT], [bpgc])
                        thc, bthc = thring.next()
                        O.act(thc[:], pgc[:, :], AF.Tanh, [bpgc], [bthc], scale=0.5)
                        pa, bpa = Gring.next()
                        for kc in range(4):
                            O.mm(pa[:, :], woc4[:, kc, jf * 128:(jf + 1) * 128], yaT[:, kc, :], kc == 0, kc == 3,
                                 [bslot[s_oc], byaT], [bpa])
                        pga, bpga = Gring.next()
                        for kc in range(KC):
                            O.mm(pga[:, :], g8[:, kc, 256 + j2 * 128: 256 + (j2 + 1) * 128], hT[:, kc, :], kc == 0,
                                 kc == KC - 1, [bslot[sg], bhT], [bpga])
                        tha, btha = thring.next()
                        O.act(tha[:], pga[:, :], AF.Tanh, [bpga], [btha], scale=0.5)
                        t1, bt1 = t12ring.next()
                        O.stt(t1[:], thc[:], 1.0, pa[:, :], ALU.add, ALU.mult, [bthc, bpa], [bt1])
                        pb_, bpb_ = Gring.next()
                        for kc in range(4):
                            O.mm(pb_[:, :], woa4[:, kc, jf * 128:(jf + 1) * 128], ybT[:, kc, :], kc == 0, kc == 3,
                                 [bslot[s_oa], bybT], [bpb_])
                        t2, bt2 = t12ring.next()
                        O.stt(t2[:], tha[:], 1.0, pb_[:, :], ALU.add, ALU.mult, [btha, bpb_], [bt2])
                        O.tt("pool", mT[:, jf, :], t1[:], t2[:], ALU.add, [bt1, bt2], [bmT])
                if b == 0 and c == 0:
                    dtap("hT", hT[:], [bhT], [128, KC, 512], BF16)
                    dtap("yaT", yaT[:], [byaT], [128, 4, 512], BF16)
                    dtap("qT", qT[:], [bqT], [128, NH, 512], BF16)
                    dtap("Kc", Kc[:, :, 0:512], [bKc[0]], [128, 4, 512], BF16)
                    dtap("Vc", Vc[:, 0:4, :, :], [bVc[0], bVones], [128, 4, NH, HD + 1], BF16)
                    dtap("Gall", Gall[:, 0:4, :], bGall[0:4], [128, 4, NH], F32)
                    dtap("nbias", nbias[:, 0:4, :], [bnbias], [128, 4, NH], F32)
                    dtap("ybT", ybT[:], [bybT], [128, 4, 512], BF16)
                    dtap("mT", mT[:], [bmT], [128, KC, 512], BF16)
                s_o = []
                for n in range(2):
                    so = slots.next()
                    O.dma("sp", v8(wslot[so]), wos_bf[b].rearrange("(kc p) n -> p kc n", p=128)[:, :, n * 512:(n + 1) * 512],
                          [b_wos], [bslot[so]], "wsl%d" % so)
                    s_o.append(so)
                for i in range(4):
                    xt, bxt, chn = xring.next()
                    O.dma("sp", xt[:], x_d[tok0 + i * 128: tok0 + (i + 1) * 128, :], (), [bxt], chn)
                    x1t, bx1t, x1ch = x1ring.next()
                    for n in range(2):
                        po, bpo = Gring.next()
                        w8 = v8(wslot[s_o[n]])
                        for kc in range(KC):
                            O.mm(po[:, :], mT[:, kc, i * 128:(i + 1) * 128], w8[:, kc, :], kc == 0, kc == KC - 1,
                                 [bslot[s_o[n]], bmT], [bpo])
                        O.tt("dve", x1t[:, n * 512:(n + 1) * 512], po[:, :], xt[:, n * 512:(n + 1) * 512], ALU.add,
                             [bpo, bxt], [bx1t])
                    ti = (tok0 // 128) + i
                    O.dma("pool", out_d[ti * 128:(ti + 1) * 128, :], x1t[:], [bx1t], [b_outd[ti]], x1ch)

            NG = len(chunks)
            nzf = (NST_ * 512) // 1024
            zf_i = [0]
            bXs0 = Buf("Xs_zero")

            def issue_zero(n):
                for _ in range(n):
                    if zf_i[0] >= nzf:
                        return
                    r0 = zf_i[0] * 1024
                    O.dma("act", Xs[r0:r0 + 1024, :], zeros_d[:, :], (), [bXs0], "zf%d" % (zf_i[0] % 4))
                    zf_i[0] += 1
            zper = -(-nzf // NG)
            stage_norm(0)
            stage_tr(0)
            for g in range(NG):
                issue_zero(zper)
                stage_proj(g)
                stage_attn(g)
                if g + 1 < NG:
                    stage_tr(g + 1)
                stage_post(g)
            issue_casts(len(moe_casts))
            issue_zero(nzf)
            S_.finish()

        if not run_b:
            return nc
        NTT = NTOK // 128
        NST = (2 * NTOK) // 512 + NE
        NSLOTS = NST * 512
        Ybuf = nc.dram_tensor("Ybuf", [NSLOTS, D], BF16).ap()
        I32 = mybir.dt.int32
        sloti = sbt(top, "sloti", [128, NTT * 2], I32)
        wk = sbt(top, "wk", [128, NTT * 2], F32)
        widx = sbt(top, "widx", [128, NST], I32)
        g2bc = sbt(top, "g2bc_t", [128, NB, D], F32)

        def bcast_rows(O, dst, src_fn, ring, bufs_in, bdst, diag, bdiag):
            k = 0
            for b in range(NB):
                for half in range(2):
                    pb_, bb_ = ring.next()
                    for q in range(4):
                        kc = half * 4 + q
                        dg, bdg = diag[k % 2], bdiag[k % 2]
                        k += 1
                        O.ts("dve", dg[:], identF, src_fn(b, kc), None, ALU.mult, None, bufs_in, [bdg])
                        O.mm(pb_[:, q * 128:(q + 1) * 128], onesF, dg[:], True, True, [bconst, bdg], [bb_])
                    O.copy("dve", dst[:, b, half * 512:(half + 1) * 512], pb_[:, :], [bb_], [bdst])

        with contextlib.ExitStack() as ph:
            S_ = Sched(nc, "B1")
            tap_state["S"] = S_
            O = Ops(S_)
            H2all = sbt(ph, "H2all", [128, NTT, D], BF16)
            bH2 = [Buf("H2_%d" % i) for i in range(NTT)]
            OH1 = sbt(ph, "OH1", [128, NTT, NE], F32)
            OH2 = sbt(ph, "OH2", [128, NTT, NE], F32)
            bOH = [Buf("OH%d" % i) for i in range(NTT)]
            A2row = sbt(ph, "A2row", [128, NB, D], F32)
            B2row = sbt(ph, "B2row", [128, NB, D], F32)
            bA2r = Buf("A2row")
            bg2 = Buf("g2bc")
            diag = [sbt(ph, "bdiag%d" % i, [128, 128], F32) for i in range(2)]
            bdiag = [Buf("bdiag%d" % i) for i in range(2)]
            xr = [sbt(ph, "bxr%d" % i, [128, D], F32) for i in range(2)]
            xring = Ring([(xr[i], Buf("bxr%d" % i), "bxr%d" % i) for i in range(2)])
            tmpf = [sbt(ph, "tmpf%d" % i, [128, D], F32) for i in range(1)]
            tmpring = Ring([(tmpf[i], Buf("tmpf%d" % i)) for i in range(1)])
            Ar = [sbt(ph, "Ar%d" % i, [128, NE], F32) for i in range(2)]
            Aring = Ring([(Ar[i], Buf("Ar%d" % i)) for i in range(2)])
            ss = sbt(ph, "bss", [128, 4], F32); bss = Buf("bss")
            lnv = sbt(ph, "blnv", [128, 4], F32)
            rstd = sbt(ph, "brstd", [128, 4], F32); brstd = Buf("brstd")
            h2T = sbt(ph, "h2T", [128, KC, 512], BF16); bh2T = Buf("h2T")
            rb = {n: sbt(ph, "rb_" + n, [128, w], F32) for n, w in
                  (("lg", 144), ("gmax", 4), ("ohg", 16), ("sh", 16), ("ex", 16), ("sume", 4), ("psel", 4), ("pen", 16),
                   ("lem", 128), ("m8", 32), ("d21", 4), ("e2", 4), ("den", 4), ("rden", 4))}
            brt_ = Buf("rt")
            cntf = sbt(ph, "cntf", [128, NE], F32)
            padi = sbt(ph, "padi", [128, NE], I32)
            padf = sbt(ph, "padf", [128, NE], F32)
            base = sbt(ph, "base", [128, NE], F32)
            jv = sbt(ph, "jv_sb", [128, NST + 1], F32)
            texpf = sbt(ph, "texpf", [128, NST], F32)
            Rsum = sbt(ph, "Rsum", [128, NE], F32)
            Tt = sbt(ph, "Tt", [128, NE], F32)
            Tm = sbt(ph, "Tm", [128, NE], F32)
            slotf = sbt(ph, "slotf", [128, NTT * 2], F32)
            bcnt = Buf("cnt"); bRsum = Buf("Rsum"); bT = Buf("T"); bslot = Buf("slot")
            Dring = Ring([(psum[i], pbuf[i]) for i in range(8)])
            striF = constsF[:, 5, :]
            striB = sbt(ph, "striB", [128, 128], BF16)
            onesBb = sbt(ph, "onesBb", [128, 128], BF16)
            bstri = Buf("striB")

            bcast_rows(O, g2bc, lambda b, kc: modT[:, 40 + kc, b:b + 1], Dring, [bconst], bg2, diag, bdiag)
            bcast_rows(O, A2row, lambda b, kc: A2[:, b, kc:kc + 1], Dring, [bconst], bA2r, diag, bdiag)
            bcast_rows(O, B2row, lambda b, kc: B2[:, b, kc:kc + 1], Dring, [bconst], bA2r, diag, bdiag)
            O.dma("sp", jv[:], jv_d[:, :], (), [bcnt], "jv")

            TRring = Ring([(psum[i], pbuf[i]) for i in range(6)])
            PRring = Ring([(psum[i], pbuf[i]) for i in (6, 7)])

            def b1_norm(scn):
                    b = (scn * 512) // S
                    for i in range(4):
                        ti = scn * 4 + i
                        xt, bxt, chn = xring.next()
                        O.dma("sp", xt[:], out_d[ti * 128:(ti + 1) * 128, :], [b_outd[ti]], [bxt], chn)
                        O.act(H2all[:, ti, :], xt[:], AF.Square, [bxt], [bH2[ti], bss], accum=ss[:, i:i + 1])
                        O.act(lnv[:, i:i + 1], ss[:, i:i + 1], AF.Ln, [bss], [brstd], bias=EPS, scale=1.0 / D)
                        O.act(rstd[:, i:i + 1], lnv[:, i:i + 1], AF.Exp, [brstd], [brstd], scale=-0.5)
                        tf, btf = tmpring.next()
                        O.stt(tf[:], xt[:], rstd[:, i:i + 1], A2row[:, b, :], ALU.mult, ALU.mult, [bxt, brstd, bA2r], [btf])
                        O.tt("pool", H2all[:, ti, :], tf[:], B2row[:, b, :], ALU.add, [btf, bA2r], [bH2[ti]])

            def b1_tr(scn):
                    for kc in range(KC):
                        tp, btp = TRring.next()
                        tpb = tp[:, :].bitcast(BF16)
                        for i in range(4):
                            ti = scn * 4 + i
                            O.tr(tpb[:, i * 128:(i + 1) * 128], H2all[:, ti, kc * 128:(kc + 1) * 128], identB[:],
                                 [bH2[ti], bconst], [btp])
                        O.act(h2T[:, kc, :], tpb[:, 0:512], AF.Copy, [btp], [bh2T])

            def b1_router_mm(scn):
                    pr, bpr = PRring.next()
                    for i in range(4):
                        for kc in range(KC):
                            O.mm(pr[:, i * 36:(i + 1) * 36], h2T[:, kc, i * 128:(i + 1) * 128], wrB[:, kc, :], kc == 0,
                                 kc == KC - 1, [bh2T, bconst], [bpr])
                    return pr, bpr

            def b1_router_math(scn, pr, bpr):
                    R = [brt_]
                    BIG = 1.0e30
                    t0 = scn * 4
                    lg3 = rb["lg"][:, :].rearrange("p (t c) -> p t c", t=4)
                    O.tt("dve", lg3, pr[:, 0:144].rearrange("p (t c) -> p t c", t=4),
                         brt[:, None, :].broadcast_to([128, 4, 36]), ALU.add, [bpr, bconst], R)
                    S_.op("dve", lambda e, lg3=lg3: e.tensor_reduce(out=rb["gmax"][:], in_=lg3[:, :, 0:4], axis=AX.X,
                                                                   op=ALU.max), R, R)
                    gm_b = rb["gmax"][:, :, None].broadcast_to([128, 4, 4])
                    O.tt("dve", rb["ohg"][:, :].rearrange("p (t g) -> p t g", t=4), lg3[:, :, 0:4], gm_b, ALU.is_equal, R, R)
                    O.tt("dve", rb["sh"][:, :].rearrange("p (t g) -> p t g", t=4), lg3[:, :, 0:4], gm_b, ALU.subtract, R, R)
                    O.act(rb["ex"][:], rb["sh"][:], AF.Exp, R, R)
                    S_.op("dve", lambda e: e.tensor_reduce(out=rb["sume"][:], in_=rb["ex"][:, :].rearrange("p (t g) -> p t g", t=4),
                                                          axis=AX.X, op=ALU.add), R, R)
                    S_.op("dve", lambda e: e.reciprocal(out=rb["psel"][:], in_=rb["sume"][:]), R, R)
                    O.ts("dve", rb["pen"][:], rb["ohg"][:], BIG, -BIG, ALU.mult, ALU.add, R, R)
                    O.tt("dve", rb["lem"][:, :].rearrange("p (t g e) -> p t g e", t=4, g=4),
                         lg3[:, :, 4:36].rearrange("p t (g e) -> p t g e", g=4),
                         rb["pen"][:, :].rearrange("p (t g) -> p t g", t=4)[:, :, :, None].broadcast_to([128, 4, 4, 8]),
                         ALU.add, R, R)
                    for i in range(4):
                        S_.op("dve", lambda e, i=i: e.max(out=rb["m8"][:, i * 8:(i + 1) * 8], in_=rb["lem"][:, i * 32:(i + 1) * 32]), R, R)
                    m83 = rb["m8"][:, :].rearrange("p (t k) -> p t k", t=4)
                    lem3 = rb["lem"][:, :].rearrange("p (t e) -> p t e", t=4)
                    O.tt("dve", OH1[:, t0:t0 + 4, :], lem3, m83[:, :, 0:1].broadcast_to([128, 4, NE]), ALU.is_equal,
                         R, bOH[t0:t0 + 4])
                    O.tt("dve", OH2[:, t0:t0 + 4, :], lem3, m83[:, :, 1:2].broadcast_to([128, 4, NE]), ALU.is_equal,
                         R, bOH[t0:t0 + 4])
                    O.tt("dve", rb["d21"][:, :, None], m83[:, :, 1:2], m83[:, :, 0:1], ALU.subtract, R, R)
                    O.act(rb["e2"][:], rb["d21"][:], AF.Exp, R, R)
                    O.ts("dve", rb["den"][:], rb["e2"][:], 1.0, None, ALU.add, None, R, R)
                    S_.op("dve", lambda e: e.reciprocal(out=rb["rden"][:], in_=rb["den"][:]), R, R)
                    wk3 = wk[:, t0 * 2:(t0 + 4) * 2].rearrange("p (t k) -> p t k", k=2)
                    O.tt("dve", wk3[:, :, 0:1], rb["rden"][:, :, None], rb["psel"][:, :, None], ALU.mult, R, [bslot])
                    O.tt("dve", wk3[:, :, 1:2], wk3[:, :, 0:1], rb["e2"][:, :, None], ALU.mult, R + [bslot], [bslot])


            NSCN = NTOK // 512
            prs = {}
            for scn in range(NSCN):
                b1_norm(scn)
                if scn > 0:
                    b1_router_math(scn - 1, *prs.pop(scn - 1))
                b1_tr(scn)
                prs[scn] = b1_router_mm(scn)
            b1_router_math(NSCN - 1, *prs.pop(NSCN - 1))

            Ab = h2T[:, :, :].rearrange("p k n -> p (k n)")[:, 0:NTT * NE].rearrange("p (t e) -> p t e", e=NE)
            O.tt("dve", Ab, OH1[:, :, :], OH2[:, :, :], ALU.add, bOH, [bh2T])
            O.copy("dve", striB[:], striF, [bconst], [bstri])
            O.memset("dve", onesBb[:], 1.0, [bstri])
            pc_, bpc_ = Dring.next()
            for ti in range(NTT):
                O.mm(pc_[:, 0:NE], onesBb[:], Ab[:, ti, :], ti == 0, ti == NTT - 1, [bstri, bh2T], [bpc_])
            O.ts("dve", cntf[:], pc_[:, 0:NE], 511.0, None, ALU.add, None, [bpc_], [bcnt])
            O.copy("dve", padi[:], cntf[:], [bcnt], [bcnt])
            O.ts("dve", padi[:], padi[:], 9, 9, ALU.arith_shift_right, ALU.logical_shift_left, [bcnt], [bcnt])
            O.copy("dve", padf[:], padi[:], [bcnt], [bcnt])
            O.memset("dve", base[:, 0:1], 0.0, [bcnt])
            for e in range(1, NE):
                O.tt("dve", base[:, e:e + 1], base[:, e - 1:e], padf[:, e - 1:e], ALU.add, [bcnt], [bcnt])
            O.memset("dve", texpf[:], -1.0, [bcnt])
            for e in range(NE):
                O.stt(texpf[:], jv[:, 0:NST], base[:, e:e + 1], texpf[:], ALU.is_ge, ALU.add, [bcnt], [bcnt])
            O.ts("dve", texpf[:], texpf[:], 128.0, jv[:, NST:NST + 1], ALU.mult, ALU.add, [bcnt], [bcnt])
            O.copy("dve", widx[:], texpf[:], [bcnt], [bconst])
            TPB = 16 if NTT >= 16 else NTT
            NBK = NTT // TPB
            Abk = rb["lem"][:, :].bitcast(BF16)[:, 0:NBK * NE].rearrange("p (b e) -> p b e", e=NE)
            for bk in range(NBK):
                def red(e, bk=bk):
                    with nc.allow_low_precision(reason="exact small integer counts"):
                        return e.tensor_reduce(
                            out=Abk[:, bk, :], in_=Ab[:, bk * TPB:(bk + 1) * TPB, :].rearrange("p t e -> p e t"),
                            axis=AX.X, op=ALU.add)
                S_.op("dve", red, [bh2T], [brt_])
            tfT, btfT = tmpring.next()
            for bk in range(NBK):
                pk, bpk = Dring.next()
                for tl in range(TPB):
                    ti = bk * TPB + tl
                    reg = pk[:, tl * NE:(tl + 1) * NE]
                    last = (tl == 0 and bk == 0)
                    O.mm(reg, striB[:], Ab[:, ti, :], True, last, [bstri, bh2T], [bpk])
                    srcs = [Ab[:, bk * TPB + tj, :] for tj in range(tl)] + [Abk[:, bj, :] for bj in range(bk)]
                    for n_, src in enumerate(srcs):
                        O.mm(reg, onesBb[:], src, False, n_ == len(srcs) - 1, [bstri, bh2T, brt_], [bpk])
                W = TPB * NE
                Tb = tfT[:, 0:W].rearrange("p (t e) -> p t e", e=NE)
                Tm = tfT[:, 512:512 + W].rearrange("p (t e) -> p t e", e=NE)
                O.tt("dve", Tb, pk[:, 0:W].rearrange("p (t e) -> p t e", e=NE),
                     base[:, None, :].broadcast_to([128, TPB, NE]), ALU.add, [bpk, bcnt], [btfT])
                for k2, OHk in ((0, OH1), (1, OH2)):
                    O.tt("dve", Tm, Tb, OHk[:, bk * TPB:(bk + 1) * TPB, :], ALU.mult, [btfT] + bOH[bk * TPB:(bk + 1) * TPB], [btfT])
                    S_.op("dve", lambda e, bk=bk, k2=k2, Tm=Tm: e.tensor_reduce(
                        out=slotf[:, bk * TPB * 2:(bk + 1) * TPB * 2].rearrange("p (t k) -> p t k", k=2)[:, :, k2],
                        in_=Tm, axis=AX.X, op=ALU.add), [btfT], [bslot])
            O.copy("dve", sloti[:], slotf[:], [bslot], [bslot])
            dtap("slotf", slotf[:], [bslot], [128, NTT * 2], F32)
            dtap("cntf", cntf[:], [bcnt], [128, NE], F32)
            dtap("base", base[:], [bcnt], [128, NE], F32)
            dtap("wk", wk[:], [bslot], [128, NTT * 2], F32)
            for ti in range(NTT):
                for k2 in range(2):
                    col = ti * 2 + k2
                    S_.op("pool", lambda e, ti=ti, col=col: e.indirect_dma_start(
                        out=Xs[:, :], out_offset=bass.IndirectOffsetOnAxis(ap=sloti[:, col:col + 1], axis=0),
                        in_=H2all[:, ti, :], in_offset=None), [bslot, bH2[ti]], (), chan="sc%d" % (col % 8))
            S_.finish()

        with contextlib.ExitStack() as ph:
            S_ = Sched(nc, "B2")
            tap_state["S"] = S_
            O = Ops(S_)
            NWS = 4
            wsl = [sbt(ph, "ews%d" % i, [128, 3, 2048], BF16) for i in range(NWS)]
            bwsl = [Buf("ews%d" % i) for i in range(NWS)]
            wring = Ring(list(range(NWS)))
            xs = [sbt(ph, "xs%d" % i, [128, D], BF16) for i in range(8)]
            xsring = Ring([(xs[i], Buf("xs%d" % i), "xs%d" % i) for i in range(8)])
            hTs2 = [(sbt(ph, "ehT%d" % i, [128, KC, 512], BF16), Buf("ehT%d" % i)) for i in range(2)]
            aT = [sbt(ph, "aT%d" % i, [128, 2, 512], BF16) for i in range(2)]
            baT = [Buf("aT%d" % i) for i in range(2)]
            sgt = [sbt(ph, "sgt%d" % i, [128, 512], BF16) for i in range(2)]
            sgring = Ring([(sgt[i], Buf("sgt%d" % i)) for i in range(2)])
            yt = [sbt(ph, "yt%d" % i, [128, D], BF16) for i in range(4)]
            yring = Ring([(yt[i], Buf("yt%d" % i), "yst%d" % i) for i in range(4)])
            GUring = Ring([(psum[i], pbuf[i]) for i in range(4)])
            Dring = Ring([(psum[i], pbuf[i]) for i in range(4, 8)])

            def load_weights(j):
                wi = wring.next()
                S_.op("pool", lambda e: [
                    e.indirect_dma_start(out=wsl[wi][:, 0:2, :].rearrange("p a n -> p (a n)"), out_offset=None,
                                         in_=W1[:, :],
                                         in_offset=bass.IndirectOffsetOnAxis(ap=widx[:, j:j + 1], axis=0)),
                    e.indirect_dma_start(out=wsl[wi][:, 2, :], out_offset=None, in_=W2[:, :],
                                         in_offset=bass.IndirectOffsetOnAxis(ap=widx[:, j:j + 1], axis=0))],
                    [bconst], [bwsl[wi]], chan="ews%d" % wi, ndma=2)
                return wi

            def do_down(args):
                j, wi, ai = args
                wdv = wsl[wi][:, 2, :].rearrange("p (f n) -> p f n", f=2)
                for i in range(4):
                    y_, by_, ych = yring.next()
                    for n in range(2):
                        pd, bpd = Dring.next()
                        for f in range(2):
                            O.mm(pd[:, :], aT[ai][:, f, i * 128:(i + 1) * 128], wdv[:, f, n * 512:(n + 1) * 512],
                                 f == 0, f == 1, [baT[ai], bwsl[wi]], [bpd])
                        if n == 0:
                            O.act(y_[:, 0:512], pd[:, :], AF.Copy, [bpd], [by_])
                        else:
                            O.copy("dve", y_[:, 512:1024], pd[:, :], [bpd], [by_])
                    r0 = j * 512 + i * 128
                    O.dma("act", Ybuf[r0:r0 + 128, :], y_[:], [by_], (), ych)

            def load_tr(j):
                hT2, bhT2 = hTs2[j % 2]
                xts = []
                for i in range(4):
                    x_, bx_, xch = xsring.next()
                    r0 = j * 512 + i * 128
                    O.dma("sp", x_[:], Xs[r0:r0 + 128, :], (), [bx_], xch)
                    xts.append((x_, bx_))
                for kc in range(KC):
                    tp, btp = Dring.next()
                    for i in range(4):
                        O.mm(tp[:, i * 128:(i + 1) * 128], xts[i][0][:, kc * 128:(kc + 1) * 128], identB[:], True, True,
                             [xts[i][1], bconst], [btp])
                    if kc % 2 == 0:
                        O.copy("dve", hT2[:, kc, :], tp[:, :], [btp], [bhT2])
                    else:
                        O.act(hT2[:, kc, :], tp[:, :], AF.Copy, [btp], [bhT2])

            pend_down = None
            wis = {0: load_weights(0)}
            load_tr(0)
            for j in range(NST):
                wi = wis.pop(j)
                hT2, bhT2 = hTs2[j % 2]
                if j + 1 < NST:
                    wis[j + 1] = load_weights(j + 1)
                    load_tr(j + 1)
                wgv = wsl[wi][:, 0, :].rearrange("p (k n) -> p k n", k=KC)
                wuv = wsl[wi][:, 1, :].rearrange("p (k n) -> p k n", k=KC)
                ai = j % 2
                for f in range(2):
                    pg, bpg = GUring.next()
                    pu, bpu = GUring.next()
                    for kc in range(KC):
                        O.mm(pg[:, :], wgv[:, kc, f * 128:(f + 1) * 128], hT2[:, kc, :], kc == 0, kc == KC - 1,
                             [bwsl[wi], bhT2], [bpg])
                    for kc in range(KC):
                        O.mm(pu[:, :], wuv[:, kc, f * 128:(f + 1) * 128], hT2[:, kc, :], kc == 0, kc == KC - 1,
                             [bwsl[wi], bhT2], [bpu])
                    sg_, bsg_ = sgring.next()
                    O.act(sg_[:], pg[:, :], AF.Silu, [bpg], [bsg_])
                    O.tt("dve", aT[ai][:, f, :], sg_[:], pu[:, :], ALU.mult, [bsg_, bpu], [baT[ai]])
                if pend_down is not None:
                    do_down(pend_down)
                pend_down = (j, wi, ai)
            do_down(pend_down)

            S_.fence("pool", "yst")
            y1r = [sbt(ph, "y1r%d" % i, [128, D], BF16) for i in range(4)]
            y2r = [sbt(ph, "y2r%d" % i, [128, D], BF16) for i in range(4)]
            ygring = Ring([(y1r[i], y2r[i], Buf("yg%d" % i), "yg%d" % i) for i in range(4)])
            xr2 = [sbt(ph, "fxr%d" % i, [128, D], F32) for i in range(4)]
            xring2 = Ring([(xr2[i], Buf("fxr%d" % i), "fxr%d" % i) for i in range(4)])
            tf2 = [sbt(ph, "tf2_%d" % i, [128, D], F32) for i in range(4)]
            tring2 = Ring([(tf2[i], Buf("tf2_%d" % i)) for i in range(4)])
            otile = [sbt(ph, "otile%d" % i, [128, D], F32) for i in range(4)]
            oring = Ring([(otile[i], Buf("otile%d" % i), "ost%d" % i) for i in range(4)])
            for tp in range(0, NTT, 2):
                grp = []
                for ti in range(tp, min(tp + 2, NTT)):
                    b = (ti * 128) // S
                    y1, y2, byg, gch = ygring.next()
                    S_.op("pool", lambda e, ti=ti, y1=y1, y2=y2: [
                        e.indirect_dma_start(out=y1[:, :], out_offset=None, in_=Ybuf[:, :],
                                             in_offset=bass.IndirectOffsetOnAxis(ap=sloti[:, ti * 2:ti * 2 + 1], axis=0)),
                        e.indirect_dma_start(out=y2[:, :], out_offset=None, in_=Ybuf[:, :],
                                             in_offset=bass.IndirectOffsetOnAxis(ap=sloti[:, ti * 2 + 1:ti * 2 + 2], axis=0))],
                        [bconst], [byg], chan=gch, ndma=2)
                    xt, bxt, chn = xring2.next()
                    O.dma("sp", xt[:], out_d[ti * 128:(ti + 1) * 128, :], [b_outd[ti]], [bxt], chn)
                    tf, btf = tring2.next()
                    ot, bot, och = oring.next()
                    grp.append((ti, b, y1, y2, byg, xt, bxt, tf, btf, ot, bot, och))
                for (ti, b, y1, y2, byg, xt, bxt, tf, btf, ot, bot, och) in grp:
                    O.act(tf[:], y1[:], AF.Copy, [byg, bconst], [btf], scale=wk[:, ti * 2:ti * 2 + 1])
                for (ti, b, y1, y2, byg, xt, bxt, tf, btf, ot, bot, och) in grp:
                    O.stt(tf[:], y2[:], wk[:, ti * 2 + 1:ti * 2 + 2], tf[:], ALU.mult, ALU.add, [byg, bconst, btf], [btf])
                for (ti, b, y1, y2, byg, xt, bxt, tf, btf, ot, bot, och) in grp:
                    O.tt("dve", tf[:], tf[:], g2bc[:, b, :], ALU.mult, [btf, bconst], [btf])
                for (ti, b, y1, y2, byg, xt, bxt, tf, btf, ot, bot, och) in grp:
                    O.tt("dve", ot[:], tf[:], xt[:], ALU.add, [btf, bxt], [bot])
                    O.dma("act", out_d[ti * 128:(ti + 1) * 128, :], ot[:], [bot], [b_outd[ti]], och)
            S_.finish()
    return nc


def _consts():
    c = np.zeros((128, 6, 128), np.float32)
    c[:, 0, :] = np.eye(128, dtype=np.float32)
    c[:, 1, :] = np.triu(np.ones((128, 128), np.float32))
    c[127, 2, :] = 1.0
    c[0:64, 3, 0:64] = 1.0 / 64
    c[64:128, 3, 64:128] = 1.0 / 64
    c[:, 4, :] = 1.0
    c[:, 5, :] = np.triu(np.ones((128, 128), np.float32), k=1)
    return c


def _jv(nst):
    j = np.zeros((128, nst + 1), np.float32)
    j[:, 0:nst] = 512.0 * np.arange(nst, dtype=np.float32)[None, :]
    j[:, nst] = np.arange(128, dtype=np.float32)
    return j


def _fm(v):
    return np.ascontiguousarray(np.asarray(v, np.float32).reshape(-1, 128).T)


def make_in_maps(inputs, n_cores, NB, S):
    f = lambda a: np.ascontiguousarray(np.asarray(a, np.float32))
    x = f(inputs["x"])
    c = f(inputs["c"])
    shared = {
        "w_ada": f(inputs["w_ada"][0]),
        "b_adaT": _fm(inputs["b_ada"][0]),
        "n1wT": _fm(inputs["norm1_w"][0]),
        "n2wT": _fm(inputs["norm2_w"][0]),
        "w_in": f(inputs["w_in"][0]),
        "bfg": f(np.broadcast_to(np.asarray(inputs["b_forget"][0], np.float32)[None, :], (128, NH))),
        "convT": np.ascontiguousarray(np.asarray(inputs["conv_w"][0], np.float32).reshape(3, 4, 128).transpose(2, 1, 0)),
        "qkw": np.ascontiguousarray(np.stack([np.tile(np.asarray(inputs["q_norm_w"][0], np.float32), 2),
                                              np.tile(np.asarray(inputs["k_norm_w"][0], np.float32), 2)], axis=1)),
        "w_oc": f(inputs["w_out_conv"][0]),
        "w_oa": f(inputs["w_out_attn"][0]),
        "w_o": f(inputs["w_o"][0]),
        "w_r": np.ascontiguousarray(np.concatenate([np.asarray(inputs["w_router_group"][0], np.float32),
                                                    np.asarray(inputs["w_router_expert"][0], np.float32)], axis=1)),
        "b_r": f(np.broadcast_to(np.concatenate([np.asarray(inputs["b_router_group"][0], np.float32),
                                                 np.asarray(inputs["b_router_expert"][0], np.float32)])[None, :], (128, 36))),
        "w_gate": f(inputs["w_gate"][0]),
        "w_up": f(inputs["w_up"][0]),
        "w_down": f(inputs["w_down"][0]),
        "consts": _consts(),
        "zeros_bf": np.zeros((1024, D), dtype=ml_dtypes.bfloat16),
        "jv": _jv((2 * NB * S) // 512 + NE),
    }
    maps = []
    for core in range(n_cores):
        bs = slice(core * NB, (core + 1) * NB)
        m = dict(shared)
        m["x"] = np.ascontiguousarray(x[bs].reshape(NB * S, D))
        cb = c[bs]
        m["cT"] = np.ascontiguousarray(cb.reshape(NB, KC, 128).transpose(2, 1, 0))
        maps.append(m)
    return maps


def kernel(**inputs):
    x = np.asarray(inputs["x"])
    B, S, _ = x.shape
    n_cores = 8
    NB = B // n_cores
    nc = build_nc(NB, S)
    in_maps = make_in_maps(inputs, n_cores, NB, S)
    res = run_bass_kernel_spmd(nc, in_maps, core_ids=list(range(n_cores)))
    outs = [np.asarray(r["out"]).reshape(NB, S, D) for r in res.results]
    return np.concatenate(outs, axis=0).astype(np.float32)
```
